# Optimizing a Trainium2 kernel written in Bass

```python
import jax, jax.numpy as jnp
from jax import lax
import numpy as np

D_MODEL = 1024
BATCH = 2
SEQ = 8192
DEPTH = 2

GRID_W = 64
CTX_LEN = 256
EPS = 1e-6
ROPE_THETA = 10000.0
Q_BLOCK = 128
N_MOD = 6
FNET_GROUPS = 4
FNET_GROUP_DIM = 128
FNET_WIDTH = FNET_GROUPS * FNET_GROUP_DIM
MLA_HEADS = 8
MLA_Q_LORA = 256
MLA_KV_LORA = 128
MLA_NOPE = 64
MLA_ROPE = 32
MLA_V = 64
MLA_QK = MLA_NOPE + MLA_ROPE
AB_IN = FNET_WIDTH + MLA_Q_LORA + MLA_KV_LORA + MLA_ROPE
AB_OUT = FNET_WIDTH + MLA_HEADS * MLA_V
GQA_HEADS = 8
GQA_KV_HEADS = 2
GQA_GROUP = GQA_HEADS // GQA_KV_HEADS
GQA_HEAD_DIM = 128
C_IN = (GQA_HEADS + 2 * GQA_KV_HEADS) * GQA_HEAD_DIM
C_OUT = GQA_HEADS * GQA_HEAD_DIM
N_EXPERTS = 16
EXPERT_FF = 2048
CAPACITY_FACTOR = 2
N_EVEN = (DEPTH + 1) // 2
N_ODD = DEPTH // 2

kernel_name = "hybrid_fnet_mla_gqa_ecmoe_dit"


def rms_norm(x, g):
    xf = x.astype(jnp.float32)
    y = xf * lax.rsqrt(jnp.mean(xf * xf, axis=-1, keepdims=True) + EPS)
    return (y * g.astype(jnp.float32)).astype(x.dtype)


def modulate(x, g, shift, scale):
    return rms_norm(x, g) * (1 + scale) + shift


def rope_1d(x, pos):
    half = x.shape[-1] // 2
    inv = ROPE_THETA ** (-jnp.arange(half, dtype=jnp.float32) / half)
    ang = pos[:, None] * inv[None, :]
    cos = jnp.cos(ang)[None, :, None, :]
    sin = jnp.sin(ang)[None, :, None, :]
    xf = x.astype(jnp.float32)
    x1, x2 = xf[..., :half], xf[..., half:]
    return jnp.concatenate([x1 * cos - x2 * sin, x1 * sin + x2 * cos], axis=-1).astype(x.dtype)


def axial_rope(x, row, col):
    half = x.shape[-1] // 2
    return jnp.concatenate([rope_1d(x[..., :half], row), rope_1d(x[..., half:], col)], axis=-1)


def block_attention(q, k, v, scale):
    B, S, Hk, G, dk = q.shape
    dv = v.shape[-1]
    nb = S // Q_BLOCK
    qb = q.reshape(B, nb, Q_BLOCK, Hk, G, dk).transpose(1, 0, 2, 3, 4, 5)

    def one_block(qi):
        s = jnp.einsum('bqkgd,btkd->bkgqt', qi, k).astype(jnp.float32) * scale
        p = jax.nn.softmax(s, axis=-1).astype(v.dtype)
        return jnp.einsum('bkgqt,btkd->bqkgd', p, v)

    o = lax.map(one_block, qb)
    return o.transpose(1, 0, 2, 3, 4, 5).reshape(B, S, Hk, G, dv)


def fourier_mix(f):
    B, S, _ = f.shape
    ff = f.reshape(B, S, FNET_GROUPS, FNET_GROUP_DIM).astype(jnp.float32)
    y = jnp.fft.fft2(ff, axes=(1, 3), norm='ortho').real
    return y.reshape(B, S, FNET_WIDTH).astype(f.dtype)


def mla_project(proj, q_norm, w_uq, kv_norm, w_ukv, row, col):
    B, S, _ = proj.shape
    o = FNET_WIDTH
    f = proj[..., :o]
    cq = proj[..., o:o + MLA_Q_LORA]
    o += MLA_Q_LORA
    ckv = proj[..., o:o + MLA_KV_LORA]
    o += MLA_KV_LORA
    kr = proj[..., o:o + MLA_ROPE][:, :, None, :]
    q = (rms_norm(cq, q_norm) @ w_uq).reshape(B, S, MLA_HEADS, MLA_QK)
    kv = (rms_norm(ckv, kv_norm) @ w_ukv).reshape(B, S, MLA_HEADS, MLA_NOPE + MLA_V)
    q_nope, q_rope = q[..., :MLA_NOPE], q[..., MLA_NOPE:]
    k_nope, v = kv[..., :MLA_NOPE], kv[..., MLA_NOPE:]
    if row is not None:
        q_rope = axial_rope(q_rope, row, col)
        kr = axial_rope(kr, row, col)
    q = jnp.concatenate([q_nope, q_rope], axis=-1)
    k = jnp.concatenate([k_nope, jnp.broadcast_to(kr, (B, S, MLA_HEADS, MLA_ROPE))], axis=-1)
    return f, q, k, v


def mixer_ab(h_lat, h_ctx, w_in, q_norm, w_uq, kv_norm, w_ukv, w_o, row, col, ctx_out):
    B, S, _ = h_lat.shape
    scale = MLA_QK ** -0.5
    f_l, q_l, k_l, v_l = mla_project(h_lat @ w_in, q_norm, w_uq, kv_norm, w_ukv, row, col)
    f_c, q_c, k_c, v_c = mla_project(h_ctx @ w_in, q_norm, w_uq, kv_norm, w_ukv, None, None)
    k_all = jnp.concatenate([k_c, k_l], axis=1)
    v_all = jnp.concatenate([v_c, v_l], axis=1)
    a_l = block_attention(q_l[:, :, :, None, :], k_all, v_all, scale).reshape(B, S, MLA_HEADS * MLA_V)
    y_l = jnp.concatenate([fourier_mix(f_l), a_l], axis=-1) @ w_o
    y_c = None
    if ctx_out:
        L = h_ctx.shape[1]
        a_c = block_attention(q_c[:, :, :, None, :], k_c, v_c, scale).reshape(B, L, MLA_HEADS * MLA_V)
        y_c = jnp.concatenate([fourier_mix(f_c), a_c], axis=-1) @ w_o
    return y_l, y_c


def gqa_project(h, w_in, q_gain, k_gain, row, col):
    B, S, _ = h.shape
    p = h @ w_in
    nq = GQA_HEADS * GQA_HEAD_DIM
    nk = GQA_KV_HEADS * GQA_HEAD_DIM
    q = rms_norm(p[..., :nq].reshape(B, S, GQA_HEADS, GQA_HEAD_DIM), q_gain)
    k = rms_norm(p[..., nq:nq + nk].reshape(B, S, GQA_KV_HEADS, GQA_HEAD_DIM), k_gain)
    v = p[..., nq + nk:].reshape(B, S, GQA_KV_HEADS, GQA_HEAD_DIM)
    if row is not None:
        q = axial_rope(q, row, col)
        k = axial_rope(k, row, col)
    return q.reshape(B, S, GQA_KV_HEADS, GQA_GROUP, GQA_HEAD_DIM), k, v


def mixer_c(h_lat, h_ctx, w_in, q_gain, k_gain, w_o, row, col, ctx_out):
    B, S, _ = h_lat.shape
    scale = GQA_HEAD_DIM ** -0.5
    q_l, k_l, v_l = gqa_project(h_lat, w_in, q_gain, k_gain, row, col)
    q_c, k_c, v_c = gqa_project(h_ctx, w_in, q_gain, k_gain, None, None)
    k_all = jnp.concatenate([k_c, k_l], axis=1)
    v_all = jnp.concatenate([v_c, v_l], axis=1)
    y_l = block_attention(q_l, k_all, v_all, scale).reshape(B, S, C_OUT) @ w_o
    y_c = None
    if ctx_out:
        L = h_ctx.shape[1]
        y_c = block_attention(q_c, k_c, v_c, scale).reshape(B, L, C_OUT) @ w_o
    return y_l, y_c


def expert_choice_ffn(h, w_router, w_gate, w_up, w_down):
    B, N, D = h.shape
    cap = CAPACITY_FACTOR * N // N_EXPERTS
    logits = jnp.einsum('bnd,de->bne', h, w_router).astype(jnp.float32)
    aff = jax.nn.softmax(logits, axis=-1)
    g, idx = lax.top_k(jnp.swapaxes(aff, 1, 2), cap)
    xs = jax.vmap(lambda hb, ib: hb[ib])(h, idx)
    a = jnp.einsum('becd,edf->becf', xs, w_gate)
    u = jnp.einsum('becd,edf->becf', xs, w_up)
    y = jnp.einsum('becf,efd->becd', jax.nn.silu(a) * u, w_down)
    y = y * g[..., None].astype(y.dtype)
    return jax.vmap(lambda yb, ib: jnp.zeros((N, D), yb.dtype).at[ib.reshape(-1)].add(yb.reshape(-1, D)))(y, idx)


def setup_inputs(seed: int = 0) -> dict:
    key = jax.random.key(seed)
    ks = jax.random.split(key, 24)
    D = D_MODEL
    nrm = lambda k, shape, s: jax.random.normal(k, shape, jnp.float32) * s
    gain = lambda k, shape: 1.0 + 0.05 * jax.random.normal(k, shape, jnp.float32)
    return {
        "x": nrm(ks[0], (BATCH, SEQ, D), 1.0),
        "c": nrm(ks[1], (BATCH, D), 1.0),
        "ctx": nrm(ks[2], (BATCH, CTX_LEN, D), 1.0),
        "c_ctx": nrm(ks[3], (D,), 1.0),
        "ada_w": nrm(ks[4], (DEPTH, D, N_MOD * D), 0.5 * D ** -0.5),
        "ada_b": nrm(ks[5], (DEPTH, N_MOD * D), 0.02),
        "norm1": gain(ks[6], (DEPTH, D)),
        "norm2": gain(ks[7], (DEPTH, D)),
        "ab_w_in": nrm(ks[8], (N_EVEN, D, AB_IN), D ** -0.5),
        "ab_q_norm": gain(ks[9], (N_EVEN, MLA_Q_LORA)),
        "ab_w_uq": nrm(ks[10], (N_EVEN, MLA_Q_LORA, MLA_HEADS * MLA_QK), MLA_Q_LORA ** -0.5),
        "ab_kv_norm": gain(ks[11], (N_EVEN, MLA_KV_LORA)),
        "ab_w_ukv": nrm(ks[12], (N_EVEN, MLA_KV_LORA, MLA_HEADS * (MLA_NOPE + MLA_V)), MLA_KV_LORA ** -0.5),
        "ab_w_o": nrm(ks[13], (N_EVEN, AB_OUT, D), AB_OUT ** -0.5),
        "c_w_in": nrm(ks[14], (N_ODD, D, C_IN), D ** -0.5),
        "c_q_gain": gain(ks[15], (N_ODD, GQA_HEAD_DIM)),
        "c_k_gain": gain(ks[16], (N_ODD, GQA_HEAD_DIM)),
        "c_w_o": nrm(ks[17], (N_ODD, C_OUT, D), C_OUT ** -0.5),
        "moe_router": nrm(ks[18], (DEPTH, D, N_EXPERTS), D ** -0.5),
        "moe_w_gate": nrm(ks[19], (DEPTH, N_EXPERTS, D, EXPERT_FF), D ** -0.5),
        "moe_w_up": nrm(ks[20], (DEPTH, N_EXPERTS, D, EXPERT_FF), D ** -0.5),
        "moe_w_down": nrm(ks[21], (DEPTH, N_EXPERTS, EXPERT_FF, D), EXPERT_FF ** -0.5),
        "final_norm": gain(ks[22], (D,)),
    }


def reference(x, c, ctx, c_ctx, ada_w, ada_b, norm1, norm2, ab_w_in, ab_q_norm, ab_w_uq, ab_kv_norm,
              ab_w_ukv, ab_w_o, c_w_in, c_q_gain, c_k_gain, c_w_o, moe_router, moe_w_gate, moe_w_up,
              moe_w_down, final_norm):
    B, S, D = x.shape
    rows = S // GRID_W
    row = jnp.repeat(jnp.arange(rows, dtype=jnp.float32), GRID_W)
    col = jnp.tile(jnp.arange(GRID_W, dtype=jnp.float32), rows)
    h_lat, h_ctx = x, ctx
    for i in range(DEPTH):
        last = i == DEPTH - 1
        mod_l = (jax.nn.silu(c) @ ada_w[i] + ada_b[i])[:, None, :]
        mod_c = jax.nn.silu(c_ctx) @ ada_w[i] + ada_b[i]
        sh1_l, sc1_l, g1_l, sh2_l, sc2_l, g2_l = jnp.split(mod_l, N_MOD, axis=-1)
        sh1_c, sc1_c, g1_c, sh2_c, sc2_c, g2_c = jnp.split(mod_c, N_MOD, axis=-1)
        a_l = modulate(h_lat, norm1[i], sh1_l, sc1_l)
        a_c = modulate(h_ctx, norm1[i], sh1_c, sc1_c)
        j = i // 2
        if i % 2 == 0:
            y_l, y_c = mixer_ab(a_l, a_c, ab_w_in[j], ab_q_norm[j], ab_w_uq[j], ab_kv_norm[j],
                                ab_w_ukv[j], ab_w_o[j], row, col, not last)
        else:
            y_l, y_c = mixer_c(a_l, a_c, c_w_in[j], c_q_gain[j], c_k_gain[j], c_w_o[j], row, col, not last)
        h_lat = h_lat + g1_l * y_l
        m_l = modulate(h_lat, norm2[i], sh2_l, sc2_l)
        h_lat = h_lat + g2_l * expert_choice_ffn(m_l, moe_router[i], moe_w_gate[i], moe_w_up[i], moe_w_down[i])
        if not last:
            h_ctx = h_ctx + g1_c * y_c
            m_c = modulate(h_ctx, norm2[i], sh2_c, sc2_c)
            h_ctx = h_ctx + g2_c * expert_choice_ffn(m_c, moe_router[i], moe_w_gate[i], moe_w_up[i], moe_w_down[i])
    return rms_norm(h_lat, final_norm)
```

```python
import contextlib
import math
import numpy as np
import ml_dtypes
import concourse.bass as bass
import concourse.mybir as mybir
from concourse.bass_utils import run_bass_kernel_spmd

F32 = mybir.dt.float32
BF16 = mybir.dt.bfloat16
I32 = mybir.dt.int32
ALU = mybir.AluOpType
AF = mybir.ActivationFunctionType
AX = mybir.AxisListType

D = 1024
S_LAT = 8192
L_CTX = 256
EPS = 1e-6
NT = 66
NQ = 18
NKEY = NT * 128
NQRY = NQ * 128
GROUPS = [[0, 1, 2, 3], [4, 5, 6, 7]]
DEBUG = {}


class Sched:
    ENGS = ("sp", "act", "dve", "pool", "pe")

    def __init__(self, nc, n_streams=20):
        self.nc = nc
        self.ops = []
        self.last_w = {}
        self.readers = {}
        self.n_streams = n_streams
        self.stream_last = {}
        self.rr = 0
        self.pool_regs = {}
        self.pool_reg_vals = [8191, 12 * 4 * 352 - 1]

    def op(self, eng, fn, r=(), w=(), ndma=0, stream=None, inc=16, after=()):
        i = len(self.ops)
        deps = {d: True for d in after}
        w = list(w) + [k for k in r if isinstance(k, tuple) and k[0] == "ps" and k not in w]
        for k in r:
            d = self.last_w.get(k)
            if d is not None:
                deps[d] = True
        for k in w:
            d = self.last_w.get(k)
            if d is not None:
                deps.setdefault(d, False)
            for d in self.readers.get(k, ()):
                deps.setdefault(d, False)
        if ndma:
            if stream is None:
                stream = self.rr % self.n_streams
                self.rr += 1
            p = self.stream_last.get(stream)
            if p is not None:
                deps[p] = True
            self.stream_last[stream] = i
        for k in w:
            self.last_w[k] = i
            self.readers[k] = []
        for k in r:
            lst = self.readers.setdefault(k, [])
            if not ndma:
                lst[:] = [j for j in lst if self.ops[j]["ndma"] or self.ops[j]["eng"] != eng]
            lst.append(i)
        fdeps = []
        for d, raw in deps.items():
            o = self.ops[d]
            if o["fn"] is None:
                continue
            if ndma or o["ndma"]:
                fdeps.append(d)
            elif o["eng"] == eng:
                if (raw and eng != "pe") or eng == "pool":
                    fdeps.append(d)
            else:
                fdeps.append(d)
        self.ops.append(dict(eng=eng, fn=fn, deps=fdeps, ndma=ndma, stream=stream, sig=None, inc=inc))
        return i

    def dma(self, fn, r=(), w=(), n=1, eng="sp", stream=None, after=()):
        return self.op(eng, fn, r, w, ndma=n, stream=stream, after=after)

    def cc(self, fn, r=(), w=(), after=()):
        return self.op("pool", fn, r, w, ndma=1, stream="cc", inc=1, after=after)

    def barrier(self):
        last = {}
        for i, o in enumerate(self.ops):
            if o["fn"] is None:
                continue
            if o["ndma"]:
                last[("d", o["stream"])] = i
            else:
                last[o["eng"]] = i
        deps = list(last.values())
        for e in self.ENGS:
            self.ops.append(dict(eng=e, fn=None, deps=list(deps), ndma=0, stream=None, sig=None, inc=0))
        self.last_w = {}
        self.readers = {}

    def emit(self):
        nc = self.nc
        ops = self.ops
        need = [False] * len(ops)
        for o in ops:
            for d in o["deps"]:
                need[d] = True
        with contextlib.ExitStack() as st:
            esem = {e: st.enter_context(nc.semaphore("s_" + e)) for e in self.ENGS}
            ssem = {}
            cnt = {}
            for i, o in enumerate(ops):
                if o["ndma"]:
                    s = o["stream"]
                    if s not in ssem:
                        ssem[s] = st.enter_context(nc.semaphore("d_%s" % (s,)))
                    cnt[("d", s)] = cnt.get(("d", s), 0) + o["inc"] * o["ndma"]
                    o["sig"] = (ssem[s], cnt[("d", s)], ("d", s))
                elif need[i]:
                    e = o["eng"]
                    cnt[e] = cnt.get(e, 0) + 1
                    o["sig"] = (esem[e], cnt[e], e)
            block = st.enter_context(nc.Block())

            def run(engname, eng):
                known = {}
                if engname == "pool":
                    for v in self.pool_reg_vals:
                        self.pool_regs[v] = eng.to_reg(v)
                for o in ops:
                    if o["eng"] != engname:
                        continue
                    for d in o["deps"]:
                        sem, val, key = ops[d]["sig"]
                        if known.get(key, 0) < val:
                            eng.wait_ge(sem, val)
                            known[key] = val
                    if o["fn"] is None:
                        continue
                    inst = o["fn"](eng)
                    if o["sig"] is not None:
                        sem, val, key = o["sig"]
                        if o["ndma"]:
                            insts = inst if isinstance(inst, (list, tuple)) else [inst]
                            assert len(insts) == o["ndma"], (len(insts), o["ndma"])
                            for ins in insts:
                                ins.then_inc(sem, o["inc"])
                        else:
                            inst.then_inc(sem, 1)

            used = set(o["eng"] for o in ops)
            if "sp" in used:
                @block.sync
                def _(e):
                    run("sp", e)
            if "act" in used:
                @block.scalar
                def _(e):
                    run("act", e)
            if "dve" in used:
                @block.vector
                def _(e):
                    run("dve", e)
            if "pool" in used:
                @block.gpsimd
                def _(e):
                    run("pool", e)
            if "pe" in used:
                @block.tensor
                def _(e):
                    run("pe", e)


class Region:
    def __init__(self, nc, base, size, tag):
        self.nc, self.base, self.size, self.tag = nc, base, size, tag
        self.ptr = 0
        self.n = 0

    def alloc(self, name, shape, dt):
        esz = 2 if dt == BF16 else 4
        nb = esz
        for s in shape[1:]:
            nb *= s
        nb = (nb + 63) // 64 * 64
        off = self.ptr
        self.ptr += nb
        assert self.ptr <= self.size, ("region overflow", self.tag, name, self.ptr, self.size)
        self.n += 1
        return self.nc.alloc_sbuf_tensor_at("%s_%s%d" % (name, self.tag, self.n), list(shape), dt,
                                            offset=self.base + off)

    def reset(self):
        self.ptr = 0


def _rope_tables_l0():
    t = np.arange(S_LAT)
    row = (t // 64).astype(np.float64)
    col = (t % 64).astype(np.float64)
    inv = 10000.0 ** (-np.arange(8, dtype=np.float64) / 8.0)
    C = np.zeros((32, S_LAT)); Sg = np.zeros((32, S_LAT))
    for d in range(32):
        pos = row if d < 16 else col
        j = d % 8
        ang = pos * inv[j]
        C[d] = np.cos(ang)
        Sg[d] = (-1.0 if (d % 16) < 8 else 1.0) * np.sin(ang)
    return C.astype(np.float32), Sg.astype(np.float32)


SW16 = np.array([d + 8 if (d % 16) < 8 else d - 8 for d in range(32)])


def host_prep(inp):
    f32 = np.float32
    x = np.asarray(inp["x"], f32); ctx = np.asarray(inp["ctx"], f32)
    c = np.asarray(inp["c"], f32); c_ctx = np.asarray(inp["c_ctx"], f32)
    ada_w = np.asarray(inp["ada_w"], f32); ada_b = np.asarray(inp["ada_b"], f32)
    shared = {}
    w_in = np.asarray(inp["ab_w_in"], f32)[0]
    f_c, cq_c, ckv_c, kr_c = w_in[:, 0:512], w_in[:, 512:768], w_in[:, 768:896], w_in[:, 896:928]
    z64 = np.zeros((1024, 64), f32)
    shared["w_in"] = np.ascontiguousarray(np.concatenate([f_c, ckv_c, z64, kr_c, z64, kr_c[:, SW16], cq_c], 1))
    w_uq = np.asarray(inp["ab_w_uq"], f32)[0].reshape(256, 8, 96)
    shared["w_uq"] = np.ascontiguousarray(w_uq.reshape(256, 768))
    wqs = np.concatenate([np.zeros((256, 8, 64), f32), w_uq[:, :, 64:96][:, :, SW16]], 2)
    shared["w_uqsw"] = np.ascontiguousarray(wqs.reshape(256, 768))
    w_ukv = np.asarray(inp["ab_w_ukv"], f32)[0].reshape(128, 8, 128)
    shared["w_uk"] = np.ascontiguousarray(w_ukv[:, :, 0:64].reshape(128, 512))
    shared["w_uv"] = np.ascontiguousarray(w_ukv[:, :, 64:128].reshape(128, 512))
    shared["ab_w_o"] = np.asarray(inp["ab_w_o"], f32)[0]
    shared["q_norm"] = np.ascontiguousarray(np.asarray(inp["ab_q_norm"], f32)[0].reshape(2, 128).T)
    shared["kv_norm"] = np.ascontiguousarray(np.asarray(inp["ab_kv_norm"], f32)[0].reshape(128, 1))
    shared["norm1"] = np.asarray(inp["norm1"], f32)
    shared["norm2"] = np.asarray(inp["norm2"], f32)
    C32, S32 = _rope_tables_l0()
    shared["ident_f"] = np.eye(128, dtype=f32)
    bf = ml_dtypes.bfloat16
    i128 = np.arange(128, dtype=np.float64)
    a128 = 2 * np.pi * np.outer(i128, i128) / 128.0
    shared["C128b"] = np.cos(a128).astype(bf); shared["S128b"] = np.sin(a128).astype(bf)
    atw = 2 * np.pi * np.outer(i128, np.arange(64, dtype=np.float64)) / 8192.0
    shared["tw"] = np.ascontiguousarray(np.stack([np.cos(atw), np.sin(atw), -np.cos(atw)], 1).astype(f32))
    t256 = np.arange(256, dtype=np.float64)
    a256 = 2 * np.pi * np.outer(t256, t256) / 256.0
    shared["C256b"] = np.ascontiguousarray(np.cos(a256).reshape(2, 128, 256).transpose(1, 0, 2)).astype(bf)
    shared["NS256b"] = np.ascontiguousarray((-np.sin(a256)).reshape(2, 128, 256).transpose(1, 0, 2)).astype(bf)
    wg_all = np.asarray(inp["moe_w_gate"], f32); wu_all = np.asarray(inp["moe_w_up"], f32)
    wd_all = np.asarray(inp["moe_w_down"], f32)
    shared["w_router"] = np.asarray(inp["moe_router"], f32)
    pp = np.arange(128)[:, None]; tt = np.arange(64)[None, :]
    rowid = ((tt % 16) // 4) * 2048 + (tt // 16) * 512 + 128 * (tt % 4) + pp
    shared["rowid"] = np.ascontiguousarray(np.stack([rowid % 64, rowid // 64], -1)).astype(bf)
    shared["iota_row"] = np.tile(np.arange(1024, dtype=f32)[None, :], (128, 1))
    shared["ustrict"] = np.triu(np.ones((128, 128), f32), 1).astype(bf)
    tg = np.zeros((128, 2, 16), f32); tg[:, 0, :] = 1024.0; tg[:, 1, :] = 32.0
    shared["tgt"] = tg
    e16 = np.arange(16)
    cb = (3 * (e16 % 4)) * 1408 + (e16 // 4) * 352
    shared["cbe"] = np.tile(cb.astype(f32)[None, :], (128, 1))
    shared["c_w_in"] = np.asarray(inp["c_w_in"], f32)[0]
    shared["c_w_o"] = np.asarray(inp["c_w_o"], f32)[0]
    shared["c_q_gain"] = np.asarray(inp["c_q_gain"], f32)[0]
    shared["c_k_gain"] = np.asarray(inp["c_k_gain"], f32)[0]
    shared["final_norm"] = np.asarray(inp["final_norm"], f32)
    tt_ = np.arange(S_LAT); row_ = (tt_ // 64).astype(np.float64); col_ = (tt_ % 64).astype(np.float64)
    inv32 = 10000.0 ** (-np.arange(32, dtype=np.float64) / 32.0)
    ar = row_[:, None] * inv32[None, :]; ac = col_[:, None] * inv32[None, :]
    cos1 = np.concatenate([np.cos(ar), np.cos(ar), np.cos(ac), np.cos(ac)], 1).astype(f32)
    sin1 = np.concatenate([-np.sin(ar), np.sin(ar), -np.sin(ac), np.sin(ac)], 1).astype(f32)
    per_core = []
    for r in range(8):
        b, q = r // 4, r % 4
        m = dict(shared)
        m["x_own"] = np.ascontiguousarray(x[b, 2048 * q:2048 * (q + 1)])
        m["ctx_b"] = ctx[b]
        cT = np.stack([c[b], c_ctx], 0)
        m["cT"] = np.ascontiguousarray(cT.reshape(2, 8, 128).transpose(2, 1, 0))
        sh = np.stack([ada_w[v // 6][:, (v % 6) * 1024 + 256 * q:(v % 6) * 1024 + 256 * q + 256] for v in range(12)], 1)
        m["ada_s"] = np.ascontiguousarray(sh.reshape(1024, 3072))
        m["ada_bs"] = np.ascontiguousarray(
            np.stack([ada_b[v // 6][(v % 6) * 1024 + 256 * q:(v % 6) * 1024 + 256 * q + 256] for v in range(12)], 0).reshape(3072))
        a64 = 2 * np.pi * np.outer(np.arange(64, dtype=np.float64), np.arange(16 * q, 16 * q + 16, dtype=np.float64)) / 64.0
        m["dft64o"] = np.ascontiguousarray(np.stack([np.concatenate([np.cos(a64), -np.sin(a64)], 1),
                                                       np.concatenate([np.sin(a64), np.cos(a64)], 1)], 1)).astype(bf)
        m["moe_g"] = np.ascontiguousarray(wg_all[:, 4 * q:4 * q + 4])
        m["moe_u"] = np.ascontiguousarray(wu_all[:, 4 * q:4 * q + 4])
        m["moe_d"] = np.ascontiguousarray(wd_all[:, 4 * q:4 * q + 4])
        es = np.zeros((128, 4, 16), f32)
        for j in range(4):
            es[:, j, 4 * q + j] = 1.0
        m["esel"] = es
        lt = np.zeros((128, 4), f32); lt[:, :q] = 1.0
        m["ltq"] = lt
        m["cos1o"] = np.ascontiguousarray(cos1[2048 * q:2048 * (q + 1)].reshape(16, 128, 128).transpose(1, 0, 2))
        m["sin1o"] = np.ascontiguousarray(sin1[2048 * q:2048 * (q + 1)].reshape(16, 128, 128).transpose(1, 0, 2))
        m["C32o"] = np.ascontiguousarray(C32[:, 2048 * q:2048 * (q + 1)])
        m["S32o"] = np.ascontiguousarray(S32[:, 2048 * q:2048 * (q + 1)])
        per_core.append(m)
    return per_core


def build(stage=99, dbg=()):
    nc = bass.Bass("TRN2", target_bir_lowering=False)
    S = Sched(nc)
    dram = {}

    def din(name, shape, dt=F32):
        dram[name] = nc.dram_tensor(name, list(shape), dt, kind="ExternalInput").ap()
        return dram[name]

    def dout(name, shape, dt=F32):
        dram[name] = nc.dram_tensor(name, list(shape), dt, kind="ExternalOutput").ap()
        return dram[name]

    def dscr(name, shape, dt=F32):
        dram[name] = nc.dram_tensor(name, list(shape), dt, kind="Internal").ap()
        return dram[name]

    x_own = din("x_own", [2048, D]); ctx_b = din("ctx_b", [L_CTX, D])
    cT = din("cT", [128, 8, 2]); ada_s = din("ada_s", [D, 3072]); ada_bs = din("ada_bs", [3072])
    w_in = din("w_in", [D, 1088]); w_uq = din("w_uq", [256, 768]); w_uqsw = din("w_uqsw", [256, 768])
    w_uk = din("w_uk", [128, 512]); w_uv = din("w_uv", [128, 512]); ab_w_o = din("ab_w_o", [D, D])
    q_norm = din("q_norm", [128, 2]); kv_norm = din("kv_norm", [128, 1])
    norm1 = din("norm1", [2, D]); norm2 = din("norm2", [2, D])
    C32o = din("C32o", [32, 2048]); S32o = din("S32o", [32, 2048])
    ident_f_d = din("ident_f", [128, 128])
    C128b_d = din("C128b", [128, 128], BF16); S128b_d = din("S128b", [128, 128], BF16)
    tw_d = din("tw", [128, 3, 64]); dft64o_d = din("dft64o", [64, 2, 32], BF16)
    C256b_d = din("C256b", [128, 2, 256], BF16); NS256b_d = din("NS256b", [128, 2, 256], BF16)
    hp_scr = dscr("hp_scr", [128, 64, 2, 512], BF16)
    moe_g = din("moe_g", [2, 4, D, 2048]); moe_u = din("moe_u", [2, 4, D, 2048]); moe_d = din("moe_d", [2, 4, 2048, D])
    esel_d = din("esel", [128, 4, 16]); ltq_d = din("ltq", [128, 4]); w_router = din("w_router", [2, D, 16])
    rowid_d = din("rowid", [128, 64, 2], BF16); iota_d = din("iota_row", [128, 1024]); ustrict_d = din("ustrict", [128, 128], BF16)
    tgt_d = din("tgt", [128, 2, 16]); cbe_d = din("cbe", [128, 16])
    h_scr = dscr("h_scr", [NQRY, D])
    c_w_in = din("c_w_in", [D, 1536]); c_w_o = din("c_w_o", [D, D]); c_q_gain = din("c_q_gain", [128]); c_k_gain = din("c_k_gain", [128])
    final_norm = din("final_norm", [D]); cos1o = din("cos1o", [128, 16, 128]); sin1o = din("sin1o", [128, 16, 128])
    kT_loc = dscr("kT_loc", [128, 2, 2048], BF16); kT_all = dscr("kT_all", [4, 128, 2, 2048], BF16)
    v_loc = dscr("v_loc", [2048, 256], BF16); v_all = dscr("v_all", [8192, 256], BF16)
    m_loc = dscr("m_loc", [2048, D], BF16); m_all = dscr("m_all", [4, 4, 512, D], BF16)
    aff_loc = dscr("aff_loc", [128, 16, 16]); aff_all = dscr("aff_all", [4, 128, 16, 16])
    y_loc = dscr("y_loc", [12, 352, D], BF16); y_all = dscr("y_all", [12, 4, 352, D], BF16)

    mods_loc = dscr("mods_loc", [2, 3072]); mods_all = dscr("mods_all", [4, 2, 3072])
    f_scr = dscr("f_scr", [NKEY, 512], BF16)
    ckv_loc = dscr("ckv_loc", [128, 2048], BF16); ckv_all = dscr("ckv_all", [4, 128, 2048], BF16)
    kr_loc = dscr("kr_loc", [32, 2048], BF16); kr_all = dscr("kr_all", [4, 32, 2048], BF16)
    f_loc = dscr("f_loc", [2, 1024, 512], BF16); f_all = dscr("f_all", [2, 4, 1024, 512], BF16)

    outs = []

    with contextlib.ExitStack() as st:
        total = (nc.sbuf_bytes_remaining // 64) * 64 - 128
        arena = st.enter_context(nc.sbuf_tensor("arena", [128, total // 4], F32))
        abase = nc.lookup_mloc(arena).addr
        assert abase % 32 == 0
        SZ_P = 16 * 1024
        SZ_R1 = 64 * 1024
        SZ_R2 = 71 * 1024
        SZ_R3 = 36 * 1024
        RP = Region(nc, abase, SZ_P, "P")
        R1 = Region(nc, abase + SZ_P, SZ_R1, "A")
        R2 = Region(nc, abase + SZ_P + SZ_R1, SZ_R2, "B")
        R3 = Region(nc, abase + SZ_P + SZ_R1 + SZ_R2, SZ_R3, "C")
        r4base = SZ_P + SZ_R1 + SZ_R2 + SZ_R3
        R4 = Region(nc, abase + r4base, total - r4base, "D")
        psw = [st.enter_context(nc.psum_tensor("psw%d" % i, [128, 1024], F32)) for i in range(4)]
        psb = [psw[i // 2][:, (i % 2) * 512:(i % 2 + 1) * 512] for i in range(8)]
        psn = [0]
        ps_rng = [0, 8]

        def PS():
            i = ps_rng[0] + psn[0] % (ps_rng[1] - ps_rng[0])
            psn[0] += 1
            return i

        def dbg_dump(name, src_ap, shape, dt, rkeys):
            if name in dbg:
                o = dout("dbg_" + name, shape, dt)
                S.dma(lambda e, o=o: e.dma_start(out=o, in_=src_ap), r=rkeys, w=["dbg_" + name])
                outs.append("dbg_" + name)

        ident_f = RP.alloc("ident_f", [128, 128], F32)
        ident_b = RP.alloc("ident_b", [128, 128], BF16)
        ones_f = RP.alloc("ones_f", [128, 128], F32)
        ones_b = RP.alloc("ones_b", [128, 128], BF16)
        h_ctx = RP.alloc("h_ctx", [128, 2, D], F32)
        S.dma(lambda e: e.dma_start(out=ident_f[:], in_=ident_f_d), w=["ident_f"])
        C128b = RP.alloc("C128b", [128, 128], BF16); S128b = RP.alloc("S128b", [128, 128], BF16)
        C256b = RP.alloc("C256b", [128, 2, 256], BF16); NS256b = RP.alloc("NS256b", [128, 2, 256], BF16)
        S.dma(lambda e: [e.dma_start(out=C128b[:], in_=C128b_d), e.dma_start(out=S128b[:], in_=S128b_d),
                         e.dma_start(out=C256b[:], in_=C256b_d), e.dma_start(out=NS256b[:], in_=NS256b_d)], w=["dfttab"], n=4)
        S.op("dve", lambda e: e.tensor_copy(ident_b[:], ident_f[:]), r=["ident_f"], w=["ident_b"])
        S.op("pool", lambda e: e.memset(ones_f[:], 1.0), w=["ones_f"])
        S.op("pool", lambda e: e.memset(ones_b[:], 1.0), w=["ones_b"])

        cT_sb = R3.alloc("cT", [128, 8, 2], F32)
        silc = R3.alloc("silc", [128, 8, 2], F32)
        adab2 = R3.alloc("adab2", [2, 3072], F32)
        modl = R3.alloc("modl", [2, 3072], F32)
        adas = R2.alloc("adas", [128, 8, 1536], F32)
        S.dma(lambda e: e.dma_start(out=cT_sb[:], in_=cT), w=["cT"])
        S.dma(lambda e: e.dma_start(out=adab2[:], in_=ada_bs.partition_broadcast(2)), w=["adab2"])
        S.op("act", lambda e: e.activation(silc[:], cT_sb[:], AF.Silu), r=["cT"], w=["silc"])
        for hf in range(2):
            S.dma(lambda e, hf=hf: e.dma_start(
                out=adas[:], in_=ada_s[:, hf * 1536:(hf + 1) * 1536].rearrange("(k p) c -> p k c", p=128)), w=["adas"])
            for cb in range(3):
                pi = PS()
                c0 = hf * 1536 + cb * 512
                for kt in range(8):
                    S.op("pe", lambda e, pi=pi, kt=kt, cb=cb: e.matmul(
                        psb[pi][0:2, :], silc[:, kt, :], adas[:, kt, cb * 512:(cb + 1) * 512],
                        start=(kt == 0), stop=(kt == 7)), r=["silc", "adas"], w=[("ps", pi)])
                S.op("dve", lambda e, pi=pi, c0=c0: e.tensor_tensor(
                    modl[:, c0:c0 + 512], psb[pi][0:2, :], adab2[:, c0:c0 + 512], ALU.add),
                    r=[("ps", pi), "adab2"], w=["modl"])
        S.dma(lambda e: e.dma_start(out=mods_loc, in_=modl[:]), r=["modl"], w=["mods_loc"])
        S.cc(lambda e: e.collective_compute("AllGather", ALU.bypass, replica_groups=GROUPS,
                                            ins=[mods_loc], outs=[mods_all.rearrange("r v c -> (r v) c")]),
             r=["mods_loc"], w=["mods_all"])

        def load_mod(dst, v, vi, key):
            src = mods_all[:, v, vi * 256:(vi + 1) * 256].partition_broadcast(128)
            S.dma(lambda e: e.dma_start(out=dst[:].rearrange("p (a b) -> p a b", a=4), in_=src),
                  r=["mods_all"], w=[key])

        def load_rep(dst, row_ap, key):
            S.dma(lambda e: e.dma_start(out=dst[:], in_=row_ap.partition_broadcast(128)), w=[key])

        if "mods" in dbg:
            o = dout("dbg_mods", [8, 3072])
            tmpm = R4.alloc("tmpm", [8, 3072], F32)
            S.dma(lambda e, tmpm=tmpm: e.dma_start(out=tmpm[:], in_=mods_all.rearrange("r v c -> (r v) c")), r=["mods_all"], w=["tmpm"])
            S.dma(lambda e, o=o, tmpm=tmpm: e.dma_start(out=o, in_=tmpm[:]), r=["tmpm"], w=["dbg_mods"])
            outs.append("dbg_mods")

        S.barrier()
        for R in (R1, R2, R3, R4):
            R.reset()

        if stage >= 1 and "skip12" not in dbg:
            ckvnT = R2.alloc("ckvnT", [128, NKEY], BF16)
            krT = R2.alloc("krT", [96, NKEY], BF16)
            QT = R2.alloc("QT", [96, 8, NQRY], BF16)
            xt = [R1.alloc("xt", [128, D], F32) for _ in range(3)]
            t1 = [R1.alloc("t1", [128, D], F32) for _ in range(2)]
            abf = [R1.alloc("abf", [128, D], BF16) for _ in range(2)]
            junk = R1.alloc("junk", [128, D], BF16)
            aT = [R1.alloc("aT", [128, 8, 512], BF16) for _ in range(2)]
            fb = [R1.alloc("fb", [128, 512], BF16) for _ in range(2)]
            sqc = R1.alloc("sqc", [128, 512], F32)
            rr = R1.alloc("rr", [128, 512], F32)
            sq01 = R1.alloc("sq01", [128, 2, 512], F32)
            rq = R1.alloc("rq", [128, 512], F32)
            cqg = R1.alloc("cqg", [128, 2, 512], BF16)
            ckvO = R1.alloc("ckvO", [128, 2048], BF16)
            krO = R1.alloc("krO", [96, 2048], BF16)
            win_b = R3.alloc("win_b", [128, 8, 1088], BF16)
            wuq_b = R3.alloc("wuq_b", [128, 2, 768], BF16)
            wuqsw_b = R3.alloc("wuqsw_b", [128, 2, 768], BF16)
            gs_rep = R3.alloc("gs_rep", [128, D], F32)
            sh_rep = R3.alloc("sh_rep", [128, D], F32)
            wst = [R4.alloc("wst", [128, 1088], F32) for _ in range(2)]
            Cc = R4.alloc("Cc", [96, 512], F32); Sc = R4.alloc("Sc", [96, 512], F32)
            k1 = R4.alloc("k1", [96, 512], F32); k2 = R4.alloc("k2", [96, 512], F32); u3 = R4.alloc("u3", [96, 512], F32)
            u1, u2 = k1, k2
            ssb = R4.alloc("ssb", [128, 8], F32)
            gq = R4.alloc("gq", [128, 2], F32); gkv = R4.alloc("gkv", [128, 1], F32)

            for k in range(8):
                S.dma(lambda e, k=k: e.dma_start(out=wst[k % 2][:], in_=w_in[k * 128:(k + 1) * 128, :]), w=[("wst", k % 2)])
                S.op("pool", lambda e, k=k: e.tensor_copy(win_b[:, k, :], wst[k % 2][:]), r=[("wst", k % 2)], w=[("win", k)])
            for j in range(2):
                S.dma(lambda e, j=j: e.dma_start(out=wst[j][:, 0:768], in_=w_uq[j * 128:(j + 1) * 128, :]), w=[("wst", j)])
                S.op("pool", lambda e, j=j: e.tensor_copy(wuq_b[:, j, :], wst[j][:, 0:768]), r=[("wst", j)], w=[("wuq", j)])
            for j in range(2):
                S.dma(lambda e, j=j: e.dma_start(out=wst[j][:, 0:768], in_=w_uqsw[j * 128:(j + 1) * 128, :]), w=[("wst", j)])
                S.op("pool", lambda e, j=j: e.tensor_copy(wuqsw_b[:, j, :], wst[j][:, 0:768]), r=[("wst", j)], w=[("wuqsw", j)])
            WIN = [("win", k) for k in range(8)]
            S.dma(lambda e: e.dma_start(out=gq[:], in_=q_norm), w=["gq"])
            S.dma(lambda e: e.dma_start(out=gkv[:], in_=kv_norm), w=["gkv"])

            cnt = {"t": 0}

            def set_mods(v):
                load_mod(sh_rep, v, 0, "sh_rep")
                load_mod(t1[0], v, 1, ("t1", 0))
                load_rep(t1[1], norm1[0, :], ("t1", 1))
                S.op("dve", lambda e: e.scalar_tensor_tensor(gs_rep[:], t1[0][:], 1.0, t1[1][:], ALU.add, ALU.mult),
                     r=[("t1", 0), ("t1", 1)], w=["gs_rep"])

            def norm_mod_T(src_rows, cb, slot):
                i = cnt["t"]; cnt["t"] += 1
                b3, b2 = i % 3, i % 2
                S.dma(lambda e: e.dma_start(out=xt[b3][:], in_=src_rows), w=[("xt", b3)])
                S.op("act", lambda e: e.activation(junk[:], xt[b3][:], AF.Square, accum_out=ssb[:, b3:b3 + 1]),
                     r=[("xt", b3)], w=["junk", ("ss", b3)])
                S.op("act", lambda e: e.activation(ssb[:, 3 + b3:4 + b3], ssb[:, b3:b3 + 1], AF.Sqrt, bias=EPS, scale=1.0 / D),
                     r=[("ss", b3)], w=[("sd", b3)])
                S.op("dve", lambda e: e.reciprocal(ssb[:, 3 + b3:4 + b3], ssb[:, 3 + b3:4 + b3]), r=[("sd", b3)], w=[("sd", b3)])
                S.op("dve", lambda e: e.scalar_tensor_tensor(t1[b2][:], xt[b3][:], ssb[:, 3 + b3:4 + b3], gs_rep[:],
                                                             ALU.mult, ALU.mult),
                     r=[("xt", b3), ("sd", b3), "gs_rep"], w=[("t1", b2)])
                S.op("pool", lambda e: e.tensor_tensor(abf[b2][:], t1[b2][:], sh_rep[:], ALU.add),
                     r=[("t1", b2), "sh_rep"], w=[("abf", b2)])

                def stageB():
                    pi = PS()
                    psT = psb[pi][:].bitcast(BF16).rearrange("p (k c) -> p k c", k=8)
                    for k in range(8):
                        S.op("pe", lambda e, k=k: e.transpose(psT[:, k, :], abf[b2][:, k * 128:(k + 1) * 128], ident_b[:]),
                             r=[("abf", b2), "ident_b"], w=[("ps", pi)])
                    dst = aT[cb][:, :, slot * 128:(slot + 1) * 128]
                    if i % 2 == 0:
                        S.op("act", lambda e: e.copy(dst, psT), r=[("ps", pi)], w=[("aT", cb, slot)])
                    else:
                        S.op("dve", lambda e: e.tensor_copy(dst, psT), r=[("ps", pi)], w=[("aT", cb, slot)])
                return stageB

            def kv_side(cb, nslot, key0, is_ctx):
                N = nslot * 128
                AT = [("aT", cb, s) for s in range(nslot)]
                if is_ctx:
                    ckv_dst = ckvnT[:, key0:key0 + N]
                    kr_dst = krT[64:96, key0:key0 + N]
                    f_rows = lambda s_: f_scr[key0 + s_ * 128:key0 + (s_ + 1) * 128, :]
                else:
                    ckv_dst = ckvO[:, key0:key0 + N]
                    kr_dst = krO[64:96, key0:key0 + N]
                    f_rows = lambda s_: f_loc.rearrange("c i d -> (c i) d")[key0 + s_ * 128:key0 + (s_ + 1) * 128, :]
                for s in range(nslot):
                    pi = PS()
                    for k in range(8):
                        S.op("pe", lambda e, pi=pi, k=k, s=s: e.matmul(
                            psb[pi][:, :], aT[cb][:, k, s * 128:(s + 1) * 128], win_b[:, k, 0:512],
                            start=(k == 0), stop=(k == 7)), r=[("aT", cb, s)] + WIN, w=[("ps", pi)])
                    fi = cnt.setdefault("f", 0) % 2; cnt["f"] += 1
                    S.op("dve", lambda e, pi=pi, fi=fi: e.tensor_copy(fb[fi][:], psb[pi][:, :]), r=[("ps", pi)], w=[("fb", fi)])
                    S.dma(lambda e, fi=fi, s=s: e.dma_start(out=f_rows(s), in_=fb[fi][:]),
                          r=[("fb", fi)], w=["f_scr"])
                pc, pk, pks = PS(), PS(), PS()
                for k in range(8):
                    S.op("pe", lambda e, k=k: e.matmul(psb[pc][:, 0:N], win_b[:, k, 512:640], aT[cb][:, k, 0:N],
                                                       start=(k == 0), stop=(k == 7)), r=AT + WIN, w=[("ps", pc)])
                for k in range(8):
                    S.op("pe", lambda e, k=k: e.matmul(psb[pk][0:96, 0:N], win_b[:, k, 640:736], aT[cb][:, k, 0:N],
                                                       start=(k == 0), stop=(k == 7)), r=AT + WIN, w=[("ps", pk)])
                if not is_ctx:
                    for k in range(8):
                        S.op("pe", lambda e, k=k: e.matmul(psb[pks][0:96, 0:N], win_b[:, k, 736:832], aT[cb][:, k, 0:N],
                                                           start=(k == 0), stop=(k == 7)), r=AT + WIN, w=[("ps", pks)])
                S.op("act", lambda e: e.activation(sqc[:, 0:N], psb[pc][:, 0:N], AF.Square), r=[("ps", pc)], w=["sqc"])
                pss = PS()
                S.op("pe", lambda e: e.matmul(psb[pss][:, 0:N], ones_f[:], sqc[:, 0:N], start=True, stop=True),
                     r=["ones_f", "sqc"], w=[("ps", pss)])
                S.op("act", lambda e: e.activation(rr[:, 0:N], psb[pss][:, 0:N], AF.Sqrt, bias=EPS, scale=1.0 / 128),
                     r=[("ps", pss)], w=["rr"])
                S.op("dve", lambda e: e.reciprocal(rr[:, 0:N], rr[:, 0:N]), r=["rr"], w=["rr"])
                S.op("dve", lambda e: e.scalar_tensor_tensor(ckv_dst, psb[pc][:, 0:N], gkv[:, 0:1], rr[:, 0:N],
                                                             ALU.mult, ALU.mult),
                     r=[("ps", pc), "gkv", "rr"], w=[("ckvnT", key0)])
                if is_ctx:
                    S.op("act", lambda e: e.copy(kr_dst, psb[pk][64:96, 0:N]), r=[("ps", pk)], w=[("krT", key0)])
                else:
                    S.dma(lambda e: [e.dma_start(out=Cc[64:96, 0:N], in_=C32o[:, key0:key0 + N]),
                                     e.dma_start(out=Sc[64:96, 0:N], in_=S32o[:, key0:key0 + N])], w=["CcSc"], n=2)
                    S.op("dve", lambda e: e.tensor_tensor(k1[64:96, 0:N], psb[pk][64:96, 0:N], Cc[64:96, 0:N], ALU.mult),
                         r=[("ps", pk), "CcSc"], w=["k1"])
                    S.op("dve", lambda e: e.tensor_tensor(k2[64:96, 0:N], psb[pks][64:96, 0:N], Sc[64:96, 0:N], ALU.mult),
                         r=[("ps", pks), "CcSc"], w=["k2"])
                    S.op("pool", lambda e: e.tensor_tensor(kr_dst, k1[64:96, 0:N], k2[64:96, 0:N], ALU.add),
                         r=["k1", "k2"], w=[("krT", key0)])

            def q_side(cb, nslot, qc0, is_ctx, tab0):
                N = nslot * 128
                AT = [("aT", cb, s) for s in range(nslot)]
                pcq = [PS(), PS()]
                for j in range(2):
                    for k in range(8):
                        S.op("pe", lambda e, j=j, k=k: e.matmul(
                            psb[pcq[j]][:, 0:N], win_b[:, k, 832 + j * 128:832 + (j + 1) * 128], aT[cb][:, k, 0:N],
                            start=(k == 0), stop=(k == 7)), r=AT + WIN, w=[("ps", pcq[j])])
                    S.op("act", lambda e, j=j: e.activation(sq01[:, j, 0:N], psb[pcq[j]][:, 0:N], AF.Square),
                         r=[("ps", pcq[j])], w=[("sq01", j)])
                    S.op("act", lambda e, j=j: e.activation(cqg[:, j, 0:N], psb[pcq[j]][:, 0:N], AF.Copy, scale=gq[:, j:j + 1]),
                         r=[("ps", pcq[j]), "gq"], w=[("cqg", j)])
                pss = PS()
                for j in range(2):
                    S.op("pe", lambda e, j=j: e.matmul(psb[pss][:, 0:N], ones_f[:], sq01[:, j, 0:N], start=(j == 0), stop=(j == 1)),
                         r=["ones_f", ("sq01", j)], w=[("ps", pss)])
                S.op("act", lambda e: e.activation(rq[:, 0:N], psb[pss][:, 0:N], AF.Sqrt, bias=EPS, scale=1.0 / 256),
                     r=[("ps", pss)], w=["rq"])
                S.op("dve", lambda e: e.reciprocal(rq[:, 0:N], rq[:, 0:N]), r=["rq"], w=["rq"])
                if not is_ctx:
                    S.dma(lambda e: [e.dma_start(out=Cc[64:96, 0:N], in_=C32o[:, tab0:tab0 + N]),
                                     e.dma_start(out=Sc[64:96, 0:N], in_=S32o[:, tab0:tab0 + N])], w=["CcSc"], n=2)
                for h in range(8):
                    pq, pqs = PS(), PS()
                    for j in range(2):
                        S.op("pe", lambda e, j=j, h=h, pq=pq: e.matmul(
                            psb[pq][0:96, 0:N], wuq_b[:, j, h * 96:(h + 1) * 96], cqg[:, j, 0:N], start=(j == 0), stop=(j == 1)),
                            r=[("wuq", 0), ("wuq", 1), ("cqg", 0), ("cqg", 1)], w=[("ps", pq)])
                    if is_ctx:
                        S.op("dve", lambda e, h=h, pq=pq: e.tensor_tensor(
                            QT[0:96, h, qc0:qc0 + N], psb[pq][0:96, 0:N], rq[0:96, 0:N], ALU.mult),
                            r=[("ps", pq), "rq"], w=[("QT", h, qc0)])
                        continue
                    for j in range(2):
                        S.op("pe", lambda e, j=j, h=h, pqs=pqs: e.matmul(
                            psb[pqs][0:96, 0:N], wuqsw_b[:, j, h * 96:(h + 1) * 96], cqg[:, j, 0:N], start=(j == 0), stop=(j == 1)),
                            r=[("wuqsw", 0), ("wuqsw", 1), ("cqg", 0), ("cqg", 1)], w=[("ps", pqs)])
                    S.op("dve", lambda e, h=h, pq=pq: e.tensor_tensor(
                        QT[0:64, h, qc0:qc0 + N], psb[pq][0:64, 0:N], rq[0:64, 0:N], ALU.mult),
                        r=[("ps", pq), "rq"], w=[("QTn", h, qc0)])
                    S.op("dve", lambda e, pq=pq: e.tensor_tensor(u1[64:96, 0:N], psb[pq][64:96, 0:N], Cc[64:96, 0:N], ALU.mult),
                         r=[("ps", pq), "CcSc"], w=["k1"])
                    S.op("dve", lambda e, pqs=pqs: e.tensor_tensor(u2[64:96, 0:N], psb[pqs][64:96, 0:N], Sc[64:96, 0:N], ALU.mult),
                         r=[("ps", pqs), "CcSc"], w=["k2"])
                    S.op("pool", lambda e: e.tensor_tensor(u3[64:96, 0:N], u1[64:96, 0:N], u2[64:96, 0:N], ALU.add),
                         r=["k1", "k2"], w=["u3"])
                    S.op("pool", lambda e, h=h: e.tensor_tensor(QT[64:96, h, qc0:qc0 + N], u3[64:96, 0:N], rq[64:96, 0:N], ALU.mult),
                         r=["u3", "rq"], w=[("QTr", h, qc0)])

            jobs = []
            jobs.append(dict(pre=lambda: set_mods(1), a=(ctx_b[0:128, :], 0, 0), post=None))
            jobs.append(dict(pre=None, a=(ctx_b[128:256, :], 0, 1),
                             post=lambda: (kv_side(0, 2, 8192, True), q_side(0, 2, 2048, True, 0))))
            for c in range(4):
                cb = (c + 1) % 2
                for s4 in range(4):
                    t = 4 * c + s4
                    jobs.append(dict(pre=(lambda: set_mods(0)) if (c == 0 and s4 == 0) else None,
                                     a=(x_own[t * 128:(t + 1) * 128, :], cb, s4),
                                     post=(lambda cb=cb, c=c: (kv_side(cb, 4, c * 512, False), q_side(cb, 4, c * 512, False, c * 512))) if s4 == 3 else None))
            prevB, prevPost = None, None
            for jb in jobs:
                if jb["pre"] is not None:
                    jb["pre"]()
                curB = norm_mod_T(*jb["a"])
                if prevB is not None:
                    prevB()
                    if prevPost is not None:
                        prevPost()
                prevB, prevPost = curB, jb["post"]
            prevB()
            if prevPost is not None:
                prevPost()

            S.dma(lambda e: [e.dma_start(out=ckv_loc, in_=ckvO[:]), e.dma_start(out=kr_loc, in_=krO[64:96, :])],
                  r=[("ckvnT", k0) for k0 in range(0, 2048, 512)] + [("krT", k0) for k0 in range(0, 2048, 512)], w=["kvloc"], n=2)
            S.barrier()
            S.cc(lambda e: e.collective_compute("AllGather", ALU.bypass, replica_groups=GROUPS, ins=[ckv_loc],
                                                outs=[ckv_all.rearrange("r d t -> (r d) t")]), w=["ckv_all"])
            S.cc(lambda e: e.collective_compute("AllGather", ALU.bypass, replica_groups=GROUPS, ins=[kr_loc],
                                                outs=[kr_all.rearrange("r d t -> (r d) t")]), w=["kr_all"])
            for c2 in range(2):
                S.cc(lambda e, c2=c2: e.collective_compute("AllGather", ALU.bypass, replica_groups=GROUPS, ins=[f_loc[c2]],
                                                           outs=[f_all[c2].rearrange("r i d -> (r i) d")]), w=[("f_all", c2)])
            S.dma(lambda e: e.dma_start(out=ckvnT[:, 0:8192].rearrange("d (r t) -> d r t", r=4), in_=ckv_all.rearrange("r d t -> d r t")),
                  r=["ckv_all"], w=["ckvnT_all"])
            S.dma(lambda e: e.dma_start(out=krT[64:96, 0:8192].rearrange("d (r t) -> d r t", r=4), in_=kr_all.rearrange("r d t -> d r t")),
                  r=["kr_all"], w=["krT_all"])
            dbg_dump("ckvnT", ckvnT[:], [128, NKEY], BF16, [("ckvnT", k0) for k0 in list(range(0, 8192, 512)) + [8192]])
            dbg_dump("krT", krT[64:96, :], [32, NKEY], BF16, [("krT", k0) for k0 in list(range(0, 8192, 512)) + [8192]])
            S.barrier()
            dbg_dump("QT", QT[:], [96, 8, NQRY], BF16, [])
            if "f" in dbg:
                o = dout("dbg_f", [NKEY, 512], BF16)
                S.dma(lambda e, o=o: e.dma_start(out=o, in_=f_scr), r=["f_scr"], w=["dbg_f"])
                outs.append("dbg_f")

        if stage >= 2 and "skip12" not in dbg:
            for R in (R1, R3, R4):
                R.reset()
            KT = [R1.alloc("KT", [96, NKEY], BF16) for _ in range(2)]
            Vh1 = R1.alloc("Vh", [128, NT, 128], BF16)
            PTw = [R1.alloc("PTw", [128, 1024], BF16) for _ in range(3)]
            rden = R1.alloc("rden", [64, 512], F32)
            dsb = R1.alloc("dsb", [128, 512], F32)
            wuk_b = R1.alloc("wuk_b", [128, 512], BF16)
            wuv_b = R1.alloc("wuv_b", [128, 512], BF16)
            OT = R3.alloc("OT", [64, 8, NQRY], BF16)
            wst2 = [R4.alloc("wst2", [128, 512], F32) for _ in range(2)]
            S.dma(lambda e: e.dma_start(out=wst2[0][:], in_=w_uk), w=[("wst2", 0)])
            S.dma(lambda e: e.dma_start(out=wst2[1][:], in_=w_uv), w=[("wst2", 1)])
            S.op("pool", lambda e: e.tensor_copy(wuk_b[:], wst2[0][:]), r=[("wst2", 0)], w=["wuk_b"])
            S.op("pool", lambda e: e.tensor_copy(wuv_b[:], wst2[1][:]), r=[("wst2", 1)], w=["wuv_b"])
            S.op("pool", lambda e: e.memset(Vh1[:, :, 64:128], 1.0), w=["Vones"])
            psr = [0]

            def PSs():
                i = psr[0] % 4
                psr[0] += 1
                return i
            chunks = [(c * 512, 512) for c in range(16)] + [(8192, 256)]
            SCALE0 = 96.0 ** -0.5

            def build_k(h):
                hb = h % 2
                for ci, (k0, N) in enumerate(chunks):
                    pi = PSs()
                    S.op("pe", lambda e, pi=pi, k0=k0, N=N: e.matmul(
                        psb[pi][0:64, 0:N], wuk_b[:, h * 64:(h + 1) * 64], ckvnT[:, k0:k0 + N], start=True, stop=True),
                        r=["wuk_b"], w=[("ps", pi)])
                    S.op("dve", lambda e, pi=pi, k0=k0, N=N: e.tensor_copy(KT[hb][0:64, k0:k0 + N], psb[pi][0:64, 0:N]),
                         r=[("ps", pi)], w=[("KTn", hb, ci)])
                S.op("pool", lambda e: e.tensor_copy(KT[hb][64:96, :], krT[64:96, :]), w=[("KTr", hb)])

            def build_v(h):
                for g in range(9):
                    nt = 8 if g < 8 else 2
                    pi = PSs()
                    for j in range(nt):
                        tt = g * 8 + j
                        S.op("pe", lambda e, pi=pi, j=j, tt=tt: e.matmul(
                            psb[pi][:, j * 64:(j + 1) * 64], ckvnT[:, tt * 128:(tt + 1) * 128], wuv_b[:, h * 64:(h + 1) * 64],
                            start=True, stop=True), r=["wuv_b"], w=[("ps", pi)])
                    S.op("dve", lambda e, pi=pi, g=g, nt=nt: e.tensor_copy(
                        Vh1[:, g * 8:g * 8 + nt, 0:64], psb[pi][:, 0:nt * 64].rearrange("p (a b) -> p a b", b=64)),
                        r=[("ps", pi)], w=[("V", g)])
            st0 = {"blk": 0, "pt": 0, "sp": 0}

            def attend(h, qc0, NQc, tiles):
                hb = h % 2
                ab = st0["blk"] % 2
                st0["blk"] += 1
                po, pd = 4 + ab, 6 + ab
                pairs = [(tiles[2 * i], tiles[2 * i + 1]) for i in range(len(tiles) // 2)]
                n = len(pairs)
                sbank = {}

                def Sm(i):
                    sp = st0["sp"] % 2
                    st0["sp"] += 1
                    sbank[i] = sp
                    for hf, tt in enumerate(pairs[i]):
                        S.op("pe", lambda e, hf=hf, tt=tt, sp=sp: e.matmul(psw[sp][:, hf * 512:hf * 512 + NQc], KT[hb][0:96, tt * 128:(tt + 1) * 128],
                                                                       QT[0:96, h, qc0:qc0 + NQc], start=True, stop=True),
                             r=[("KTn", hb, tt // 4), ("KTr", hb)], w=[("ps", 2 * sp), ("ps", 2 * sp + 1)])
                Sm(0)
                for i in range(n):
                    sp = sbank[i]
                    pbi = st0["pt"] % 3
                    st0["pt"] += 1
                    S.op("act", lambda e, sp=sp, pbi=pbi: e.activation(
                        PTw[pbi][:].rearrange("p (a b) -> p a b", a=2)[:, :, 0:NQc], psw[sp][:].rearrange("p (a b) -> p a b", a=2)[:, :, 0:NQc],
                        AF.Exp, scale=SCALE0), r=[("ps", 2 * sp), ("ps", 2 * sp + 1)], w=[("PTw", pbi)])
                    if i + 1 < n:
                        Sm(i + 1)
                    for hf, tt in enumerate(pairs[i]):
                        first = (i == 0 and hf == 0)
                        last = (i == n - 1 and hf == 1)
                        S.op("pe", lambda e, tt=tt, pbi=pbi, hf=hf, first=first, last=last: e.matmul(
                            psb[po][:, 0:NQc], Vh1[:, tt, :], PTw[pbi][:, hf * 512:hf * 512 + NQc], start=first, stop=last),
                            r=[("V", tt // 8), "Vones", ("PTw", pbi)], w=[("ps", po)])
                S.op("act", lambda e: e.copy(dsb[64:128, 0:NQc], psb[po][64:128, 0:NQc]), r=[("ps", po)], w=["dsb"])
                S.op("pe", lambda e: e.matmul(psb[pd][0:64, 0:NQc], ident_f[64:128, 64:128], dsb[64:128, 0:NQc], start=True, stop=True),
                     r=["dsb"], w=[("ps", pd)])
                S.op("dve", lambda e: e.reciprocal(rden[:, 0:NQc], psb[pd][0:64, 0:NQc]), r=[("ps", pd)], w=["rden"])
                S.op("dve", lambda e: e.tensor_tensor(OT[0:64, h, qc0:qc0 + NQc], psb[po][0:64, 0:NQc], rden[:, 0:NQc], ALU.mult),
                     r=[("ps", po), "rden"], w=[("OT", h, qc0)])

            build_k(0)
            for h in range(8):
                build_v(h)
                for qb in range(4):
                    if qb == 2 and h + 1 < 8:
                        build_k(h + 1)
                    attend(h, qb * 512, 512, list(range(NT)))
                attend(h, 2048, 256, [64, 65])
            S.barrier()
            dbg_dump("OT", OT[:], [64, 8, NQRY], BF16, [])

        if stage >= 3:
            for R in (R1, R2, R4):
                R.reset()
            F2 = R1.alloc("F2", [128, 64, 512], BF16)
            tA = [R2.alloc("tA", [128, 512], F32) for _ in range(2)]
            tB = [R2.alloc("tB", [128, 512], F32) for _ in range(2)]
            Hp = [R2.alloc("Hp", [128, 2, 512], BF16) for _ in range(3)]
            tw = R4.alloc("tw", [128, 3, 64], F32)
            S.dma(lambda e: e.dma_start(out=tw[:], in_=tw_d), w=["tw"])
            S.dma(lambda e: [e.dma_start(out=F2[(r_ * 2 + c_) * 16:(r_ * 2 + c_ + 1) * 16, :, :],
                                         in_=f_all[c_, r_].rearrange("(a b) ch -> a b ch", b=64)) for r_ in range(4) for c_ in range(2)],
                  w=["F2"], n=8)
            for t2 in range(64):
                pc, ps_ = PS(), PS()
                S.op("pe", lambda e, pc=pc, t2=t2: e.matmul(psb[pc][:, :], C128b[:], F2[:, t2, :], start=True, stop=True),
                     r=["F2"], w=[("ps", pc)])
                S.op("pe", lambda e, ps_=ps_, t2=t2: e.matmul(psb[ps_][:, :], S128b[:], F2[:, t2, :], start=True, stop=True),
                     r=["F2"], w=[("ps", ps_)])
                a2, h3 = t2 % 2, t2 % 3
                if "noTw" in dbg:
                    continue
                S.op("act", lambda e, ps_=ps_, t2=t2, a2=a2: e.activation(tA[a2][:], psb[ps_][:, :], AF.Copy, scale=tw[:, 1, t2:t2 + 1]),
                     r=[("ps", ps_), "tw"], w=[("tA", a2)])
                S.op("act", lambda e, pc=pc, t2=t2, a2=a2: e.activation(tB[a2][:], psb[pc][:, :], AF.Copy, scale=tw[:, 1, t2:t2 + 1]),
                     r=[("ps", pc), "tw"], w=[("tB", a2)])
                S.op("dve", lambda e, pc=pc, t2=t2, a2=a2, h3=h3: e.scalar_tensor_tensor(
                    Hp[h3][:, 0, :], psb[pc][:, :], tw[:, 0, t2:t2 + 1], tA[a2][:], ALU.mult, ALU.subtract),
                    r=[("ps", pc), ("tA", a2), "tw"], w=[("Hp", h3)])
                S.op("dve", lambda e, ps_=ps_, t2=t2, a2=a2, h3=h3: e.scalar_tensor_tensor(
                    Hp[h3][:, 1, :], psb[ps_][:, :], tw[:, 2, t2:t2 + 1], tB[a2][:], ALU.mult, ALU.subtract),
                    r=[("ps", ps_), ("tB", a2), "tw"], w=[("Hp", h3)])
                if "noHpDma" not in dbg:
                    S.dma(lambda e, t2=t2, h3=h3: e.dma_start(out=hp_scr[:, t2, :, :], in_=Hp[h3][:]), r=[("Hp", h3)], w=["hp_scr"])
            S.barrier()
            for R in (R1, R2):
                R.reset()
            if "stopA" in dbg:
                if "noDump" not in dbg:
                    o = dout("dbg_hp", [128, 64, 2, 512], BF16)
                    S.dma(lambda e, o=o: e.dma_start(out=o, in_=hp_scr), w=["dbg_hp"])
                    outs.append("dbg_hp")
                else:
                    o = dout("dbg_hp", [128, 2, 512], BF16)
                    S.dma(lambda e, o=o: e.dma_start(out=o, in_=hp_scr[:, 5, :, :]), w=["dbg_hp"])
                    outs.append("dbg_hp")
                stage = 2.5
        if stage >= 3:
            HpT = [R2.alloc("HpT", [64, 8, 2, 512], BF16) for _ in range(2)]
            ZT = R2.alloc("ZT", [128, 4, 2, 16, 128], BF16)
            Fc = R2.alloc("Fc", [128, 2, 512], BF16)
            ZcT = R2.alloc("ZcT", [128, 4, 2, 256], BF16)
            fmT = R4.alloc("fmT", [128, 4, NQRY], BF16)
            d64 = R4.alloc("d64", [64, 2, 32], BF16)
            S.dma(lambda e: e.dma_start(out=d64[:], in_=dft64o_d), w=["d64"])
            for k1b in range(16):
                bb = k1b % 2
                S.dma(lambda e, k1b=k1b, bb=bb: e.dma_start(
                    out=HpT[bb][:], in_=hp_scr[k1b * 8:(k1b + 1) * 8, :, :, :].rearrange("k t r c -> t k r c")), w=[("HpT", bb)])
                for g in range(4):
                    pz = PS()
                    zv = psb[pz][:, 0:256].rearrange("p (k r c) -> p k r c", k=8, r=2)
                    for kk in range(8):
                        gc = slice(g * 128, (g + 1) * 128)
                        zo = zv[:, kk, :, :].rearrange("p r c -> p (r c)")
                        S.op("pe", lambda e, zo=zo, kk=kk, gc=gc, bb=bb: e.matmul(zo, HpT[bb][:, kk, 0, gc], d64[:, 0, :],
                                                                               start=True, stop=False), r=[("HpT", bb), "d64"], w=[("ps", pz)])
                        S.op("pe", lambda e, zo=zo, kk=kk, gc=gc, bb=bb: e.matmul(zo, HpT[bb][:, kk, 1, gc], d64[:, 1, :],
                                                                               start=False, stop=True), r=[("HpT", bb), "d64"], w=[("ps", pz)])
                    dstz = ZT[:, g, :, :, k1b * 8:(k1b + 1) * 8]
                    srcz = zv.rearrange("p k r c -> p r c k")
                    if g % 2 == 0:
                        S.op("act", lambda e, dstz=dstz, srcz=srcz: e.copy(dstz, srcz), r=[("ps", pz)], w=[("ZT", g)])
                    else:
                        S.op("dve", lambda e, dstz=dstz, srcz=srcz: e.tensor_copy(dstz, srcz), r=[("ps", pz)], w=[("ZT", g)])
            S.barrier()
            NRM = 1.0 / math.sqrt(8192.0 * 128.0)
            for g in range(4):
                for kb in range(4):
                    pf = PS()
                    S.op("pe", lambda e, pf=pf, g=g, kb=kb: e.matmul(
                        psb[pf][:, :], C128b[:], ZT[:, g, 0, kb * 4:(kb + 1) * 4, :].rearrange("p a b -> p (a b)"), start=True, stop=False),
                        w=[("ps", pf)])
                    S.op("pe", lambda e, pf=pf, g=g, kb=kb: e.matmul(
                        psb[pf][:, :], S128b[:], ZT[:, g, 1, kb * 4:(kb + 1) * 4, :].rearrange("p a b -> p (a b)"), start=False, stop=True),
                        w=[("ps", pf)])
                    S.op("act", lambda e, pf=pf, g=g, kb=kb: e.activation(fmT[:, g, kb * 512:(kb + 1) * 512], psb[pf][:, :], AF.Copy, scale=NRM),
                         r=[("ps", pf)], w=[("fmT", g, kb)])
            S.dma(lambda e: e.dma_start(out=Fc[:], in_=f_scr[8192:8448, :].rearrange("(t p) c -> p t c", p=128)), w=["Fc"])
            NRMC = 1.0 / math.sqrt(256.0 * 128.0)
            for g in range(4):
                pz = PS()
                for ri, tab in ((0, C256b), (1, NS256b)):
                    for tl in range(2):
                        S.op("pe", lambda e, pz=pz, ri=ri, tab=tab, tl=tl, g=g: e.matmul(
                            psb[pz][:, ri * 256:(ri + 1) * 256], Fc[:, tl, g * 128:(g + 1) * 128], tab[:, tl, :],
                            start=(tl == 0), stop=(tl == 1)), r=["Fc", "dfttab"], w=[("ps", pz)])
                S.op("dve", lambda e, pz=pz, g=g: e.tensor_copy(ZcT[:, g, :, :], psb[pz][:, :].rearrange("p (r c) -> p r c", r=2)),
                     r=[("ps", pz)], w=[("ZcT", g)])
                pf = PS()
                S.op("pe", lambda e, pf=pf, g=g: e.matmul(psb[pf][:, 0:256], C128b[:], ZcT[:, g, 0, :], start=True, stop=False),
                     r=[("ZcT", g)], w=[("ps", pf)])
                S.op("pe", lambda e, pf=pf, g=g: e.matmul(psb[pf][:, 0:256], S128b[:], ZcT[:, g, 1, :], start=False, stop=True),
                     r=[("ZcT", g)], w=[("ps", pf)])
                S.op("act", lambda e, pf=pf, g=g: e.activation(fmT[:, g, 2048:2304], psb[pf][:, 0:256], AF.Copy, scale=NRMC),
                     r=[("ps", pf)], w=[("fmT", g, 4)])
            S.barrier()
            dbg_dump("fmT", fmT[:], [128, 4, NQRY], BF16, [])

        if stage >= 4:
            for R in (R1, R2):
                R.reset()
            h_lat = R1.alloc("h_lat", [128, 16, D], F32)
            wo_f_b = R2.alloc("wo_f_b", [128, 4, D], BF16)
            wo_a_b = R2.alloc("wo_a_b", [64, 8, D], BF16)
            wst3 = [R2.alloc("wst3", [128, D], F32) for _ in range(2)]
            g1_rep = [R2.alloc("g1_rep", [128, D], F32) for _ in range(2)]
            tmpy = [R2.alloc("tmpy", [128, 512], F32) for _ in range(2)]
            for g in range(4):
                S.dma(lambda e, g=g: e.dma_start(out=wst3[g % 2][:], in_=ab_w_o[g * 128:(g + 1) * 128, :]), w=[("wst3", g % 2)])
                S.op("pool", lambda e, g=g: e.tensor_copy(wo_f_b[:, g, :], wst3[g % 2][:]), r=[("wst3", g % 2)], w=["wo_f_b"])
            for hh in range(8):
                S.dma(lambda e, hh=hh: e.dma_start(out=wst3[hh % 2][0:64, :], in_=ab_w_o[512 + hh * 64:512 + (hh + 1) * 64, :]),
                      w=[("wst3", hh % 2)])
                S.op("pool", lambda e, hh=hh: e.tensor_copy(wo_a_b[0:64, hh, :], wst3[hh % 2][0:64, :]), r=[("wst3", hh % 2)], w=["wo_a_b"])
            load_mod(g1_rep[0], 0, 2, ("g1_rep", 0))
            load_mod(g1_rep[1], 1, 2, ("g1_rep", 1))
            S.dma(lambda e: e.dma_start(out=h_lat[:], in_=x_own.rearrange("(j p) d -> p j d", p=128)), w=["h_lat_in"])
            S.dma(lambda e: e.dma_start(out=h_ctx[:], in_=ctx_b.rearrange("(j p) d -> p j d", p=128)), w=["h_ctx_in"])
            yc = 0
            for j in range(NQ):
                isc = j >= 16
                for hf in range(2):
                    py = PS()
                    cs = slice(hf * 512, (hf + 1) * 512)
                    for g in range(4):
                        S.op("pe", lambda e, py=py, g=g, cs=cs, j=j: e.matmul(psb[py][:, :], fmT[:, g, j * 128:(j + 1) * 128], wo_f_b[:, g, cs],
                                                                          start=(g == 0), stop=False), r=["wo_f_b"], w=[("ps", py)])
                    for hh in range(8):
                        S.op("pe", lambda e, py=py, hh=hh, cs=cs, j=j: e.matmul(psb[py][:, :], OT[0:64, hh, j * 128:(j + 1) * 128], wo_a_b[0:64, hh, cs],
                                                                            start=False, stop=(hh == 7)), r=["wo_a_b"], w=[("ps", py)])
                    ti = yc % 2
                    yc += 1
                    hdst = h_ctx[:, j - 16, cs] if isc else h_lat[:, j, cs]
                    grep = g1_rep[1 if isc else 0]
                    S.op("dve", lambda e, py=py, ti=ti, cs=cs, grep=grep: e.tensor_tensor(tmpy[ti][:], psb[py][:, :], grep[:, cs], ALU.mult),
                         r=[("ps", py), ("g1_rep", 0), ("g1_rep", 1)], w=[("tmpy", ti)])
                    S.op("pool", lambda e, ti=ti, hdst=hdst: e.tensor_tensor(hdst, hdst, tmpy[ti][:], ALU.add),
                         r=[("tmpy", ti), "h_lat_in", "h_ctx_in"], w=[("h", j, hf)])
            S.barrier()
            S.dma(lambda e: [e.dma_start(out=h_scr[0:2048, :].rearrange("(j p) d -> p j d", p=128), in_=h_lat[:]),
                             e.dma_start(out=h_scr[2048:2304, :].rearrange("(j p) d -> p j d", p=128), in_=h_ctx[:])],
                  w=["h_scr"], n=2)
            if "hmix" in dbg:
                o = dout("dbg_hmix", [NQRY, D])
                S.dma(lambda e, o=o: [e.dma_start(out=o[0:2048, :].rearrange("(j p) d -> p j d", p=128), in_=h_lat[:]),
                                      e.dma_start(out=o[2048:2304, :].rearrange("(j p) d -> p j d", p=128), in_=h_ctx[:])],
                      w=["dbg_hmix"], n=2)
                outs.append("dbg_hmix")

        def moe(l, with_ctx):
            S.barrier()
            for R in (R1, R2, R3, R4):
                R.reset()
            NTm = 66 if with_ctx else 64
            NQm = 18 if with_ctx else 16
            NS = 1056 if with_ctx else 1024
            NC = NTm * 16
            R3b = Region(nc, R3.base, 16896, "Cb%d" % l)
            affA = R3.alloc("affA", [128, 66, 16], F32)
            totA = R3.alloc("totA", [128, 66, 16], F32)
            totB = R3.alloc("totB", [128, 66, 16], F32)
            mask_b = R3.alloc("mask_b", [128, 66, 16], BF16)
            R3.alloc("pad3", [128, 1056], BF16)
            aff_sb = R3.alloc("aff_sb", [128, 18, 16], F32)
            maskA = R3.alloc("maskA", [128, 66, 16], F32)
            posA = R3.alloc("posA", [128, 66, 16], F32)
            cmpA = R3.alloc("cmpA", [128, 66, 16], F32)
            thr = R4.alloc("thr", [128, 6, 2, 16], F32)
            tgt = R4.alloc("tgt", [128, 2, 16], F32)
            esel = R4.alloc("esel", [128, 4, 16], F32)
            ltq = R4.alloc("ltq", [128, 4], F32)
            cbe = R4.alloc("cbe", [128, 16], F32)
            ust = R4.alloc("ust", [128, 128], BF16)
            rowid = R4.alloc("rowid", [128, 64, 2], BF16)
            ident2 = ident_b
            posO = R4.alloc("posO", [128, 18, 16], F32)
            maskO = R4.alloc("maskO", [128, 18, 16], F32)
            gmO = R4.alloc("gmO", [128, 18, 16], F32)
            rowO = R4.alloc("rowO", [128, 18, 16], I32)
            tmpO = R4.alloc("tmpO", [128, 18, 16], F32)
            tmpO2 = R4.alloc("tmpO2", [128, 18, 16], F32)
            rt = R4.alloc("rt", [128, 4, 16], F32)
            baseq = R4.alloc("baseq", [128, 16], F32)
            wr_sb = R4.alloc("wr_sb", [128, 8, 16], F32)
            smx = R4.alloc("smx", [128, 8], F32)
            mctx = R4.alloc("mctx", [128, 2, D], BF16)
            S.dma(lambda e: [e.dma_start(out=tgt[:], in_=tgt_d), e.dma_start(out=esel[:], in_=esel_d),
                             e.dma_start(out=ltq[:], in_=ltq_d), e.dma_start(out=cbe[:], in_=cbe_d),
                             e.dma_start(out=ust[:], in_=ustrict_d), e.dma_start(out=rowid[:], in_=rowid_d),
                             e.dma_start(out=wr_sb[:], in_=w_router[l].rearrange("(k p) e -> p k e", p=128))],
                  w=["rt_consts"], n=7)
            ht = [R1.alloc("ht", [128, D], F32) for _ in range(2)]
            t1m = [R1.alloc("t1m", [128, D], F32) for _ in range(2)]
            m32 = [R1.alloc("m32", [128, D], F32) for _ in range(2)]
            mb = [R1.alloc("mb", [128, D], BF16) for _ in range(2)]
            junkm = R1.alloc("junkm", [128, D], BF16)
            m32T = [R1.alloc("m32T", [128, 8, 128], F32) for _ in range(2)]
            gs2 = [R2.alloc("gs2", [128, D], F32) for _ in range(2)]
            sh2 = [R2.alloc("sh2", [128, D], F32) for _ in range(2)]
            ex16 = R2.alloc("ex16", [128, 16], F32)
            for v in range(2 if with_ctx else 1):
                load_mod(sh2[v], v, 6 * l + 3, ("sh2", v))
                load_mod(t1m[0], v, 6 * l + 4, ("t1m", 0))
                load_rep(t1m[1], norm2[l, :], ("t1m", 1))
                S.op("dve", lambda e, v=v: e.scalar_tensor_tensor(gs2[v][:], t1m[0][:], 1.0, t1m[1][:], ALU.add, ALU.mult),
                     r=[("t1m", 0), ("t1m", 1)], w=[("gs2", v)])
            def p5_stageA(j):
                b2 = j % 2
                v = 1 if j >= 16 else 0
                S.dma(lambda e, j=j, b2=b2: e.dma_start(out=ht[b2][:], in_=h_scr[j * 128:(j + 1) * 128, :]), w=[("ht", b2)])
                S.op("act", lambda e, b2=b2: e.activation(junkm[:], ht[b2][:], AF.Square, accum_out=smx[:, b2:b2 + 1]),
                     r=[("ht", b2)], w=["junkm", ("ssm", b2)])
                S.op("act", lambda e, b2=b2: e.activation(smx[:, 2 + b2:3 + b2], smx[:, b2:b2 + 1], AF.Sqrt, bias=EPS, scale=1.0 / D),
                     r=[("ssm", b2)], w=[("sdm", b2)])
                S.op("dve", lambda e, b2=b2: e.reciprocal(smx[:, 2 + b2:3 + b2], smx[:, 2 + b2:3 + b2]), r=[("sdm", b2)], w=[("sdm", b2)])
                S.op("dve", lambda e, b2=b2, v=v: e.scalar_tensor_tensor(t1m[b2][:], ht[b2][:], smx[:, 2 + b2:3 + b2], gs2[v][:], ALU.mult, ALU.mult),
                     r=[("ht", b2), ("sdm", b2), ("gs2", v)], w=[("t1m", b2)])
                S.op("pool", lambda e, b2=b2, v=v: e.tensor_tensor(m32[b2][:], t1m[b2][:], sh2[v][:], ALU.add),
                     r=[("t1m", b2), ("sh2", v)], w=[("m32", b2)])
                if j < 16:
                    S.op("pool", lambda e, b2=b2: e.tensor_copy(mb[b2][:], m32[b2][:]), r=[("m32", b2)], w=[("mb", b2)])
                    S.dma(lambda e, j=j, b2=b2: e.dma_start(out=m_loc[j * 128:(j + 1) * 128, :], in_=mb[b2][:]), r=[("mb", b2)], w=["m_loc"])
                else:
                    S.op("pool", lambda e, b2=b2, j=j: e.tensor_copy(mctx[:, j - 16, :], m32[b2][:]), r=[("m32", b2)], w=[("mctx", j - 16)])

            def p5_stageB(j):
                b2 = j % 2
                for hf in range(2):
                    pt = PS()
                    for k4 in range(4):
                        k = hf * 4 + k4
                        S.op("pe", lambda e, pt=pt, k=k, k4=k4, b2=b2: e.transpose(psb[pt][:, k4 * 128:(k4 + 1) * 128], m32[b2][:, k * 128:(k + 1) * 128], ident_f[:]),
                             r=[("m32", b2)], w=[("ps", pt)])
                    S.op("act", lambda e, pt=pt, hf=hf, b2=b2: e.copy(m32T[b2][:, hf * 4:(hf + 1) * 4, :], psb[pt][:, :].rearrange("p (a b) -> p a b", a=4)),
                         r=[("ps", pt)], w=[("m32T", b2, hf)])
                pl = PS()
                for k in range(8):
                    S.op("pe", lambda e, pl=pl, k=k, b2=b2: e.matmul(psb[pl][:, 0:16], m32T[b2][:, k, :], wr_sb[:, k, :], start=(k == 0), stop=(k == 7)),
                         r=[("m32T", b2, 0), ("m32T", b2, 1), "rt_consts"], w=[("ps", pl)])
                S.op("dve", lambda e, pl=pl, b2=b2: e.tensor_reduce(smx[:, 4 + b2:5 + b2], psb[pl][:, 0:16], AX.X, ALU.max, negate=True),
                     r=[("ps", pl)], w=[("mx", b2)])
                S.op("act", lambda e, pl=pl, b2=b2: e.activation(ex16[:], psb[pl][:, 0:16], AF.Exp, bias=smx[:, 4 + b2:5 + b2], scale=1.0,
                                                               accum_out=smx[:, 6 + b2:7 + b2]),
                     r=[("ps", pl), ("mx", b2)], w=["ex16", ("sum", b2)])
                S.op("dve", lambda e, b2=b2: e.reciprocal(smx[:, 6 + b2:7 + b2], smx[:, 6 + b2:7 + b2]), r=[("sum", b2)], w=[("sum", b2)])
                S.op("dve", lambda e, b2=b2, j=j: e.tensor_scalar(aff_sb[:, j, :], ex16[:], smx[:, 6 + b2:7 + b2], None, ALU.mult),
                     r=["ex16", ("sum", b2)], w=["aff_sb"])
            p5_stageA(0)
            for j in range(NQm):
                if j + 1 < NQm:
                    p5_stageA(j + 1)
                p5_stageB(j)
            S.barrier()
            S.dma(lambda e: e.dma_start(out=aff_loc, in_=aff_sb[:, 0:16, :]), w=["aff_loc"])
            S.cc(lambda e: e.collective_compute("AllGather", ALU.bypass, replica_groups=GROUPS,
                                                ins=[aff_loc.rearrange("p j e -> p (j e)")],
                                                outs=[aff_all.rearrange("r p j e -> (r p) (j e)")]), r=["aff_loc"], w=["aff_all"])
            for c in range(4):
                S.cc(lambda e, c=c: e.collective_compute("AllGather", ALU.bypass, replica_groups=GROUPS,
                                                         ins=[m_loc[c * 512:(c + 1) * 512, :]],
                                                         outs=[m_all[c].rearrange("r i d -> (r i) d")]), w=[("m_all", c)])
            S.dma(lambda e: e.dma_start(out=affA[:, 0:64, :].rearrange("p (r j) e -> p r j e", r=4),
                                        in_=aff_all.rearrange("r p j e -> p r j e")), r=["aff_all"], w=["affA"])
            if with_ctx:
                S.op("dve", lambda e: e.tensor_copy(affA[:, 64:66, :], aff_sb[:, 16:18, :]), w=["affAc"])
            KN = 2 if with_ctx else 1
            lo, hi, mid, cnt, ge, tq = (thr[:, i, 0:KN, :] for i in range(6))
            S.op("dve", lambda e: e.memset(thr[:, 0, :, :], 0.0), w=["lo"])
            kinds = [(0, 0, 64)] + ([(1, 64, 66)] if with_ctx else [])
            for it in range(30):
                step = 2.0 ** -(it + 1)
                S.op("dve", lambda e, step=step: e.tensor_scalar(mid, lo, step, None, ALU.add), r=["lo"], w=["mid"])
                for (kd, t0, t1_) in kinds:
                    S.op("dve", lambda e, kd=kd, t0=t0, t1_=t1_: e.tensor_tensor(
                        cmpA[:, t0:t1_, :], affA[:, t0:t1_, :], thr[:, 2, kd:kd + 1, :].to_broadcast([128, t1_ - t0, 16]), ALU.is_ge),
                        r=["mid", "affA", "affAc"], w=[("cmp", kd)])
                    S.op("dve", lambda e, kd=kd, t0=t0, t1_=t1_: e.tensor_reduce(
                        thr[:, 3, kd, :], cmpA[:, t0:t1_, :].rearrange("p t e -> p e t"), AX.X, ALU.add),
                        r=[("cmp", kd)], w=[("cnt", kd)])
                pcn = PS()
                S.op("pe", lambda e, pcn=pcn: e.matmul(psb[pcn][:, 0:KN * 16], ones_f[:], cnt.rearrange("p k e -> p (k e)"), start=True, stop=True),
                     r=[("cnt", 0), ("cnt", 1)], w=[("ps", pcn)])
                S.op("dve", lambda e, pcn=pcn: e.tensor_tensor(ge, psb[pcn][:, 0:KN * 16].rearrange("p (k e) -> p k e", k=KN), tgt[:, 0:KN, :], ALU.is_ge),
                     r=[("ps", pcn), "rt_consts"], w=["ge"])
                S.op("dve", lambda e, step=step: e.scalar_tensor_tensor(lo, ge, step, lo, ALU.mult, ALU.add), r=["ge", "mid"], w=["lo"])
            for (kd, t0, t1_) in kinds:
                S.op("dve", lambda e, kd=kd, t0=t0, t1_=t1_: e.tensor_tensor(
                    maskA[:, t0:t1_, :], affA[:, t0:t1_, :], thr[:, 0, kd:kd + 1, :].to_broadcast([128, t1_ - t0, 16]), ALU.is_ge),
                    r=["lo", "affA", "affAc"], w=[("mask", kd)])
            MK = [("mask", 0), ("mask", 1)]
            S.op("pool", lambda e: e.tensor_copy(mask_b[:, 0:NTm, :], maskA[:, 0:NTm, :]), r=MK, w=["mask_b"])
            S.op("dve", lambda e: e.tensor_tensor(maskO[:, 0:16, :], aff_sb[:, 0:16, :], thr[:, 0, 0:1, :].to_broadcast([128, 16, 16]), ALU.is_ge),
                 r=["lo"], w=["maskO"])
            if with_ctx:
                S.op("dve", lambda e: e.tensor_copy(maskO[:, 16:18, :], maskA[:, 64:66, :]), r=MK, w=["maskOc"])
            S.op("dve", lambda e: e.tensor_tensor(gmO[:, 0:NQm, :], aff_sb[:, 0:NQm, :], maskO[:, 0:NQm, :], ALU.mult),
                 r=["maskO", "maskOc"], w=["gmO"])
            mbf = mask_b[:, 0:NTm, :].rearrange("p t e -> p (t e)")
            posf = posA[:, 0:NTm, :].rearrange("p t e -> p (t e)")
            totf = totA[:, 0:NTm, :].rearrange("p t e -> p (t e)")
            c0 = 0
            while c0 < NC:
                n = min(512, NC - c0)
                pw, ptt = PS(), PS()
                S.op("pe", lambda e, pw=pw, c0=c0, n=n: e.matmul(psb[pw][:, 0:n], ust[:], mbf[:, c0:c0 + n], start=True, stop=True),
                     r=["mask_b", "rt_consts"], w=[("ps", pw)])
                S.op("pe", lambda e, ptt=ptt, c0=c0, n=n: e.matmul(psb[ptt][:, 0:n], ones_b[:], mbf[:, c0:c0 + n], start=True, stop=True),
                     r=["mask_b"], w=[("ps", ptt)])
                S.op("act", lambda e, pw=pw, c0=c0, n=n: e.copy(posf[:, c0:c0 + n], psb[pw][:, 0:n]), r=[("ps", pw)], w=["posw"])
                S.op("act", lambda e, ptt=ptt, c0=c0, n=n: e.copy(totf[:, c0:c0 + n], psb[ptt][:, 0:n]), r=[("ps", ptt)], w=["tot"])
                c0 += n
            S.op("dve", lambda e: e.tensor_reduce(rt[:], totA[:, 0:64, :].rearrange("p (r j) e -> p r e j", r=4), AX.X, ALU.add),
                 r=["tot"], w=["rt"])
            S.op("dve", lambda e: e.tensor_tensor(rt[:], rt[:], ltq[:, :].unsqueeze(2).to_broadcast([128, 4, 16]), ALU.mult),
                 r=["rt", "rt_consts"], w=["rt"])
            S.op("dve", lambda e: e.tensor_reduce(baseq[:], rt[:].rearrange("p r e -> p e r"), AX.X, ALU.add), r=["rt"], w=["baseq"])
            src, dst = totA, totB
            S.op("dve", lambda e: e.tensor_copy(cmpA[:, 0:NTm, :], totA[:, 0:NTm, :]), r=["tot"], w=["tot0"])
            sh = 1
            while sh < 64:
                S.op("dve", lambda e, src=src, dst=dst, sh=sh: e.tensor_copy(dst[:, 0:sh, :], src[:, 0:sh, :]), r=["tot", "scan"], w=["scan_a"])
                S.op("dve", lambda e, src=src, dst=dst, sh=sh: e.tensor_tensor(dst[:, sh:64, :], src[:, sh:64, :], src[:, 0:64 - sh, :], ALU.add),
                     r=["tot", "scan", "scan_a"], w=["scan"])
                src, dst = dst, src
                sh *= 2
            incl = src
            if with_ctx:
                S.op("dve", lambda e, incl=incl: e.tensor_copy(incl[:, 64:65, :], cmpA[:, 64:65, :]), r=["tot0", "scan"], w=["scanc"])
                S.op("dve", lambda e, incl=incl: e.tensor_tensor(incl[:, 65:66, :], cmpA[:, 64:65, :], cmpA[:, 65:66, :], ALU.add),
                     r=["tot0", "scan"], w=["scanc2"])
            S.op("dve", lambda e, incl=incl: e.tensor_tensor(incl[:, 0:NTm, :], incl[:, 0:NTm, :], cmpA[:, 0:NTm, :], ALU.subtract),
                 r=["scan", "scanc", "scanc2", "tot0"], w=["excl"])
            S.op("dve", lambda e, incl=incl: e.tensor_tensor(posA[:, 0:NTm, :], posA[:, 0:NTm, :], incl[:, 0:NTm, :], ALU.add),
                 r=["excl", "posw"], w=["pos"])
            mo_b = mask_b
            S.op("pool", lambda e: e.tensor_copy(mo_b[:, 0:16, :], maskO[:, 0:16, :]), r=["maskO", "posw", "tot"], w=["mo_b"])
            mof = mo_b[:, 0:16, :].rearrange("p t e -> p (t e)")
            pw, ptt = PS(), PS()
            S.op("pe", lambda e, pw=pw: e.matmul(psb[pw][:, 0:256], ust[:], mof, start=True, stop=True), r=["mo_b"], w=[("ps", pw)])
            S.op("pe", lambda e, ptt=ptt: e.matmul(psb[ptt][:, 0:256], ones_b[:], mof, start=True, stop=True), r=["mo_b"], w=[("ps", ptt)])
            S.op("act", lambda e, pw=pw: e.copy(posO[:, 0:16, :].rearrange("p t e -> p (t e)"), psb[pw][:, 0:256]), r=[("ps", pw)], w=["posOw"])
            S.op("act", lambda e, ptt=ptt: e.copy(tmpO[:, 0:16, :].rearrange("p t e -> p (t e)"), psb[ptt][:, 0:256]), r=[("ps", ptt)], w=["totO"])
            S.op("dve", lambda e: e.tensor_copy(cmpA[:, 0:16, :], tmpO[:, 0:16, :]), r=["totO", "pos", "excl"], w=["totO0"])
            srcO, dstO = tmpO, tmpO2
            sh = 1
            while sh < 16:
                S.op("dve", lambda e, srcO=srcO, dstO=dstO, sh=sh: e.tensor_copy(dstO[:, 0:sh, :], srcO[:, 0:sh, :]), r=["totO", "scanO"], w=["scanO_a"])
                S.op("dve", lambda e, srcO=srcO, dstO=dstO, sh=sh: e.tensor_tensor(dstO[:, sh:16, :], srcO[:, sh:16, :], srcO[:, 0:16 - sh, :], ALU.add),
                     r=["totO", "scanO", "scanO_a"], w=["scanO"])
                srcO, dstO = dstO, srcO
                sh *= 2
            inclO = srcO
            S.op("dve", lambda e, inclO=inclO: e.tensor_tensor(inclO[:, 0:16, :], inclO[:, 0:16, :], cmpA[:, 0:16, :], ALU.subtract),
                 r=["scanO", "totO0"], w=["exclO"])
            S.op("dve", lambda e, inclO=inclO: e.tensor_tensor(inclO[:, 0:16, :], inclO[:, 0:16, :], baseq[:, :].unsqueeze(1).to_broadcast([128, 16, 16]), ALU.add),
                 r=["exclO", "baseq"], w=["exclO2"])
            S.op("dve", lambda e, inclO=inclO: e.tensor_tensor(posO[:, 0:16, :], posO[:, 0:16, :], inclO[:, 0:16, :], ALU.add),
                 r=["exclO2", "posOw"], w=["posO"])
            if with_ctx:
                S.op("dve", lambda e: e.tensor_scalar(posO[:, 16:18, :], posA[:, 64:66, :], 1024.0, None, ALU.add), r=["pos"], w=["posOc"])
            PO = posO[:, 0:NQm, :]; TA = tmpO[:, 0:NQm, :]; TB = tmpO2[:, 0:NQm, :]
            S.op("dve", lambda e: e.tensor_scalar(TA, PO, 352.0, 1056.0, ALU.is_ge, ALU.mult), r=["posO", "posOc", "exclO2"], w=["TA"])
            S.op("dve", lambda e: e.tensor_scalar(TB, PO, 704.0, 1056.0, ALU.is_ge, ALU.mult), r=["posO", "posOc", "exclO2"], w=["TB"])
            S.op("dve", lambda e: e.tensor_tensor(TA, TA, TB, ALU.add), r=["TA", "TB"], w=["TA"])
            S.op("dve", lambda e: e.tensor_tensor(TA, TA, PO, ALU.add), r=["TA"], w=["TA"])
            S.op("dve", lambda e: e.tensor_tensor(TA, TA, cbe[:, :].unsqueeze(1).to_broadcast([128, NQm, 16]), ALU.add), r=["TA"], w=["TA"])
            S.op("dve", lambda e: e.tensor_scalar(TB, maskO[:, 0:NQm, :], -1.0e8, 1.0e8, ALU.mult, ALU.add), r=["TB", "TA", "maskO", "maskOc"], w=["TB"])
            S.op("dve", lambda e: e.tensor_tensor(TA, TA, TB, ALU.add), r=["TA", "TB"], w=["TA"])
            S.op("dve", lambda e: e.tensor_copy(rowO[:, 0:NQm, :], TA), r=["TA"], w=["rowO"])
            S.barrier()
            dbg_dump("posA%d" % l, posA[:, 0:NTm, :], [128, NTm, 16], F32, [])
            dbg_dump("maskA%d" % l, maskA[:, 0:NTm, :], [128, NTm, 16], F32, [])
            dbg_dump("affA%d" % l, affA[:, 0:NTm, :], [128, NTm, 16], F32, [])
            dbg_dump("posO%d" % l, posO[:, 0:NQm, :], [128, NQm, 16], F32, [])
            dbg_dump("rowO%d" % l, rowO[:, 0:NQm, :], [128, NQm, 16], I32, [])
            dbg_dump("gmO%d" % l, gmO[:, 0:NQm, :], [128, NQm, 16], F32, [])
            if "stopR" in dbg:
                return
            for R in (R1, R2):
                R.reset()
            xsT = R1.alloc("xsT", [128, 8, 1056], BF16)
            hT = R1.alloc("hT", [128, 16, 1056], BF16)
            xs = [R1.alloc("xs", [128, D], BF16) for _ in range(4)]
            sa = [R1.alloc("sa", [128, 512], BF16) for _ in range(2)]
            wgu = [R2.alloc("wgu", [128, 2, 8, 256], BF16) for _ in range(2)]
            wd_b = R2.alloc("wd_b", [128, 16, 512], BF16)
            stg = [R2.alloc("stg", [128, 8, 256], F32) for _ in range(3)]
            iota = R2.alloc("iota", [128, 1024], F32)
            oh = [R2.alloc("oh", [128, 1024], BF16) for _ in range(2)]
            ohc = R2.alloc("ohc", [128, 2, 32], BF16)
            pj = R2.alloc("pj", [128, 66], F32); mj = R2.alloc("mj", [128, 66], F32)
            idxr = R2.alloc("idxr", [2, 1024], F32)
            idxc = R2.alloc("idxc", [128, 8, 2], F32)
            idxf = R2.alloc("idxf", [128, 8], F32)
            idxi = R2.alloc("idxi", [128, 8], I32)
            ybuf = [R2.alloc("ybuf", [128, 512], BF16) for _ in range(2)]
            S.dma(lambda e: e.dma_start(out=iota[:], in_=iota_d), w=["iota"])
            sc = {"stg": 0, "oh": 0, "xs": 0, "sa": 0, "yb": 0, "wgu": 0}
            MALL = [("m_all", c) for c in range(4)]
            m_flat = m_all.rearrange("c r i d -> (c r i) d")
            ps_rng[0], ps_rng[1] = 0, 6
            xsT2 = R3b.alloc("xsT2", [128, 8, 1056], BF16)
            XST = [xsT, xsT] if "samexst" in dbg else [xsT, xsT2]
            nbs = [(0, 512), (512, 512)] + ([(1024, 32)] if with_ctx else [])
            sts = [(st * 128, 128) for st in range(8)] + ([(1024, 32)] if with_ctx else [])

            def prep_select(je):
                S.op("dve", lambda e: e.tensor_tensor(cmpA[:, 0:NTm, :], posA[:, 0:NTm, :], esel[:, je:je + 1, :].to_broadcast([128, NTm, 16]), ALU.mult),
                     r=["pj", "mj"], w=["cmpsel"])
                S.op("dve", lambda e: e.tensor_reduce(pj[:, 0:NTm], cmpA[:, 0:NTm, :], AX.X, ALU.add), r=["cmpsel"], w=["pj"])
                S.op("dve", lambda e: e.tensor_tensor(cmpA[:, 0:NTm, :], maskA[:, 0:NTm, :], esel[:, je:je + 1, :].to_broadcast([128, NTm, 16]), ALU.mult),
                     r=["pj"], w=["cmpsel"])
                S.op("dve", lambda e: e.tensor_reduce(mj[:, 0:NTm], cmpA[:, 0:NTm, :], AX.X, ALU.add), r=["cmpsel"], w=["mj"])

            def prep_oh(t):
                oi = t % 2
                S.op("dve", lambda e: e.tensor_scalar(oh[oi][:], iota[:], pj[:, t:t + 1], mj[:, t:t + 1], ALU.is_equal, ALU.mult),
                     r=["pj", "mj", "iota"], w=[("oh", oi)])

            def prep_idxmm(t):
                oi = t % 2
                for hf, pr in ((0, 6), (1, 7)):
                    S.op("pe", lambda e, hf=hf, pr=pr: e.matmul(psb[pr][0:2, :], rowid[:, t, :], oh[oi][:, hf * 512:(hf + 1) * 512],
                                                                start=(t == 0), stop=(t == 63)), r=[("oh", oi)], w=[("ps", pr)])

            def prep_finalize():
                S.op("act", lambda e: e.copy(idxr[:, 0:512], psb[6][0:2, :]), r=[("ps", 6)], w=["idxr0"])
                S.op("act", lambda e: e.copy(idxr[:, 512:1024], psb[7][0:2, :]), r=[("ps", 7)], w=["idxr1"])
                pi2 = PS()
                for st in range(8):
                    S.op("pe", lambda e, st=st: e.matmul(psb[pi2][:, st * 2:st * 2 + 2], idxr[0:2, st * 128:(st + 1) * 128], ident_f[0:2, 0:2],
                                                         start=True, stop=True), r=["idxr0", "idxr1"], w=[("ps", pi2)])
                S.op("dve", lambda e: e.tensor_copy(idxc[:], psb[pi2][:, 0:16].rearrange("p (s c) -> p s c", c=2)), r=[("ps", pi2)], w=["idxc"])
                S.op("dve", lambda e: e.scalar_tensor_tensor(idxf[:], idxc[:, :, 1], 64.0, idxc[:, :, 0], ALU.mult, ALU.add), r=["idxc"], w=["idxf"])
                S.op("dve", lambda e: e.tensor_copy(idxi[:], idxf[:]), r=["idxf"], w=["idxi"])

            def prep_gather(st):
                xi = st % 4
                S.dma(lambda e: e.indirect_dma_start(
                    out=xs[xi][:], out_offset=None, in_=m_flat, in_offset=bass.IndirectOffsetOnAxis(ap=idxi[:, st:st + 1], axis=0),
                    bounds_check=S.pool_regs[8191], oob_is_err=False), r=["idxi"] + MALL, w=[("xs", xi)], eng="pool")

            def prep_transpose(je, st):
                xi = st % 4
                X = XST[je % 2]
                ptr = PS()
                psT = psb[ptr][:].bitcast(BF16).rearrange("p (k c) -> p k c", k=8)
                for k in range(8):
                    S.op("pe", lambda e, k=k: e.transpose(psT[:, k, :], xs[xi][:, k * 128:(k + 1) * 128], ident_b[:]),
                         r=[("xs", xi)], w=[("ps", ptr)])
                if st % 2 == 0:
                    S.op("act", lambda e: e.copy(X[:, :, st * 128:(st + 1) * 128], psT), r=[("ps", ptr)], w=[("xsT", je % 2, st)])
                else:
                    S.op("dve", lambda e: e.tensor_copy(X[:, :, st * 128:(st + 1) * 128], psT), r=[("ps", ptr)], w=[("xsT", je % 2, st)])

            def prep_ctx(je):
                X = XST[je % 2]
                for tl in range(2):
                    S.op("dve", lambda e, tl=tl: e.tensor_scalar(ohc[:, tl, :], iota[:, 0:32], pj[:, 64 + tl:65 + tl], mj[:, 64 + tl:65 + tl], ALU.is_equal, ALU.mult),
                         r=["pj", "mj", "iota"], w=[("ohc", tl)])
                pcx = PS()
                for k in range(8):
                    for tl in range(2):
                        S.op("pe", lambda e, k=k, tl=tl: e.matmul(psb[pcx][:, k * 32:(k + 1) * 32], mctx[:, tl, k * 128:(k + 1) * 128], ohc[:, tl, :],
                                                                  start=(tl == 0), stop=(tl == 1)), r=[("ohc", 0), ("ohc", 1)], w=[("ps", pcx)])
                S.op("act", lambda e: e.copy(X[:, :, 1024:1056], psb[pcx][:, 0:256].rearrange("p (k c) -> p k c", k=8)), r=[("ps", pcx)], w=[("xsT", je % 2, 8)])

            prep_select(0)
            for t in range(64):
                prep_oh(t)
                prep_idxmm(t)
            prep_finalize()
            for st in range(8):
                prep_gather(st)
                prep_transpose(0, st)
            if with_ctx:
                prep_ctx(0)
            for je in range(4):
                nxt = je + 1 < 4
                if "serialprep" in dbg:
                    if je > 0:
                        prep_select(je)
                        for t in range(64):
                            prep_oh(t)
                            prep_idxmm(t)
                        prep_finalize()
                        for st in range(8):
                            prep_gather(st)
                            prep_transpose(je, st)
                        if with_ctx:
                            prep_ctx(je)
                    nxt = False
                X = XST[je % 2]
                XS = [("xsT", je % 2, st) for st in range(9 if with_ctx else 8)]
                if nxt:
                    prep_select(je + 1)
                tq = list(range(64)) if nxt else []
                for fc in range(8):
                    wi = sc["wgu"] % 2; sc["wgu"] += 1
                    for gi, wsrc in ((0, moe_g), (1, moe_u)):
                        si = sc["stg"] % 3; sc["stg"] += 1
                        S.dma(lambda e, si=si, wsrc=wsrc, fc=fc, je=je: e.dma_start(
                            out=stg[si][:], in_=wsrc[l, je, :, fc * 256:(fc + 1) * 256].rearrange("(k p) f -> p k f", p=128)), w=[("stg", si)])
                        S.op("act", lambda e, si=si, wi=wi, gi=gi: e.copy(wgu[wi][:, gi, :, :], stg[si][:]), r=[("stg", si)], w=[("wgu", wi, gi)])
                    for f2 in range(2):
                        f = fc * 2 + f2
                        for (n0, nn) in nbs:
                            mine = [tq.pop(0) for _ in range(min(2, len(tq)))]
                            for t in mine:
                                prep_oh(t)
                            pa, pu = PS(), PS()
                            for k in range(8):
                                S.op("pe", lambda e, pa=pa, k=k, wi=wi, f2=f2, n0=n0, nn=nn, X=X: e.matmul(
                                    psb[pa][:, 0:nn], wgu[wi][:, 0, k, f2 * 128:(f2 + 1) * 128], X[:, k, n0:n0 + nn], start=(k == 0), stop=(k == 7)),
                                    r=[("wgu", wi, 0)] + XS, w=[("ps", pa)])
                            for k in range(8):
                                S.op("pe", lambda e, pu=pu, k=k, wi=wi, f2=f2, n0=n0, nn=nn, X=X: e.matmul(
                                    psb[pu][:, 0:nn], wgu[wi][:, 1, k, f2 * 128:(f2 + 1) * 128], X[:, k, n0:n0 + nn], start=(k == 0), stop=(k == 7)),
                                    r=[("wgu", wi, 1)] + XS, w=[("ps", pu)])
                            ai = sc["sa"] % 2; sc["sa"] += 1
                            S.op("act", lambda e, pa=pa, ai=ai, nn=nn: e.activation(sa[ai][:, 0:nn], psb[pa][:, 0:nn], AF.Silu), r=[("ps", pa)], w=[("sa", ai)])
                            S.op("dve", lambda e, pu=pu, ai=ai, f=f, n0=n0, nn=nn: e.tensor_tensor(hT[:, f, n0:n0 + nn], psb[pu][:, 0:nn], sa[ai][:, 0:nn], ALU.mult),
                                 r=[("ps", pu), ("sa", ai)], w=[("hT", f)])
                            for t in mine:
                                prep_idxmm(t)
                while tq:
                    t = tq.pop(0)
                    prep_oh(t)
                    prep_idxmm(t)
                if nxt:
                    prep_finalize()
                HT = [("hT", f) for f in range(16)]
                ydmas = []
                gq = list(range(8)) if nxt else []
                tqx = []
                for hf in range(2):
                    for f4 in range(4):
                        si = sc["stg"] % 3; sc["stg"] += 1
                        S.dma(lambda e, si=si, f4=f4, hf=hf, je=je: e.dma_start(
                            out=stg[si][:].rearrange("p (a b) c -> p a (b c)", a=4),
                            in_=moe_d[l, je, f4 * 512:(f4 + 1) * 512, hf * 512:(hf + 1) * 512].rearrange("(a p) c -> p a c", p=128)), w=[("stg", si)])
                        S.op("act", lambda e, si=si, f4=f4: e.copy(wd_b[:, f4 * 4:(f4 + 1) * 4, :], stg[si][:].rearrange("p (a b) c -> p a (b c)", a=4)),
                             r=[("stg", si)], w=[("wd_b", f4)])
                    for (s0, sn) in sts:
                        if gq:
                            g_ = gq.pop(0)
                            prep_gather(g_)
                            tqx.append(g_)
                        py = PS()
                        for f in range(16):
                            S.op("pe", lambda e, py=py, f=f, s0=s0, sn=sn: e.matmul(psb[py][0:sn, :], hT[:, f, s0:s0 + sn], wd_b[:, f, :], start=(f == 0), stop=(f == 15)),
                                 r=HT + [("wd_b", i) for i in range(4)], w=[("ps", py)])
                        yi = sc["yb"] % 2; sc["yb"] += 1
                        S.op("act", lambda e, py=py, yi=yi, sn=sn: e.copy(ybuf[yi][0:sn, :], psb[py][0:sn, :]), r=[("ps", py)], w=[("ybuf", yi)])
                        pieces = []
                        s_ = s0
                        while s_ < s0 + sn:
                            ch = s_ // 352
                            e1 = min(s0 + sn, (ch + 1) * 352)
                            pieces.append((s_, e1, ch))
                            s_ = e1
                        yd = S.dma(lambda e, pieces=pieces, yi=yi, s0=s0, hf=hf, je=je: [
                            e.dma_start(out=y_loc[3 * je + ch, a - ch * 352:b_ - ch * 352, hf * 512:(hf + 1) * 512], in_=ybuf[yi][a - s0:b_ - s0, :])
                            for (a, b_, ch) in pieces], r=[("ybuf", yi)], w=[("y_loc", je)], n=len(pieces))
                        ydmas.append(yd)
                        if len(tqx) > 1 or (tqx and not gq):
                            prep_transpose(je + 1, tqx.pop(0))
                while tqx:
                    prep_transpose(je + 1, tqx.pop(0))
                if nxt and with_ctx:
                    prep_ctx(je + 1)
                for c3 in range(3):
                    S.cc(lambda e, c3=c3, je=je: e.collective_compute("AllGather", ALU.bypass, replica_groups=GROUPS,
                                                                     ins=[y_loc[3 * je + c3]], outs=[y_all[3 * je + c3].rearrange("r i d -> (r i) d")]),
                         r=[("y_loc", je)], w=[("y_all", je)], after=ydmas)
            ps_rng[0], ps_rng[1] = 0, 8
            S.barrier()
            for R in (R1, R2):
                R.reset()
            G = [R1.alloc("G", [128, D], BF16) for _ in range(6)]
            dgm = [R1.alloc("dgm", [128, 128], BF16) for _ in range(6)]
            acc = [R1.alloc("acc", [128, D], F32) for _ in range(2)]
            hcb = [R1.alloc("hcb", [128, D], F32) for _ in range(2)]
            g2r = [R2.alloc("g2r", [128, D], F32) for _ in range(2)]
            for v in range(2 if with_ctx else 1):
                load_mod(g2r[v], v, 6 * l + 5, ("g2r", v))
            for gi in range(6):
                S.op("pool", lambda e, gi=gi: e.memset(G[gi][:], 0.0), w=[("G", gi)])
            y_flat = y_all.rearrange("c r i d -> (c r i) d")
            gc = 0

            def _dbg_ind(j, ee, gi):
                if "trace_ind" in dbg:
                    print("IND", j, ee, gi, flush=True)
                return None
            for j in range(NQm):
                b2 = j % 2
                v = 1 if j >= 16 else 0
                S.dma(lambda e, j=j, b2=b2: e.dma_start(out=hcb[b2][:], in_=h_scr[j * 128:(j + 1) * 128, :]), w=[("hcb", b2)])
                pyc = [PS(), PS()]
                for ee in range(16):
                    gi = gc % 6; gc += 1
                    S.dma(lambda e, j=j, ee=ee, gi=gi: _dbg_ind(j, ee, gi) or e.indirect_dma_start(
                        out=G[gi][:], out_offset=None, in_=y_flat, in_offset=bass.IndirectOffsetOnAxis(ap=rowO[:].rearrange("p j e -> p (j e)")[:, j * 16 + ee:j * 16 + ee + 1], axis=0),
                        bounds_check=S.pool_regs[12 * 4 * 352 - 1], oob_is_err=False), w=[("G", gi)], eng="pool")
                    di = gi
                    S.op("dve", lambda e, di=di, j=j, ee=ee: e.tensor_scalar(dgm[di][:], ident_b[:], gmO[:, j, ee:ee + 1], None, ALU.mult), w=[("dgm", di)])
                    for hf in range(2):
                        S.op("pe", lambda e, hf=hf, gi=gi, di=di, ee=ee, pyc=pyc: e.matmul(psb[pyc[hf]][:, :], dgm[di][:], G[gi][:, hf * 512:(hf + 1) * 512],
                                                                                  start=(ee == 0), stop=(ee == 15)),
                             r=[("G", gi), ("dgm", di)], w=[("ps", pyc[hf])])
                for hf in range(2):
                    S.op("dve", lambda e, hf=hf, b2=b2, v=v, pyc=pyc: e.tensor_tensor(acc[b2][:, hf * 512:(hf + 1) * 512], psb[pyc[hf]][:, :], g2r[v][:, hf * 512:(hf + 1) * 512], ALU.mult),
                         r=[("ps", pyc[hf]), ("g2r", v)], w=[("acc", b2)])
                S.op("dve", lambda e, b2=b2: e.tensor_tensor(hcb[b2][:], hcb[b2][:], acc[b2][:], ALU.add), r=[("acc", b2), ("hcb", b2)], w=[("hcb", b2)])
                S.dma(lambda e, j=j, b2=b2: e.dma_start(out=h_scr[j * 128:(j + 1) * 128, :], in_=hcb[b2][:]), r=[("hcb", b2)], w=["h_scr"])
            S.barrier()

        if stage >= 5:
            moe(0, True)
            if "hmoe" in dbg:
                o = dout("dbg_hmoe", [NQRY, D])
                S.dma(lambda e, o=o: e.dma_start(out=o, in_=h_scr), w=["dbg_hmoe"])
                outs.append("dbg_hmoe")

        if stage >= 6:
            S.barrier()
            for R in (R1, R2, R3, R4):
                R.reset()
            QT1 = R2.alloc("QT1", [128, 8, 2048], BF16)
            kTs = R2.alloc("kTs", [128, 2, 2048], BF16)
            kTc = R2.alloc("kTc", [128, 2, 256], BF16)
            vctx = R2.alloc("vctx", [128, 2, 256], BF16)
            cwin_b = R3.alloc("cwin_b", [128, 8, 1536], BF16)
            gsr = [R3.alloc("gsr", [128, D], F32) for _ in range(2)]
            cosr = R2.alloc("cosr", [128, 16, 128], F32)
            sinr = R2.alloc("sinr", [128, 16, 128], F32)
            xt1 = [R1.alloc("xt1", [128, D], F32) for _ in range(2)]
            t11 = [R1.alloc("t11", [128, D], F32) for _ in range(2)]
            ab1 = [R1.alloc("ab1", [128, D], BF16) for _ in range(2)]
            junk1 = R1.alloc("junk1", [128, D], BF16)
            aTt = [R1.alloc("aTt", [128, 8, 128], BF16) for _ in range(2)]
            sq1 = R1.alloc("sq1", [128, 10, 128], F32)
            qn1 = R1.alloc("qn1", [128, 10, 128], F32)
            tr1 = R1.alloc("tr1", [128, 10, 128], F32)
            tr2 = R1.alloc("tr2", [128, 10, 128], F32)
            qr1 = [R1.alloc("qr1", [128, 10, 128], BF16) for _ in range(2)]
            vb1 = [R1.alloc("vb1", [128, 256], BF16) for _ in range(2)]
            shr = [R4.alloc("shr", [128, D], F32) for _ in range(2)]
            gqr = R4.alloc("gqr", [128, 128], F32); gkr = R4.alloc("gkr", [128, 128], F32)
            ss1 = R4.alloc("ss1", [128, 8], F32)
            rs1 = R4.alloc("rs1", [128, 2, 10], F32)
            wst4 = [R4.alloc("wst4", [128, 768], F32) for _ in range(2)]
            wc = 0
            for k in range(8):
                for hf in range(2):
                    wi = wc % 2; wc += 1
                    S.dma(lambda e, k=k, hf=hf, wi=wi: e.dma_start(out=wst4[wi][:], in_=c_w_in[k * 128:(k + 1) * 128, hf * 768:(hf + 1) * 768]), w=[("wst4", wi)])
                    S.op("pool", lambda e, k=k, hf=hf, wi=wi: e.tensor_copy(cwin_b[:, k, hf * 768:(hf + 1) * 768], wst4[wi][:]), r=[("wst4", wi)], w=["cwin_b"])
            S.dma(lambda e: [e.dma_start(out=cosr[:], in_=cos1o), e.dma_start(out=sinr[:], in_=sin1o)], w=["ropet"], n=2)
            load_rep(gqr, c_q_gain, "gqr"); load_rep(gkr, c_k_gain, "gkr")
            for v in range(2):
                load_mod(shr[v], v, 6 + 0, ("shr", v))
                load_mod(t11[0], v, 6 + 1, ("t11", 0))
                load_rep(t11[1], norm1[1, :], ("t11", 1))
                S.op("dve", lambda e, v=v: e.scalar_tensor_tensor(gsr[v][:], t11[0][:], 1.0, t11[1][:], ALU.add, ALU.mult),
                     r=[("t11", 0), ("t11", 1)], w=[("gsr", v)])
            def p9_stageA(j):
                b2 = j % 2
                isc = j >= 16
                v = 1 if isc else 0
                S.dma(lambda e, j=j, b2=b2: e.dma_start(out=xt1[b2][:], in_=h_scr[j * 128:(j + 1) * 128, :]), w=[("xt1", b2)])
                S.op("act", lambda e, b2=b2: e.activation(junk1[:], xt1[b2][:], AF.Square, accum_out=ss1[:, b2:b2 + 1]), r=[("xt1", b2)], w=["junk1", ("ss1", b2)])
                S.op("act", lambda e, b2=b2: e.activation(ss1[:, 2 + b2:3 + b2], ss1[:, b2:b2 + 1], AF.Sqrt, bias=EPS, scale=1.0 / D), r=[("ss1", b2)], w=[("sd1", b2)])
                S.op("dve", lambda e, b2=b2: e.reciprocal(ss1[:, 2 + b2:3 + b2], ss1[:, 2 + b2:3 + b2]), r=[("sd1", b2)], w=[("sd1", b2)])
                S.op("dve", lambda e, b2=b2, v=v: e.scalar_tensor_tensor(t11[b2][:], xt1[b2][:], ss1[:, 2 + b2:3 + b2], gsr[v][:], ALU.mult, ALU.mult),
                     r=[("xt1", b2), ("sd1", b2), ("gsr", v)], w=[("t11", b2)])
                S.op("pool", lambda e, b2=b2, v=v: e.tensor_tensor(ab1[b2][:], t11[b2][:], shr[v][:], ALU.add), r=[("t11", b2), ("shr", v)], w=[("ab1", b2)])

            def p9_stageB(j):
                b2 = j % 2
                isc = j >= 16
                v = 1 if isc else 0
                pt = PS()
                psT = psb[pt][:].bitcast(BF16).rearrange("p (k c) -> p k c", k=8)
                for k in range(8):
                    S.op("pe", lambda e, psT=psT, k=k, b2=b2: e.transpose(psT[:, k, :], ab1[b2][:, k * 128:(k + 1) * 128], ident_b[:]), r=[("ab1", b2)], w=[("ps", pt)])
                S.op("act", lambda e, psT=psT, b2=b2: e.copy(aTt[b2][:], psT), r=[("ps", pt)], w=[("aTt", b2)])
                pq = [PS(), PS(), PS()]
                for c3 in range(3):
                    for k in range(8):
                        S.op("pe", lambda e, c3=c3, k=k, b2=b2, pq=pq: e.matmul(psb[pq[c3]][:, :], aTt[b2][:, k, :], cwin_b[:, k, c3 * 512:(c3 + 1) * 512],
                                                                            start=(k == 0), stop=(k == 7)), r=[("aTt", b2), "cwin_b"], w=[("ps", pq[c3])])
                for c3, (h0, nh) in enumerate(((0, 4), (4, 4), (8, 2))):
                    S.op("act", lambda e, c3=c3, h0=h0, nh=nh, pq=pq: e.activation(
                        sq1[:, h0:h0 + nh, :], psb[pq[c3]][:, 0:nh * 128].rearrange("p (h d) -> p h d", d=128), AF.Square),
                        r=[("ps", pq[c3])], w=[("sq1", c3)])
                S.op("dve", lambda e, b2=b2: e.tensor_reduce(rs1[:, b2, :], sq1[:], AX.X, ALU.add), r=[("sq1", 0), ("sq1", 1), ("sq1", 2)], w=[("rs1", b2)])
                S.op("act", lambda e, b2=b2: e.activation(rs1[:, b2, :], rs1[:, b2, :], AF.Sqrt, bias=EPS, scale=1.0 / 128), r=[("rs1", b2)], w=[("rs1", b2)])
                S.op("dve", lambda e, b2=b2: e.reciprocal(rs1[:, b2, :], rs1[:, b2, :]), r=[("rs1", b2)], w=[("rs1", b2)])
                for c3, (h0, nh) in enumerate(((0, 4), (4, 4), (8, 2))):
                    S.op("dve", lambda e, c3=c3, h0=h0, nh=nh, pq=pq, b2=b2: e.tensor_tensor(
                        qn1[:, h0:h0 + nh, :], psb[pq[c3]][:, 0:nh * 128].rearrange("p (h d) -> p h d", d=128),
                        rs1[:, b2, h0:h0 + nh].unsqueeze(2).to_broadcast([128, nh, 128]), ALU.mult),
                        r=[("ps", pq[c3]), ("rs1", b2)], w=[("qn1", c3)])
                vi = j % 2
                S.op("act", lambda e, pq=pq, vi=vi: e.copy(vb1[vi][:], psb[pq[2]][:, 256:512]), r=[("ps", pq[2])], w=[("vb1", vi)])
                QN = [("qn1", 0), ("qn1", 1), ("qn1", 2)]
                S.op("pool", lambda e: e.tensor_tensor(qn1[:, 0:8, :], qn1[:, 0:8, :], gqr[:, :].unsqueeze(1).to_broadcast([128, 8, 128]), ALU.mult),
                     r=QN + ["gqr"], w=["qg"])
                S.op("pool", lambda e: e.tensor_tensor(qn1[:, 8:10, :], qn1[:, 8:10, :], gkr[:, :].unsqueeze(1).to_broadcast([128, 2, 128]), ALU.mult),
                     r=QN + ["gkr"], w=["kg"])
                qi = j % 2
                if not isc:
                    S.op("dve", lambda e, j=j: e.tensor_tensor(tr1[:], qn1[:], cosr[:, j, :].unsqueeze(1).to_broadcast([128, 10, 128]), ALU.mult),
                         r=QN + ["qg", "kg", "ropet"], w=["tr1"])
                    qv = qn1[:].rearrange("p h (g a c) -> p h g a c", g=2, a=2)
                    tv = tr2[:].rearrange("p h (g a c) -> p h g a c", g=2, a=2)
                    sv = sinr[:, j, :].rearrange("p (g a c) -> p g a c", g=2, a=2)
                    for a in range(2):
                        S.op("pool", lambda e, a=a, qv=qv, tv=tv, sv=sv: e.tensor_tensor(
                            tv[:, :, :, a, :], qv[:, :, :, 1 - a, :], sv[:, :, a, :].unsqueeze(1).to_broadcast([128, 10, 2, 32]), ALU.mult),
                            r=QN + ["qg", "kg", "ropet"], w=[("tr2", a)])
                    S.op("dve", lambda e, qi=qi: e.tensor_tensor(qr1[qi][:], tr1[:], tr2[:], ALU.add), r=["tr1", ("tr2", 0), ("tr2", 1)], w=[("qr1", qi)])
                else:
                    S.op("dve", lambda e, qi=qi: e.tensor_copy(qr1[qi][:], qn1[:]), r=QN + ["qg", "kg"], w=[("qr1", qi)])
                if not isc:
                    ptq = PS()
                    psQ = psb[ptq][:].bitcast(BF16).rearrange("p (k c) -> p k c", k=8)
                    for hh in range(8):
                        S.op("pe", lambda e, psQ=psQ, hh=hh, qi=qi: e.transpose(psQ[:, hh, :], qr1[qi][:, hh, :], ident_b[:]), r=[("qr1", qi)], w=[("ps", ptq)])
                    S.op("act", lambda e, psQ=psQ, j=j: e.copy(QT1[:, :, j * 128:(j + 1) * 128], psQ), r=[("ps", ptq)], w=[("QT1", j)])
                ptk = PS()
                psK = psb[ptk][:].bitcast(BF16).rearrange("p (k c) -> p k c", k=8)
                for hh in range(2):
                    S.op("pe", lambda e, psK=psK, hh=hh, qi=qi: e.transpose(psK[:, hh, :], qr1[qi][:, 8 + hh, :], ident_b[:]), r=[("qr1", qi)], w=[("ps", ptk)])
                if not isc:
                    S.op("dve", lambda e, psK=psK, j=j: e.tensor_copy(kTs[:, :, j * 128:(j + 1) * 128], psK[:, 0:2, :]), r=[("ps", ptk)], w=["kTs"])
                    S.dma(lambda e, j=j, vi=vi: e.dma_start(out=v_loc[j * 128:(j + 1) * 128, :], in_=vb1[vi][:]), r=[("vb1", vi)], w=["v_loc"])
                else:
                    S.op("dve", lambda e, psK=psK, j=j: e.tensor_copy(kTc[:, :, (j - 16) * 128:(j - 15) * 128], psK[:, 0:2, :]), r=[("ps", ptk)], w=["kTc"])
                    S.op("pool", lambda e, j=j, vi=vi: e.tensor_copy(vctx[:, j - 16, :], vb1[vi][:]), r=[("vb1", vi)], w=["vctx"])
            p9_stageA(0)
            for j in range(NQ):
                if j + 1 < NQ:
                    p9_stageA(j + 1)
                p9_stageB(j)
            S.dma(lambda e: e.dma_start(out=kT_loc, in_=kTs[:]), r=["kTs"], w=["kT_loc"])
            S.barrier()
            S.cc(lambda e: e.collective_compute("AllGather", ALU.bypass, replica_groups=GROUPS,
                                                ins=[kT_loc.rearrange("d g t -> d (g t)")], outs=[kT_all.rearrange("r d g t -> (r d) (g t)")]), w=["kT_all"])
            S.cc(lambda e: e.collective_compute("AllGather", ALU.bypass, replica_groups=GROUPS, ins=[v_loc], outs=[v_all]), w=["v_all"])
            dbg_dump("QT1", QT1[:], [128, 8, 2048], BF16, [])
            for R in (R1, R3, R4):
                R.reset()
            KTg = R1.alloc("KTg", [128, NKEY], BF16)
            Vg = R1.alloc("Vg", [128, NT, 128], BF16)
            PT1 = [R1.alloc("PT1", [128, 1024], BF16) for _ in range(3)]
            rden1 = R1.alloc("rden1", [128, 512], F32)
            OT1 = R3.alloc("OT1", [128, 8, 2048], BF16)
            SCALE1 = 128.0 ** -0.5
            st1 = {"blk": 0, "pt": 0, "sp": 0, "ds": 0}
            dsum = [R1.alloc("dsum", [128, 512], BF16) for _ in range(2)]
            for g in range(2):
                S.dma(lambda e, g=g: e.dma_start(out=KTg[:, 0:8192].rearrange("d (r t) -> d r t", r=4), in_=kT_all[:, :, g, :].rearrange("r d t -> d r t")),
                      r=["kT_all"], w=["KTg"])
                S.op("dve", lambda e, g=g: e.tensor_copy(KTg[:, 8192:8448], kTc[:, g, :]), w=["KTgc"])
                S.dma(lambda e, g=g: e.dma_start(out=Vg[:, 0:64, :], in_=v_all[:, g * 128:(g + 1) * 128].rearrange("(t p) c -> p t c", p=128)),
                      r=["v_all"], w=["Vg"])
                S.op("pool", lambda e, g=g: e.tensor_copy(Vg[:, 64:66, :], vctx[:, :, g * 128:(g + 1) * 128]), w=["Vgc"])
                for hq in range(4):
                    hh = 4 * g + hq
                    for qb in range(4):
                        ab = st1["blk"] % 2; st1["blk"] += 1
                        po, pd = 4 + ab, 6 + ab
                        n = NT // 2
                        sbank = {}

                        def Sm(i, hh=hh, qb=qb, sbank=sbank):
                            sp = st1["sp"] % 2
                            st1["sp"] += 1
                            sbank[i] = sp
                            for hf in range(2):
                                tt = 2 * i + hf
                                S.op("pe", lambda e, sp=sp, hf=hf, tt=tt: e.matmul(psw[sp][:, hf * 512:(hf + 1) * 512], KTg[:, tt * 128:(tt + 1) * 128],
                                                                               QT1[:, hh, qb * 512:(qb + 1) * 512], start=True, stop=True),
                                     r=["KTg", "KTgc"], w=[("ps", 2 * sp), ("ps", 2 * sp + 1)])
                        Sm(0)
                        for i in range(n):
                            sp = sbank[i]
                            pbi = st1["pt"] % 3; st1["pt"] += 1
                            S.op("act", lambda e, sp=sp, pbi=pbi: e.activation(PT1[pbi][:], psw[sp][:], AF.Exp, scale=SCALE1),
                                 r=[("ps", 2 * sp), ("ps", 2 * sp + 1)], w=[("PT1", pbi)])
                            di = st1["ds"] % 2; st1["ds"] += 1
                            S.op("dve", lambda e, pbi=pbi, di=di: e.tensor_tensor(dsum[di][:], PT1[pbi][:, 0:512], PT1[pbi][:, 512:1024], ALU.add),
                                 r=[("PT1", pbi)], w=[("dsum", di)])
                            if i + 1 < n:
                                Sm(i + 1)
                            for hf in range(2):
                                tt = 2 * i + hf
                                first = (i == 0 and hf == 0)
                                last = (i == n - 1 and hf == 1)
                                S.op("pe", lambda e, tt=tt, pbi=pbi, hf=hf, po=po, first=first, last=last: e.matmul(
                                    psb[po][:, :], Vg[:, tt, :], PT1[pbi][:, hf * 512:(hf + 1) * 512], start=first, stop=last),
                                    r=["Vg", "Vgc", ("PT1", pbi)], w=[("ps", po)])
                            S.op("pe", lambda e, di=di, pd=pd, i=i: e.matmul(psb[pd][:, :], ones_b[:], dsum[di][:], start=(i == 0), stop=(i == n - 1)),
                                 r=[("dsum", di)], w=[("ps", pd)])
                        S.op("dve", lambda e, pd=pd: e.reciprocal(rden1[:], psb[pd][:, :]), r=[("ps", pd)], w=["rden1"])
                        S.op("dve", lambda e, po=po, hh=hh, qb=qb: e.tensor_tensor(OT1[:, hh, qb * 512:(qb + 1) * 512], psb[po][:, :], rden1[:], ALU.mult),
                             r=[("ps", po), "rden1"], w=[("OT1", hh, qb)])
            S.barrier()
            dbg_dump("OT1", OT1[:], [128, 8, 2048], BF16, [])
            for R in (R1, R2, R4):
                R.reset()
            cwo_b = R2.alloc("cwo_b", [128, 8, D], BF16)
            wst5 = [R2.alloc("wst5", [128, D], F32) for _ in range(2)]
            g1r = R2.alloc("g1r", [128, D], F32)
            tmy = [R2.alloc("tmy", [128, 512], F32) for _ in range(2)]
            hb1 = [R1.alloc("hb1", [128, D], F32) for _ in range(3)]
            for k in range(8):
                S.dma(lambda e, k=k: e.dma_start(out=wst5[k % 2][:], in_=c_w_o[k * 128:(k + 1) * 128, :]), w=[("wst5", k % 2)])
                S.op("pool", lambda e, k=k: e.tensor_copy(cwo_b[:, k, :], wst5[k % 2][:]), r=[("wst5", k % 2)], w=["cwo_b"])
            load_mod(g1r, 0, 6 + 2, "g1r")
            yc = 0
            for j in range(16):
                b3 = j % 3
                S.dma(lambda e, j=j, b3=b3: e.dma_start(out=hb1[b3][:], in_=h_scr[j * 128:(j + 1) * 128, :]), w=[("hb1", b3)])
                for hf in range(2):
                    py = PS()
                    cs = slice(hf * 512, (hf + 1) * 512)
                    for hh in range(8):
                        S.op("pe", lambda e, py=py, hh=hh, cs=cs, j=j: e.matmul(psb[py][:, :], OT1[:, hh, j * 128:(j + 1) * 128], cwo_b[:, hh, cs], start=(hh == 0), stop=(hh == 7)),
                             r=["cwo_b"], w=[("ps", py)])
                    ti = yc % 2; yc += 1
                    S.op("dve", lambda e, py=py, ti=ti, cs=cs: e.tensor_tensor(tmy[ti][:], psb[py][:, :], g1r[:, cs], ALU.mult), r=[("ps", py), "g1r"], w=[("tmy", ti)])
                    S.op("pool", lambda e, ti=ti, b3=b3, cs=cs: e.tensor_tensor(hb1[b3][:, cs], hb1[b3][:, cs], tmy[ti][:], ALU.add), r=[("tmy", ti), ("hb1", b3)], w=[("hb1", b3)])
                S.dma(lambda e, j=j, b3=b3: e.dma_start(out=h_scr[j * 128:(j + 1) * 128, :], in_=hb1[b3][:]), r=[("hb1", b3)], w=["h_scr"])
            S.barrier()
            if "hmix1" in dbg:
                o = dout("dbg_hmix1", [2048, D])
                S.dma(lambda e, o=o: e.dma_start(out=o, in_=h_scr[0:2048, :]), w=["dbg_hmix1"])
                outs.append("dbg_hmix1")

        if stage >= 7:
            moe(1, False)
            for R in (R1, R2):
                R.reset()
            out_d = dout("out", [2048, D])
            hf1 = [R1.alloc("hf1", [128, D], F32) for _ in range(3)]
            of1 = [R1.alloc("of1", [128, D], F32) for _ in range(2)]
            junkf = R1.alloc("junkf", [128, D], BF16)
            fnr = R2.alloc("fnr", [128, D], F32)
            ssf = R2.alloc("ssf", [128, 8], F32)
            load_rep(fnr, final_norm, "fnr")
            odmas = []
            for j in range(16):
                b3 = j % 3; b2 = j % 2
                S.dma(lambda e, j=j, b3=b3: e.dma_start(out=hf1[b3][:], in_=h_scr[j * 128:(j + 1) * 128, :]), w=[("hf1", b3)])
                S.op("act", lambda e, b3=b3, b2=b2: e.activation(junkf[:], hf1[b3][:], AF.Square, accum_out=ssf[:, b2:b2 + 1]), r=[("hf1", b3)], w=["junkf", ("ssf", b2)])
                S.op("act", lambda e, b2=b2: e.activation(ssf[:, 2 + b2:3 + b2], ssf[:, b2:b2 + 1], AF.Sqrt, bias=EPS, scale=1.0 / D), r=[("ssf", b2)], w=[("sdf", b2)])
                S.op("dve", lambda e, b2=b2: e.reciprocal(ssf[:, 2 + b2:3 + b2], ssf[:, 2 + b2:3 + b2]), r=[("sdf", b2)], w=[("sdf", b2)])
                S.op("dve", lambda e, b3=b3, b2=b2: e.scalar_tensor_tensor(of1[b2][:], hf1[b3][:], ssf[:, 2 + b2:3 + b2], fnr[:], ALU.mult, ALU.mult),
                     r=[("hf1", b3), ("sdf", b2), "fnr"], w=[("of1", b2)])
                odmas.append(S.dma(lambda e, j=j, b2=b2: e.dma_start(out=out_d[j * 128:(j + 1) * 128, :], in_=of1[b2][:]), r=[("of1", b2)], w=["out"]))
            S.op("sp", None, after=odmas)
            outs.append("out")

        S.op("sp", None, r=list(outs))
        S.barrier()
        S.emit()
    return nc, outs
_DIN_NAMES = ['x_own', 'ctx_b', 'cT', 'ada_s', 'ada_bs', 'w_in', 'w_uq', 'w_uqsw', 'w_uk', 'w_uv', 'ab_w_o', 'q_norm', 'kv_norm', 'norm1', 'norm2', 'C32o', 'S32o', 'ident_f', 'C128b', 'S128b', 'tw', 'dft64o', 'C256b', 'NS256b', 'moe_g', 'moe_u', 'moe_d', 'esel', 'ltq', 'w_router', 'rowid', 'iota_row', 'ustrict', 'tgt', 'cbe', 'c_w_in', 'c_w_o', 'c_q_gain', 'c_k_gain', 'final_norm', 'cos1o', 'sin1o']


_INPUT_NAMES = None


def kernel(**inputs):
    per_core = host_prep(inputs)
    nc, outs = build(stage=99, dbg=())
    names = [a for a in per_core[0].keys()]
    import re as _re
    in_maps = [{n: m[n] for n in _DIN_NAMES} for m in per_core]
    res = run_bass_kernel_spmd(nc, in_maps, core_ids=list(range(8)))
    out = np.empty((2, S_LAT, D), np.float32)
    for r in range(8):
        b, q = r // 4, r % 4
        out[b, 2048 * q:2048 * (q + 1)] = res.results[r]["out"]
    return out
```

```python
import contextlib
import math
import numpy as np
import ml_dtypes
import concourse.bass as bass
import concourse.mybir as mybir
from concourse.bass_utils import run_bass_kernel_spmd

F32 = mybir.dt.float32
BF16 = mybir.dt.bfloat16
I32 = mybir.dt.int32
ALU = mybir.AluOpType
AF = mybir.ActivationFunctionType
AX = mybir.AxisListType

D = 1024
S_LAT = 8192
L_CTX = 256
EPS = 1e-6
NT = 66
NQ = 18
NKEY = NT * 128
NQRY = NQ * 128
GROUPS = [[0, 1, 2, 3], [4, 5, 6, 7]]
DEBUG = {}


class Sched:
    ENGS = ("sp", "act", "dve", "pool", "pe")

    def __init__(self, nc, n_streams=20):
        self.nc = nc
        self.ops = []
        self.last_w = {}
        self.readers = {}
        self.n_streams = n_streams
        self.stream_last = {}
        self.rr = 0
        self.pool_regs = {}
        self.pool_reg_vals = [8191, 12 * 4 * 352 - 1]

    def op(self, eng, fn, r=(), w=(), ndma=0, stream=None, inc=16, after=()):
        i = len(self.ops)
        deps = {d: True for d in after}
        w = list(w) + [k for k in r if isinstance(k, tuple) and k[0] == "ps" and k not in w]
        for k in r:
            d = self.last_w.get(k)
            if d is not None:
                deps[d] = True
        for k in w:
            d = self.last_w.get(k)
            if d is not None:
                deps.setdefault(d, False)
            for d in self.readers.get(k, ()):
                deps.setdefault(d, False)
        if ndma:
            if stream is None:
                stream = self.rr % self.n_streams
                self.rr += 1
            p = self.stream_last.get(stream)
            if p is not None:
                deps[p] = True
            self.stream_last[stream] = i
        for k in w:
            self.last_w[k] = i
            self.readers[k] = []
        for k in r:
            lst = self.readers.setdefault(k, [])
            if not ndma:
                lst[:] = [j for j in lst if self.ops[j]["ndma"] or self.ops[j]["eng"] != eng]
            lst.append(i)
        fdeps = []
        for d, raw in deps.items():
            o = self.ops[d]
            if o["fn"] is None:
                continue
            if ndma or o["ndma"]:
                fdeps.append(d)
            elif o["eng"] == eng:
                if (raw and eng != "pe") or eng == "pool":
                    fdeps.append(d)
            else:
                fdeps.append(d)
        self.ops.append(dict(eng=eng, fn=fn, deps=fdeps, ndma=ndma, stream=stream, sig=None, inc=inc))
        return i

    def dma(self, fn, r=(), w=(), n=1, eng="sp", stream=None, after=()):
        return self.op(eng, fn, r, w, ndma=n, stream=stream, after=after)

    def cc(self, fn, r=(), w=(), after=()):
        return self.op("pool", fn, r, w, ndma=1, stream="cc", inc=1, after=after)

    def barrier(self):
        last = {}
        for i, o in enumerate(self.ops):
            if o["fn"] is None:
                continue
            if o["ndma"]:
                last[("d", o["stream"])] = i
            else:
                last[o["eng"]] = i
        deps = list(last.values())
        for e in self.ENGS:
            self.ops.append(dict(eng=e, fn=None, deps=list(deps), ndma=0, stream=None, sig=None, inc=0))
        self.last_w = {}
        self.readers = {}

    def emit(self):
        nc = self.nc
        ops = self.ops
        need = [False] * len(ops)
        for o in ops:
            for d in o["deps"]:
                need[d] = True
        with contextlib.ExitStack() as st:
            esem = {e: st.enter_context(nc.semaphore("s_" + e)) for e in self.ENGS}
            ssem = {}
            cnt = {}
            for i, o in enumerate(ops):
                if o["ndma"]:
                    s = o["stream"]
                    if s not in ssem:
                        ssem[s] = st.enter_context(nc.semaphore("d_%s" % (s,)))
                    cnt[("d", s)] = cnt.get(("d", s), 0) + o["inc"] * o["ndma"]
                    o["sig"] = (ssem[s], cnt[("d", s)], ("d", s))
                elif need[i]:
                    e = o["eng"]
                    cnt[e] = cnt.get(e, 0) + 1
                    o["sig"] = (esem[e], cnt[e], e)
            block = st.enter_context(nc.Block())

            def run(engname, eng):
                known = {}
                if engname == "pool":
                    for v in self.pool_reg_vals:
                        self.pool_regs[v] = eng.to_reg(v)
                for o in ops:
                    if o["eng"] != engname:
                        continue
                    for d in o["deps"]:
                        sem, val, key = ops[d]["sig"]
                        if known.get(key, 0) < val:
                            eng.wait_ge(sem, val)
                            known[key] = val
                    if o["fn"] is None:
                        continue
                    inst = o["fn"](eng)
                    if o["sig"] is not None:
                        sem, val, key = o["sig"]
                        if o["ndma"]:
                            insts = inst if isinstance(inst, (list, tuple)) else [inst]
                            assert len(insts) == o["ndma"], (len(insts), o["ndma"])
                            for ins in insts:
                                ins.then_inc(sem, o["inc"])
                        else:
                            inst.then_inc(sem, 1)

            used = set(o["eng"] for o in ops)
            if "sp" in used:
                @block.sync
                def _(e):
                    run("sp", e)
            if "act" in used:
                @block.scalar
                def _(e):
                    run("act", e)
            if "dve" in used:
                @block.vector
                def _(e):
                    run("dve", e)
            if "pool" in used:
                @block.gpsimd
                def _(e):
                    run("pool", e)
            if "pe" in used:
                @block.tensor
                def _(e):
                    run("pe", e)


class Region:
    def __init__(self, nc, base, size, tag):
        self.nc, self.base, self.size, self.tag = nc, base, size, tag
        self.ptr = 0
        self.n = 0

    def alloc(self, name, shape, dt):
        esz = 2 if dt == BF16 else 4
        nb = esz
        for s in shape[1:]:
            nb *= s
        nb = (nb + 63) // 64 * 64
        off = self.ptr
        self.ptr += nb
        assert self.ptr <= self.size, ("region overflow", self.tag, name, self.ptr, self.size)
        self.n += 1
        return self.nc.alloc_sbuf_tensor_at("%s_%s%d" % (name, self.tag, self.n), list(shape), dt,
                                            offset=self.base + off)

    def reset(self):
        self.ptr = 0


def _rope_tables_l0():
    t = np.arange(S_LAT)
    row = (t // 64).astype(np.float64)
    col = (t % 64).astype(np.float64)
    inv = 10000.0 ** (-np.arange(8, dtype=np.float64) / 8.0)
    C = np.zeros((32, S_LAT)); Sg = np.zeros((32, S_LAT))
    for d in range(32):
        pos = row if d < 16 else col
        j = d % 8
        ang = pos * inv[j]
        C[d] = np.cos(ang)
        Sg[d] = (-1.0 if (d % 16) < 8 else 1.0) * np.sin(ang)
    return C.astype(np.float32), Sg.astype(np.float32)


SW16 = np.array([d + 8 if (d % 16) < 8 else d - 8 for d in range(32)])


def host_prep(inp):
    f32 = np.float32
    x = np.asarray(inp["x"], f32); ctx = np.asarray(inp["ctx"], f32)
    c = np.asarray(inp["c"], f32); c_ctx = np.asarray(inp["c_ctx"], f32)
    ada_w = np.asarray(inp["ada_w"], f32); ada_b = np.asarray(inp["ada_b"], f32)
    shared = {}
    w_in = np.asarray(inp["ab_w_in"], f32)[0]
    f_c, cq_c, ckv_c, kr_c = w_in[:, 0:512], w_in[:, 512:768], w_in[:, 768:896], w_in[:, 896:928]
    z64 = np.zeros((1024, 64), f32)
    shared["w_in"] = np.ascontiguousarray(np.concatenate([f_c, ckv_c, z64, kr_c, z64, kr_c[:, SW16], cq_c], 1))
    w_uq = np.asarray(inp["ab_w_uq"], f32)[0].reshape(256, 8, 96)
    shared["w_uq"] = np.ascontiguousarray(w_uq.reshape(256, 768))
    wqs = np.concatenate([np.zeros((256, 8, 64), f32), w_uq[:, :, 64:96][:, :, SW16]], 2)
    shared["w_uqsw"] = np.ascontiguousarray(wqs.reshape(256, 768))
    w_ukv = np.asarray(inp["ab_w_ukv"], f32)[0].reshape(128, 8, 128)
    shared["w_uk"] = np.ascontiguousarray(w_ukv[:, :, 0:64].reshape(128, 512))
    shared["w_uv"] = np.ascontiguousarray(w_ukv[:, :, 64:128].reshape(128, 512))
    shared["ab_w_o"] = np.asarray(inp["ab_w_o"], f32)[0]
    shared["q_norm"] = np.ascontiguousarray(np.asarray(inp["ab_q_norm"], f32)[0].reshape(2, 128).T)
    shared["kv_norm"] = np.ascontiguousarray(np.asarray(inp["ab_kv_norm"], f32)[0].reshape(128, 1))
    shared["norm1"] = np.asarray(inp["norm1"], f32)
    shared["norm2"] = np.asarray(inp["norm2"], f32)
    C32, S32 = _rope_tables_l0()
    shared["ident_f"] = np.eye(128, dtype=f32)
    bf = ml_dtypes.bfloat16
    i128 = np.arange(128, dtype=np.float64)
    a128 = 2 * np.pi * np.outer(i128, i128) / 128.0
    shared["C128b"] = np.cos(a128).astype(bf); shared["S128b"] = np.sin(a128).astype(bf)
    atw = 2 * np.pi * np.outer(i128, np.arange(64, dtype=np.float64)) / 8192.0
    shared["tw"] = np.ascontiguousarray(np.stack([np.cos(atw), np.sin(atw), -np.cos(atw)], 1).astype(f32))
    t256 = np.arange(256, dtype=np.float64)
    a256 = 2 * np.pi * np.outer(t256, t256) / 256.0
    shared["C256b"] = np.ascontiguousarray(np.cos(a256).reshape(2, 128, 256).transpose(1, 0, 2)).astype(bf)
    shared["NS256b"] = np.ascontiguousarray((-np.sin(a256)).reshape(2, 128, 256).transpose(1, 0, 2)).astype(bf)
    wg_all = np.asarray(inp["moe_w_gate"], f32); wu_all = np.asarray(inp["moe_w_up"], f32)
    wd_all = np.asarray(inp["moe_w_down"], f32)
    shared["w_router"] = np.asarray(inp["moe_router"], f32)
    pp = np.arange(128)[:, None]; tt = np.arange(64)[None, :]
    rowid = ((tt % 16) // 4) * 2048 + (tt // 16) * 512 + 128 * (tt % 4) + pp
    shared["rowid"] = np.ascontiguousarray(np.stack([rowid % 64, rowid // 64], -1)).astype(bf)
    shared["iota_row"] = np.tile(np.arange(1024, dtype=f32)[None, :], (128, 1))
    shared["ustrict"] = np.triu(np.ones((128, 128), f32), 1).astype(bf)
    tg = np.zeros((128, 2, 16), f32); tg[:, 0, :] = 1024.0; tg[:, 1, :] = 32.0
    shared["tgt"] = tg
    e16 = np.arange(16)
    cb = (3 * (e16 % 4)) * 1408 + (e16 // 4) * 352
    shared["cbe"] = np.tile(cb.astype(f32)[None, :], (128, 1))
    shared["c_w_in"] = np.asarray(inp["c_w_in"], f32)[0]
    shared["c_w_o"] = np.asarray(inp["c_w_o"], f32)[0]
    shared["c_q_gain"] = np.asarray(inp["c_q_gain"], f32)[0]
    shared["c_k_gain"] = np.asarray(inp["c_k_gain"], f32)[0]
    shared["final_norm"] = np.asarray(inp["final_norm"], f32)
    tt_ = np.arange(S_LAT); row_ = (tt_ // 64).astype(np.float64); col_ = (tt_ % 64).astype(np.float64)
    inv32 = 10000.0 ** (-np.arange(32, dtype=np.float64) / 32.0)
    ar = row_[:, None] * inv32[None, :]; ac = col_[:, None] * inv32[None, :]
    cos1 = np.concatenate([np.cos(ar), np.cos(ar), np.cos(ac), np.cos(ac)], 1).astype(f32)
    sin1 = np.concatenate([-np.sin(ar), np.sin(ar), -np.sin(ac), np.sin(ac)], 1).astype(f32)
    per_core = []
    for r in range(8):
        b, q = r // 4, r % 4
        m = dict(shared)
        m["x_own"] = np.ascontiguousarray(x[b, 2048 * q:2048 * (q + 1)])
        m["ctx_b"] = ctx[b]
        cT = np.stack([c[b], c_ctx], 0)
        m["cT"] = np.ascontiguousarray(cT.reshape(2, 8, 128).transpose(2, 1, 0))
        sh = np.stack([ada_w[v // 6][:, (v % 6) * 1024 + 256 * q:(v % 6) * 1024 + 256 * q + 256] for v in range(12)], 1)
        m["ada_s"] = np.ascontiguousarray(sh.reshape(1024, 3072))
        m["ada_bs"] = np.ascontiguousarray(
            np.stack([ada_b[v // 6][(v % 6) * 1024 + 256 * q:(v % 6) * 1024 + 256 * q + 256] for v in range(12)], 0).reshape(3072))
        a64 = 2 * np.pi * np.outer(np.arange(64, dtype=np.float64), np.arange(16 * q, 16 * q + 16, dtype=np.float64)) / 64.0
        m["dft64o"] = np.ascontiguousarray(np.stack([np.concatenate([np.cos(a64), -np.sin(a64)], 1),
                                                       np.concatenate([np.sin(a64), np.cos(a64)], 1)], 1)).astype(bf)
        m["moe_g"] = np.ascontiguousarray(wg_all[:, 4 * q:4 * q + 4])
        m["moe_u"] = np.ascontiguousarray(wu_all[:, 4 * q:4 * q + 4])
        m["moe_d"] = np.ascontiguousarray(wd_all[:, 4 * q:4 * q + 4])
        es = np.zeros((128, 4, 16), f32)
        for j in range(4):
            es[:, j, 4 * q + j] = 1.0
        m["esel"] = es
        lt = np.zeros((128, 4), f32); lt[:, :q] = 1.0
        m["ltq"] = lt
        m["cos1o"] = np.ascontiguousarray(cos1[2048 * q:2048 * (q + 1)].reshape(16, 128, 128).transpose(1, 0, 2))
        m["sin1o"] = np.ascontiguousarray(sin1[2048 * q:2048 * (q + 1)].reshape(16, 128, 128).transpose(1, 0, 2))
        m["C32o"] = np.ascontiguousarray(C32[:, 2048 * q:2048 * (q + 1)])
        m["S32o"] = np.ascontiguousarray(S32[:, 2048 * q:2048 * (q + 1)])
        per_core.append(m)
    return per_core


def build(stage=99, dbg=()):
    nc = bass.Bass("TRN2", target_bir_lowering=False)
    S = Sched(nc)
    dram = {}

    def din(name, shape, dt=F32):
        dram[name] = nc.dram_tensor(name, list(shape), dt, kind="ExternalInput").ap()
        return dram[name]

    def dout(name, shape, dt=F32):
        dram[name] = nc.dram_tensor(name, list(shape), dt, kind="ExternalOutput").ap()
        return dram[name]

    def dscr(name, shape, dt=F32):
        dram[name] = nc.dram_tensor(name, list(shape), dt, kind="Internal").ap()
        return dram[name]

    x_own = din("x_own", [2048, D]); ctx_b = din("ctx_b", [L_CTX, D])
    cT = din("cT", [128, 8, 2]); ada_s = din("ada_s", [D, 3072]); ada_bs = din("ada_bs", [3072])
    w_in = din("w_in", [D, 1088]); w_uq = din("w_uq", [256, 768]); w_uqsw = din("w_uqsw", [256, 768])
    w_uk = din("w_uk", [128, 512]); w_uv = din("w_uv", [128, 512]); ab_w_o = din("ab_w_o", [D, D])
    q_norm = din("q_norm", [128, 2]); kv_norm = din("kv_norm", [128, 1])
    norm1 = din("norm1", [2, D]); norm2 = din("norm2", [2, D])
    C32o = din("C32o", [32, 2048]); S32o = din("S32o", [32, 2048])
    ident_f_d = din("ident_f", [128, 128])
    C128b_d = din("C128b", [128, 128], BF16); S128b_d = din("S128b", [128, 128], BF16)
    tw_d = din("tw", [128, 3, 64]); dft64o_d = din("dft64o", [64, 2, 32], BF16)
    C256b_d = din("C256b", [128, 2, 256], BF16); NS256b_d = din("NS256b", [128, 2, 256], BF16)
    hp_scr = dscr("hp_scr", [128, 64, 2, 512], BF16)
    moe_g = din("moe_g", [2, 4, D, 2048]); moe_u = din("moe_u", [2, 4, D, 2048]); moe_d = din("moe_d", [2, 4, 2048, D])
    esel_d = din("esel", [128, 4, 16]); ltq_d = din("ltq", [128, 4]); w_router = din("w_router", [2, D, 16])
    rowid_d = din("rowid", [128, 64, 2], BF16); iota_d = din("iota_row", [128, 1024]); ustrict_d = din("ustrict", [128, 128], BF16)
    tgt_d = din("tgt", [128, 2, 16]); cbe_d = din("cbe", [128, 16])
    h_scr = dscr("h_scr", [NQRY, D])
    c_w_in = din("c_w_in", [D, 1536]); c_w_o = din("c_w_o", [D, D]); c_q_gain = din("c_q_gain", [128]); c_k_gain = din("c_k_gain", [128])
    final_norm = din("final_norm", [D]); cos1o = din("cos1o", [128, 16, 128]); sin1o = din("sin1o", [128, 16, 128])
    kT_loc = dscr("kT_loc", [128, 2, 2048], BF16); kT_all = dscr("kT_all", [4, 128, 2, 2048], BF16)
    v_loc = dscr("v_loc", [2048, 256], BF16); v_all = dscr("v_all", [8192, 256], BF16)
    m_loc = dscr("m_loc", [2048, D], BF16); m_all = dscr("m_all", [4, 4, 512, D], BF16)
    aff_loc = dscr("aff_loc", [128, 16, 16]); aff_all = dscr("aff_all", [4, 128, 16, 16])
    y_loc = dscr("y_loc", [12, 352, D], BF16); y_all = dscr("y_all", [12, 4, 352, D], BF16)

    mods_loc = dscr("mods_loc", [2, 3072]); mods_all = dscr("mods_all", [4, 2, 3072])
    f_scr = dscr("f_scr", [NKEY, 512], BF16)
    ckv_loc = dscr("ckv_loc", [128, 2048], BF16); ckv_all = dscr("ckv_all", [4, 128, 2048], BF16)
    kr_loc = dscr("kr_loc", [32, 2048], BF16); kr_all = dscr("kr_all", [4, 32, 2048], BF16)
    f_loc = dscr("f_loc", [2, 1024, 512], BF16); f_all = dscr("f_all", [2, 4, 1024, 512], BF16)

    outs = []

    with contextlib.ExitStack() as st:
        total = (nc.sbuf_bytes_remaining // 64) * 64 - 128
        arena = st.enter_context(nc.sbuf_tensor("arena", [128, total // 4], F32))
        abase = nc.lookup_mloc(arena).addr
        assert abase % 32 == 0
        SZ_P = 16 * 1024
        SZ_R1 = 64 * 1024
        SZ_R2 = 71 * 1024
        SZ_R3 = 36 * 1024
        RP = Region(nc, abase, SZ_P, "P")
        R1 = Region(nc, abase + SZ_P, SZ_R1, "A")
        R2 = Region(nc, abase + SZ_P + SZ_R1, SZ_R2, "B")
        R3 = Region(nc, abase + SZ_P + SZ_R1 + SZ_R2, SZ_R3, "C")
        r4base = SZ_P + SZ_R1 + SZ_R2 + SZ_R3
        R4 = Region(nc, abase + r4base, total - r4base, "D")
        psw = [st.enter_context(nc.psum_tensor("psw%d" % i, [128, 1024], F32)) for i in range(4)]
        psb = [psw[i // 2][:, (i % 2) * 512:(i % 2 + 1) * 512] for i in range(8)]
        psn = [0]
        ps_rng = [0, 8]

        def PS():
            i = ps_rng[0] + psn[0] % (ps_rng[1] - ps_rng[0])
            psn[0] += 1
            return i

        def dbg_dump(name, src_ap, shape, dt, rkeys):
            if name in dbg:
                o = dout("dbg_" + name, shape, dt)
                S.dma(lambda e, o=o: e.dma_start(out=o, in_=src_ap), r=rkeys, w=["dbg_" + name])
                outs.append("dbg_" + name)

        ident_f = RP.alloc("ident_f", [128, 128], F32)
        ident_b = RP.alloc("ident_b", [128, 128], BF16)
        ones_f = RP.alloc("ones_f", [128, 128], F32)
        ones_b = RP.alloc("ones_b", [128, 128], BF16)
        h_ctx = RP.alloc("h_ctx", [128, 2, D], F32)
        S.dma(lambda e: e.dma_start(out=ident_f[:], in_=ident_f_d), w=["ident_f"])
        C128b = RP.alloc("C128b", [128, 128], BF16); S128b = RP.alloc("S128b", [128, 128], BF16)
        C256b = RP.alloc("C256b", [128, 2, 256], BF16); NS256b = RP.alloc("NS256b", [128, 2, 256], BF16)
        S.dma(lambda e: [e.dma_start(out=C128b[:], in_=C128b_d), e.dma_start(out=S128b[:], in_=S128b_d),
                         e.dma_start(out=C256b[:], in_=C256b_d), e.dma_start(out=NS256b[:], in_=NS256b_d)], w=["dfttab"], n=4)
        S.op("dve", lambda e: e.tensor_copy(ident_b[:], ident_f[:]), r=["ident_f"], w=["ident_b"])
        S.op("pool", lambda e: e.memset(ones_f[:], 1.0), w=["ones_f"])
        S.op("pool", lambda e: e.memset(ones_b[:], 1.0), w=["ones_b"])

        cT_sb = R3.alloc("cT", [128, 8, 2], F32)
        silc = R3.alloc("silc", [128, 8, 2], F32)
        adab2 = R3.alloc("adab2", [2, 3072], F32)
        modl = R3.alloc("modl", [2, 3072], F32)
        adas = R2.alloc("adas", [128, 8, 1536], F32)
        S.dma(lambda e: e.dma_start(out=cT_sb[:], in_=cT), w=["cT"])
        S.dma(lambda e: e.dma_start(out=adab2[:], in_=ada_bs.partition_broadcast(2)), w=["adab2"])
        S.op("act", lambda e: e.activation(silc[:], cT_sb[:], AF.Silu), r=["cT"], w=["silc"])
        for hf in range(2):
            S.dma(lambda e, hf=hf: e.dma_start(
                out=adas[:], in_=ada_s[:, hf * 1536:(hf + 1) * 1536].rearrange("(k p) c -> p k c", p=128)), w=["adas"])
            for cb in range(3):
                pi = PS()
                c0 = hf * 1536 + cb * 512
                for kt in range(8):
                    S.op("pe", lambda e, pi=pi, kt=kt, cb=cb: e.matmul(
                        psb[pi][0:2, :], silc[:, kt, :], adas[:, kt, cb * 512:(cb + 1) * 512],
                        start=(kt == 0), stop=(kt == 7)), r=["silc", "adas"], w=[("ps", pi)])
                S.op("dve", lambda e, pi=pi, c0=c0: e.tensor_tensor(
                    modl[:, c0:c0 + 512], psb[pi][0:2, :], adab2[:, c0:c0 + 512], ALU.add),
                    r=[("ps", pi), "adab2"], w=["modl"])
        S.dma(lambda e: e.dma_start(out=mods_loc, in_=modl[:]), r=["modl"], w=["mods_loc"])
        S.cc(lambda e: e.collective_compute("AllGather", ALU.bypass, replica_groups=GROUPS,
                                            ins=[mods_loc], outs=[mods_all.rearrange("r v c -> (r v) c")]),
             r=["mods_loc"], w=["mods_all"])

        def load_mod(dst, v, vi, key):
            src = mods_all[:, v, vi * 256:(vi + 1) * 256].partition_broadcast(128)
            S.dma(lambda e: e.dma_start(out=dst[:].rearrange("p (a b) -> p a b", a=4), in_=src),
                  r=["mods_all"], w=[key])

        def load_rep(dst, row_ap, key):
            S.dma(lambda e: e.dma_start(out=dst[:], in_=row_ap.partition_broadcast(128)), w=[key])

        if "mods" in dbg:
            o = dout("dbg_mods", [8, 3072])
            tmpm = R4.alloc("tmpm", [8, 3072], F32)
            S.dma(lambda e, tmpm=tmpm: e.dma_start(out=tmpm[:], in_=mods_all.rearrange("r v c -> (r v) c")), r=["mods_all"], w=["tmpm"])
            S.dma(lambda e, o=o, tmpm=tmpm: e.dma_start(out=o, in_=tmpm[:]), r=["tmpm"], w=["dbg_mods"])
            outs.append("dbg_mods")

        S.barrier()
        for R in (R1, R2, R3, R4):
            R.reset()

        if stage >= 1 and "skip12" not in dbg:
            ckvnT = R2.alloc("ckvnT", [128, NKEY], BF16)
            krT = R2.alloc("krT", [96, NKEY], BF16)
            QT = R2.alloc("QT", [96, 8, NQRY], BF16)
            xt = [R1.alloc("xt", [128, D], F32) for _ in range(3)]
            t1 = [R1.alloc("t1", [128, D], F32) for _ in range(2)]
            abf = [R1.alloc("abf", [128, D], BF16) for _ in range(2)]
            junk = R1.alloc("junk", [128, D], BF16)
            aT = [R1.alloc("aT", [128, 8, 512], BF16) for _ in range(2)]
            fb = [R1.alloc("fb", [128, 512], BF16) for _ in range(2)]
            sqc = R1.alloc("sqc", [128, 512], F32)
            rr = R1.alloc("rr", [128, 512], F32)
            sq01 = R1.alloc("sq01", [128, 2, 512], F32)
            rq = R1.alloc("rq", [128, 512], F32)
            cqg = R1.alloc("cqg", [128, 2, 512], BF16)
            ckvO = R1.alloc("ckvO", [128, 2048], BF16)
            krO = R1.alloc("krO", [96, 2048], BF16)
            win_b = R3.alloc("win_b", [128, 8, 1088], BF16)
            wuq_b = R3.alloc("wuq_b", [128, 2, 768], BF16)
            wuqsw_b = R3.alloc("wuqsw_b", [128, 2, 768], BF16)
            gs_rep = R3.alloc("gs_rep", [128, D], F32)
            sh_rep = R3.alloc("sh_rep", [128, D], F32)
            wst = [R4.alloc("wst", [128, 1088], F32) for _ in range(2)]
            Cc = R4.alloc("Cc", [96, 512], F32); Sc = R4.alloc("Sc", [96, 512], F32)
            k1 = R4.alloc("k1", [96, 512], F32); k2 = R4.alloc("k2", [96, 512], F32); u3 = R4.alloc("u3", [96, 512], F32)
            u1, u2 = k1, k2
            ssb = R4.alloc("ssb", [128, 8], F32)
            gq = R4.alloc("gq", [128, 2], F32); gkv = R4.alloc("gkv", [128, 1], F32)

            for k in range(8):
                S.dma(lambda e, k=k: e.dma_start(out=wst[k % 2][:], in_=w_in[k * 128:(k + 1) * 128, :]), w=[("wst", k % 2)])
                S.op("pool", lambda e, k=k: e.tensor_copy(win_b[:, k, :], wst[k % 2][:]), r=[("wst", k % 2)], w=[("win", k)])
            for j in range(2):
                S.dma(lambda e, j=j: e.dma_start(out=wst[j][:, 0:768], in_=w_uq[j * 128:(j + 1) * 128, :]), w=[("wst", j)])
                S.op("pool", lambda e, j=j: e.tensor_copy(wuq_b[:, j, :], wst[j][:, 0:768]), r=[("wst", j)], w=[("wuq", j)])
            for j in range(2):
                S.dma(lambda e, j=j: e.dma_start(out=wst[j][:, 0:768], in_=w_uqsw[j * 128:(j + 1) * 128, :]), w=[("wst", j)])
                S.op("pool", lambda e, j=j: e.tensor_copy(wuqsw_b[:, j, :], wst[j][:, 0:768]), r=[("wst", j)], w=[("wuqsw", j)])
            WIN = [("win", k) for k in range(8)]
            S.dma(lambda e: e.dma_start(out=gq[:], in_=q_norm), w=["gq"])
            S.dma(lambda e: e.dma_start(out=gkv[:], in_=kv_norm), w=["gkv"])

            cnt = {"t": 0}

            def set_mods(v):
                load_mod(sh_rep, v, 0, "sh_rep")
                load_mod(t1[0], v, 1, ("t1", 0))
                load_rep(t1[1], norm1[0, :], ("t1", 1))
                S.op("dve", lambda e: e.scalar_tensor_tensor(gs_rep[:], t1[0][:], 1.0, t1[1][:], ALU.add, ALU.mult),
                     r=[("t1", 0), ("t1", 1)], w=["gs_rep"])

            def norm_mod_T(src_rows, cb, slot):
                i = cnt["t"]; cnt["t"] += 1
                b3, b2 = i % 3, i % 2
                S.dma(lambda e: e.dma_start(out=xt[b3][:], in_=src_rows), w=[("xt", b3)])
                S.op("act", lambda e: e.activation(junk[:], xt[b3][:], AF.Square, accum_out=ssb[:, b3:b3 + 1]),
                     r=[("xt", b3)], w=["junk", ("ss", b3)])
                S.op("act", lambda e: e.activation(ssb[:, 3 + b3:4 + b3], ssb[:, b3:b3 + 1], AF.Sqrt, bias=EPS, scale=1.0 / D),
                     r=[("ss", b3)], w=[("sd", b3)])
                S.op("dve", lambda e: e.reciprocal(ssb[:, 3 + b3:4 + b3], ssb[:, 3 + b3:4 + b3]), r=[("sd", b3)], w=[("sd", b3)])
                S.op("dve", lambda e: e.scalar_tensor_tensor(t1[b2][:], xt[b3][:], ssb[:, 3 + b3:4 + b3], gs_rep[:],
                                                             ALU.mult, ALU.mult),
                     r=[("xt", b3), ("sd", b3), "gs_rep"], w=[("t1", b2)])
                S.op("pool", lambda e: e.tensor_tensor(abf[b2][:], t1[b2][:], sh_rep[:], ALU.add),
                     r=[("t1", b2), "sh_rep"], w=[("abf", b2)])

                def stageB():
                    pi = PS()
                    psT = psb[pi][:].bitcast(BF16).rearrange("p (k c) -> p k c", k=8)
                    for k in range(8):
                        S.op("pe", lambda e, k=k: e.transpose(psT[:, k, :], abf[b2][:, k * 128:(k + 1) * 128], ident_b[:]),
                             r=[("abf", b2), "ident_b"], w=[("ps", pi)])
                    dst = aT[cb][:, :, slot * 128:(slot + 1) * 128]
                    if i % 2 == 0:
                        S.op("act", lambda e: e.copy(dst, psT), r=[("ps", pi)], w=[("aT", cb, slot)])
                    else:
                        S.op("dve", lambda e: e.tensor_copy(dst, psT), r=[("ps", pi)], w=[("aT", cb, slot)])
                return stageB

            def kv_side(cb, nslot, key0, is_ctx):
                N = nslot * 128
                AT = [("aT", cb, s) for s in range(nslot)]
                if is_ctx:
                    ckv_dst = ckvnT[:, key0:key0 + N]
                    kr_dst = krT[64:96, key0:key0 + N]
                    f_rows = lambda s_: f_scr[key0 + s_ * 128:key0 + (s_ + 1) * 128, :]
                else:
                    ckv_dst = ckvO[:, key0:key0 + N]
                    kr_dst = krO[64:96, key0:key0 + N]
                    f_rows = lambda s_: f_loc.rearrange("c i d -> (c i) d")[key0 + s_ * 128:key0 + (s_ + 1) * 128, :]
                for s in range(nslot):
                    pi = PS()
                    for k in range(8):
                        S.op("pe", lambda e, pi=pi, k=k, s=s: e.matmul(
                            psb[pi][:, :], aT[cb][:, k, s * 128:(s + 1) * 128], win_b[:, k, 0:512],
                            start=(k == 0), stop=(k == 7)), r=[("aT", cb, s)] + WIN, w=[("ps", pi)])
                    fi = cnt.setdefault("f", 0) % 2; cnt["f"] += 1
                    S.op("dve", lambda e, pi=pi, fi=fi: e.tensor_copy(fb[fi][:], psb[pi][:, :]), r=[("ps", pi)], w=[("fb", fi)])
                    S.dma(lambda e, fi=fi, s=s: e.dma_start(out=f_rows(s), in_=fb[fi][:]),
                          r=[("fb", fi)], w=["f_scr"])
                pc, pk, pks = PS(), PS(), PS()
                for k in range(8):
                    S.op("pe", lambda e, k=k: e.matmul(psb[pc][:, 0:N], win_b[:, k, 512:640], aT[cb][:, k, 0:N],
                                                       start=(k == 0), stop=(k == 7)), r=AT + WIN, w=[("ps", pc)])
                for k in range(8):
                    S.op("pe", lambda e, k=k: e.matmul(psb[pk][0:96, 0:N], win_b[:, k, 640:736], aT[cb][:, k, 0:N],
                                                       start=(k == 0), stop=(k == 7)), r=AT + WIN, w=[("ps", pk)])
                if not is_ctx:
                    for k in range(8):
                        S.op("pe", lambda e, k=k: e.matmul(psb[pks][0:96, 0:N], win_b[:, k, 736:832], aT[cb][:, k, 0:N],
                                                           start=(k == 0), stop=(k == 7)), r=AT + WIN, w=[("ps", pks)])
                S.op("act", lambda e: e.activation(sqc[:, 0:N], psb[pc][:, 0:N], AF.Square), r=[("ps", pc)], w=["sqc"])
                pss = PS()
                S.op("pe", lambda e: e.matmul(psb[pss][:, 0:N], ones_f[:], sqc[:, 0:N], start=True, stop=True),
                     r=["ones_f", "sqc"], w=[("ps", pss)])
                S.op("act", lambda e: e.activation(rr[:, 0:N], psb[pss][:, 0:N], AF.Sqrt, bias=EPS, scale=1.0 / 128),
                     r=[("ps", pss)], w=["rr"])
                S.op("dve", lambda e: e.reciprocal(rr[:, 0:N], rr[:, 0:N]), r=["rr"], w=["rr"])
                S.op("dve", lambda e: e.scalar_tensor_tensor(ckv_dst, psb[pc][:, 0:N], gkv[:, 0:1], rr[:, 0:N],
                                                             ALU.mult, ALU.mult),
                     r=[("ps", pc), "gkv", "rr"], w=[("ckvnT", key0)])
                if is_ctx:
                    S.op("act", lambda e: e.copy(kr_dst, psb[pk][64:96, 0:N]), r=[("ps", pk)], w=[("krT", key0)])
                else:
                    S.dma(lambda e: [e.dma_start(out=Cc[64:96, 0:N], in_=C32o[:, key0:key0 + N]),
                                     e.dma_start(out=Sc[64:96, 0:N], in_=S32o[:, key0:key0 + N])], w=["CcSc"], n=2)
                    S.op("dve", lambda e: e.tensor_tensor(k1[64:96, 0:N], psb[pk][64:96, 0:N], Cc[64:96, 0:N], ALU.mult),
                         r=[("ps", pk), "CcSc"], w=["k1"])
                    S.op("dve", lambda e: e.tensor_tensor(k2[64:96, 0:N], psb[pks][64:96, 0:N], Sc[64:96, 0:N], ALU.mult),
                         r=[("ps", pks), "CcSc"], w=["k2"])
                    S.op("pool", lambda e: e.tensor_tensor(kr_dst, k1[64:96, 0:N], k2[64:96, 0:N], ALU.add),
                         r=["k1", "k2"], w=[("krT", key0)])

            def q_side(cb, nslot, qc0, is_ctx, tab0):
                N = nslot * 128
                AT = [("aT", cb, s) for s in range(nslot)]
                pcq = [PS(), PS()]
                for j in range(2):
                    for k in range(8):
                        S.op("pe", lambda e, j=j, k=k: e.matmul(
                            psb[pcq[j]][:, 0:N], win_b[:, k, 832 + j * 128:832 + (j + 1) * 128], aT[cb][:, k, 0:N],
                            start=(k == 0), stop=(k == 7)), r=AT + WIN, w=[("ps", pcq[j])])
                    S.op("act", lambda e, j=j: e.activation(sq01[:, j, 0:N], psb[pcq[j]][:, 0:N], AF.Square),
                         r=[("ps", pcq[j])], w=[("sq01", j)])
                    S.op("act", lambda e, j=j: e.activation(cqg[:, j, 0:N], psb[pcq[j]][:, 0:N], AF.Copy, scale=gq[:, j:j + 1]),
                         r=[("ps", pcq[j]), "gq"], w=[("cqg", j)])
                pss = PS()
                for j in range(2):
                    S.op("pe", lambda e, j=j: e.matmul(psb[pss][:, 0:N], ones_f[:], sq01[:, j, 0:N], start=(j == 0), stop=(j == 1)),
                         r=["ones_f", ("sq01", j)], w=[("ps", pss)])
                S.op("act", lambda e: e.activation(rq[:, 0:N], psb[pss][:, 0:N], AF.Sqrt, bias=EPS, scale=1.0 / 256),
                     r=[("ps", pss)], w=["rq"])
                S.op("dve", lambda e: e.reciprocal(rq[:, 0:N], rq[:, 0:N]), r=["rq"], w=["rq"])
                if not is_ctx:
                    S.dma(lambda e: [e.dma_start(out=Cc[64:96, 0:N], in_=C32o[:, tab0:tab0 + N]),
                                     e.dma_start(out=Sc[64:96, 0:N], in_=S32o[:, tab0:tab0 + N])], w=["CcSc"], n=2)
                for h in range(8):
                    pq, pqs = PS(), PS()
                    for j in range(2):
                        S.op("pe", lambda e, j=j, h=h, pq=pq: e.matmul(
                            psb[pq][0:96, 0:N], wuq_b[:, j, h * 96:(h + 1) * 96], cqg[:, j, 0:N], start=(j == 0), stop=(j == 1)),
                            r=[("wuq", 0), ("wuq", 1), ("cqg", 0), ("cqg", 1)], w=[("ps", pq)])
                    if is_ctx:
                        S.op("dve", lambda e, h=h, pq=pq: e.tensor_tensor(
                            QT[0:96, h, qc0:qc0 + N], psb[pq][0:96, 0:N], rq[0:96, 0:N], ALU.mult),
                            r=[("ps", pq), "rq"], w=[("QT", h, qc0)])
                        continue
                    for j in range(2):
                        S.op("pe", lambda e, j=j, h=h, pqs=pqs: e.matmul(
                            psb[pqs][0:96, 0:N], wuqsw_b[:, j, h * 96:(h + 1) * 96], cqg[:, j, 0:N], start=(j == 0), stop=(j == 1)),
                            r=[("wuqsw", 0), ("wuqsw", 1), ("cqg", 0), ("cqg", 1)], w=[("ps", pqs)])
                    S.op("dve", lambda e, h=h, pq=pq: e.tensor_tensor(
                        QT[0:64, h, qc0:qc0 + N], psb[pq][0:64, 0:N], rq[0:64, 0:N], ALU.mult),
                        r=[("ps", pq), "rq"], w=[("QTn", h, qc0)])
                    S.op("dve", lambda e, pq=pq: e.tensor_tensor(u1[64:96, 0:N], psb[pq][64:96, 0:N], Cc[64:96, 0:N], ALU.mult),
                         r=[("ps", pq), "CcSc"], w=["k1"])
                    S.op("dve", lambda e, pqs=pqs: e.tensor_tensor(u2[64:96, 0:N], psb[pqs][64:96, 0:N], Sc[64:96, 0:N], ALU.mult),
                         r=[("ps", pqs), "CcSc"], w=["k2"])
                    S.op("pool", lambda e: e.tensor_tensor(u3[64:96, 0:N], u1[64:96, 0:N], u2[64:96, 0:N], ALU.add),
                         r=["k1", "k2"], w=["u3"])
                    S.op("pool", lambda e, h=h: e.tensor_tensor(QT[64:96, h, qc0:qc0 + N], u3[64:96, 0:N], rq[64:96, 0:N], ALU.mult),
                         r=["u3", "rq"], w=[("QTr", h, qc0)])

            jobs = []
            jobs.append(dict(pre=lambda: set_mods(1), a=(ctx_b[0:128, :], 0, 0), post=None))
            jobs.append(dict(pre=None, a=(ctx_b[128:256, :], 0, 1),
                             post=lambda: (kv_side(0, 2, 8192, True), q_side(0, 2, 2048, True, 0))))
            for c in range(4):
                cb = (c + 1) % 2
                for s4 in range(4):
                    t = 4 * c + s4
                    jobs.append(dict(pre=(lambda: set_mods(0)) if (c == 0 and s4 == 0) else None,
                                     a=(x_own[t * 128:(t + 1) * 128, :], cb, s4),
                                     post=(lambda cb=cb, c=c: (kv_side(cb, 4, c * 512, False), q_side(cb, 4, c * 512, False, c * 512))) if s4 == 3 else None))
            prevB, prevPost = None, None
            for jb in jobs:
                if jb["pre"] is not None:
                    jb["pre"]()
                curB = norm_mod_T(*jb["a"])
                if prevB is not None:
                    prevB()
                    if prevPost is not None:
                        prevPost()
                prevB, prevPost = curB, jb["post"]
            prevB()
            if prevPost is not None:
                prevPost()

            S.dma(lambda e: [e.dma_start(out=ckv_loc, in_=ckvO[:]), e.dma_start(out=kr_loc, in_=krO[64:96, :])],
                  r=[("ckvnT", k0) for k0 in range(0, 2048, 512)] + [("krT", k0) for k0 in range(0, 2048, 512)], w=["kvloc"], n=2)
            S.barrier()
            S.cc(lambda e: e.collective_compute("AllGather", ALU.bypass, replica_groups=GROUPS, ins=[ckv_loc],
                                                outs=[ckv_all.rearrange("r d t -> (r d) t")]), w=["ckv_all"])
            S.cc(lambda e: e.collective_compute("AllGather", ALU.bypass, replica_groups=GROUPS, ins=[kr_loc],
                                                outs=[kr_all.rearrange("r d t -> (r d) t")]), w=["kr_all"])
            for c2 in range(2):
                S.cc(lambda e, c2=c2: e.collective_compute("AllGather", ALU.bypass, replica_groups=GROUPS, ins=[f_loc[c2]],
                                                           outs=[f_all[c2].rearrange("r i d -> (r i) d")]), w=[("f_all", c2)])
            S.dma(lambda e: e.dma_start(out=ckvnT[:, 0:8192].rearrange("d (r t) -> d r t", r=4), in_=ckv_all.rearrange("r d t -> d r t")),
                  r=["ckv_all"], w=["ckvnT_all"])
            S.dma(lambda e: e.dma_start(out=krT[64:96, 0:8192].rearrange("d (r t) -> d r t", r=4), in_=kr_all.rearrange("r d t -> d r t")),
                  r=["kr_all"], w=["krT_all"])
            dbg_dump("ckvnT", ckvnT[:], [128, NKEY], BF16, [("ckvnT", k0) for k0 in list(range(0, 8192, 512)) + [8192]])
            dbg_dump("krT", krT[64:96, :], [32, NKEY], BF16, [("krT", k0) for k0 in list(range(0, 8192, 512)) + [8192]])
            S.barrier()
            dbg_dump("QT", QT[:], [96, 8, NQRY], BF16, [])
            if "f" in dbg:
                o = dout("dbg_f", [NKEY, 512], BF16)
                S.dma(lambda e, o=o: e.dma_start(out=o, in_=f_scr), r=["f_scr"], w=["dbg_f"])
                outs.append("dbg_f")

        if stage >= 2 and "skip12" not in dbg:
            for R in (R1, R3, R4):
                R.reset()
            KT = [R1.alloc("KT", [96, NKEY], BF16) for _ in range(2)]
            Vh1 = R1.alloc("Vh", [128, NT, 128], BF16)
            PTw = [R1.alloc("PTw", [128, 1024], BF16) for _ in range(3)]
            rden = R1.alloc("rden", [64, 512], F32)
            dsb = R1.alloc("dsb", [128, 512], F32)
            wuk_b = R1.alloc("wuk_b", [128, 512], BF16)
            wuv_b = R1.alloc("wuv_b", [128, 512], BF16)
            OT = R3.alloc("OT", [64, 8, NQRY], BF16)
            wst2 = [R4.alloc("wst2", [128, 512], F32) for _ in range(2)]
            S.dma(lambda e: e.dma_start(out=wst2[0][:], in_=w_uk), w=[("wst2", 0)])
            S.dma(lambda e: e.dma_start(out=wst2[1][:], in_=w_uv), w=[("wst2", 1)])
            S.op("pool", lambda e: e.tensor_copy(wuk_b[:], wst2[0][:]), r=[("wst2", 0)], w=["wuk_b"])
            S.op("pool", lambda e: e.tensor_copy(wuv_b[:], wst2[1][:]), r=[("wst2", 1)], w=["wuv_b"])
            S.op("pool", lambda e: e.memset(Vh1[:, :, 64:128], 1.0), w=["Vones"])
            psr = [0]

            def PSs():
                i = psr[0] % 4
                psr[0] += 1
                return i
            chunks = [(c * 512, 512) for c in range(16)] + [(8192, 256)]
            SCALE0 = 96.0 ** -0.5

            def build_k(h):
                hb = h % 2
                for ci, (k0, N) in enumerate(chunks):
                    pi = PSs()
                    S.op("pe", lambda e, pi=pi, k0=k0, N=N: e.matmul(
                        psb[pi][0:64, 0:N], wuk_b[:, h * 64:(h + 1) * 64], ckvnT[:, k0:k0 + N], start=True, stop=True),
                        r=["wuk_b"], w=[("ps", pi)])
                    S.op("dve", lambda e, pi=pi, k0=k0, N=N: e.tensor_copy(KT[hb][0:64, k0:k0 + N], psb[pi][0:64, 0:N]),
                         r=[("ps", pi)], w=[("KTn", hb, ci)])
                S.op("pool", lambda e: e.tensor_copy(KT[hb][64:96, :], krT[64:96, :]), w=[("KTr", hb)])

            def build_v(h):
                for g in range(9):
                    nt = 8 if g < 8 else 2
                    pi = PSs()
                    for j in range(nt):
                        tt = g * 8 + j
                        S.op("pe", lambda e, pi=pi, j=j, tt=tt: e.matmul(
                            psb[pi][:, j * 64:(j + 1) * 64], ckvnT[:, tt * 128:(tt + 1) * 128], wuv_b[:, h * 64:(h + 1) * 64],
                            start=True, stop=True), r=["wuv_b"], w=[("ps", pi)])
                    S.op("dve", lambda e, pi=pi, g=g, nt=nt: e.tensor_copy(
                        Vh1[:, g * 8:g * 8 + nt, 0:64], psb[pi][:, 0:nt * 64].rearrange("p (a b) -> p a b", b=64)),
                        r=[("ps", pi)], w=[("V", g)])
            st0 = {"blk": 0, "pt": 0, "sp": 0}

            def attend(h, qc0, NQc, tiles):
                hb = h % 2
                ab = st0["blk"] % 2
                st0["blk"] += 1
                po, pd = 4 + ab, 6 + ab
                pairs = [(tiles[2 * i], tiles[2 * i + 1]) for i in range(len(tiles) // 2)]
                n = len(pairs)
                sbank = {}

                def Sm(i):
                    sp = st0["sp"] % 2
                    st0["sp"] += 1
                    sbank[i] = sp
                    for hf, tt in enumerate(pairs[i]):
                        S.op("pe", lambda e, hf=hf, tt=tt, sp=sp: e.matmul(psw[sp][:, hf * 512:hf * 512 + NQc], KT[hb][0:96, tt * 128:(tt + 1) * 128],
                                                                       QT[0:96, h, qc0:qc0 + NQc], start=True, stop=True),
                             r=[("KTn", hb, tt // 4), ("KTr", hb)], w=[("ps", 2 * sp), ("ps", 2 * sp + 1)])
                Sm(0)
                for i in range(n):
                    sp = sbank[i]
                    pbi = st0["pt"] % 3
                    st0["pt"] += 1
                    S.op("act", lambda e, sp=sp, pbi=pbi: e.activation(
                        PTw[pbi][:].rearrange("p (a b) -> p a b", a=2)[:, :, 0:NQc], psw[sp][:].rearrange("p (a b) -> p a b", a=2)[:, :, 0:NQc],
                        AF.Exp, scale=SCALE0), r=[("ps", 2 * sp), ("ps", 2 * sp + 1)], w=[("PTw", pbi)])
                    if i + 1 < n:
                        Sm(i + 1)
                    for hf, tt in enumerate(pairs[i]):
                        first = (i == 0 and hf == 0)
                        last = (i == n - 1 and hf == 1)
                        S.op("pe", lambda e, tt=tt, pbi=pbi, hf=hf, first=first, last=last: e.matmul(
                            psb[po][:, 0:NQc], Vh1[:, tt, :], PTw[pbi][:, hf * 512:hf * 512 + NQc], start=first, stop=last),
                            r=[("V", tt // 8), "Vones", ("PTw", pbi)], w=[("ps", po)])
                S.op("act", lambda e: e.copy(dsb[64:128, 0:NQc], psb[po][64:128, 0:NQc]), r=[("ps", po)], w=["dsb"])
                S.op("pe", lambda e: e.matmul(psb[pd][0:64, 0:NQc], ident_f[64:128, 64:128], dsb[64:128, 0:NQc], start=True, stop=True),
                     r=["dsb"], w=[("ps", pd)])
                S.op("dve", lambda e: e.reciprocal(rden[:, 0:NQc], psb[pd][0:64, 0:NQc]), r=[("ps", pd)], w=["rden"])
                S.op("dve", lambda e: e.tensor_tensor(OT[0:64, h, qc0:qc0 + NQc], psb[po][0:64, 0:NQc], rden[:, 0:NQc], ALU.mult),
                     r=[("ps", po), "rden"], w=[("OT", h, qc0)])

            build_k(0)
            for h in range(8):
                build_v(h)
                for qb in range(4):
                    if qb == 2 and h + 1 < 8:
                        build_k(h + 1)
                    attend(h, qb * 512, 512, list(range(NT)))
                attend(h, 2048, 256, [64, 65])
            S.barrier()
            dbg_dump("OT", OT[:], [64, 8, NQRY], BF16, [])

        if stage >= 3:
            for R in (R1, R2, R4):
                R.reset()
            F2 = R1.alloc("F2", [128, 64, 512], BF16)
            tA = [R2.alloc("tA", [128, 512], F32) for _ in range(2)]
            tB = [R2.alloc("tB", [128, 512], F32) for _ in range(2)]
            Hp = [R2.alloc("Hp", [128, 2, 512], BF16) for _ in range(3)]
            tw = R4.alloc("tw", [128, 3, 64], F32)
            S.dma(lambda e: e.dma_start(out=tw[:], in_=tw_d), w=["tw"])
            S.dma(lambda e: [e.dma_start(out=F2[(r_ * 2 + c_) * 16:(r_ * 2 + c_ + 1) * 16, :, :],
                                         in_=f_all[c_, r_].rearrange("(a b) ch -> a b ch", b=64)) for r_ in range(4) for c_ in range(2)],
                  w=["F2"], n=8)
            for t2 in range(64):
                pc, ps_ = PS(), PS()
                S.op("pe", lambda e, pc=pc, t2=t2: e.matmul(psb[pc][:, :], C128b[:], F2[:, t2, :], start=True, stop=True),
                     r=["F2"], w=[("ps", pc)])
                S.op("pe", lambda e, ps_=ps_, t2=t2: e.matmul(psb[ps_][:, :], S128b[:], F2[:, t2, :], start=True, stop=True),
                     r=["F2"], w=[("ps", ps_)])
                a2, h3 = t2 % 2, t2 % 3
                if "noTw" in dbg:
                    continue
                S.op("act", lambda e, ps_=ps_, t2=t2, a2=a2: e.activation(tA[a2][:], psb[ps_][:, :], AF.Copy, scale=tw[:, 1, t2:t2 + 1]),
                     r=[("ps", ps_), "tw"], w=[("tA", a2)])
                S.op("act", lambda e, pc=pc, t2=t2, a2=a2: e.activation(tB[a2][:], psb[pc][:, :], AF.Copy, scale=tw[:, 1, t2:t2 + 1]),
                     r=[("ps", pc), "tw"], w=[("tB", a2)])
                S.op("dve", lambda e, pc=pc, t2=t2, a2=a2, h3=h3: e.scalar_tensor_tensor(
                    Hp[h3][:, 0, :], psb[pc][:, :], tw[:, 0, t2:t2 + 1], tA[a2][:], ALU.mult, ALU.subtract),
                    r=[("ps", pc), ("tA", a2), "tw"], w=[("Hp", h3)])
                S.op("dve", lambda e, ps_=ps_, t2=t2, a2=a2, h3=h3: e.scalar_tensor_tensor(
                    Hp[h3][:, 1, :], psb[ps_][:, :], tw[:, 2, t2:t2 + 1], tB[a2][:], ALU.mult, ALU.subtract),
                    r=[("ps", ps_), ("tB", a2), "tw"], w=[("Hp", h3)])
                if "noHpDma" not in dbg:
                    S.dma(lambda e, t2=t2, h3=h3: e.dma_start(out=hp_scr[:, t2, :, :], in_=Hp[h3][:]), r=[("Hp", h3)], w=["hp_scr"])
            S.barrier()
            for R in (R1, R2):
                R.reset()
            if "stopA" in dbg:
                if "noDump" not in dbg:
                    o = dout("dbg_hp", [128, 64, 2, 512], BF16)
                    S.dma(lambda e, o=o: e.dma_start(out=o, in_=hp_scr), w=["dbg_hp"])
                    outs.append("dbg_hp")
                else:
                    o = dout("dbg_hp", [128, 2, 512], BF16)
                    S.dma(lambda e, o=o: e.dma_start(out=o, in_=hp_scr[:, 5, :, :]), w=["dbg_hp"])
                    outs.append("dbg_hp")
                stage = 2.5
        if stage >= 3:
            HpT = [R2.alloc("HpT", [64, 8, 2, 512], BF16) for _ in range(2)]
            ZT = R2.alloc("ZT", [128, 4, 2, 16, 128], BF16)
            Fc = R2.alloc("Fc", [128, 2, 512], BF16)
            ZcT = R2.alloc("ZcT", [128, 4, 2, 256], BF16)
            fmT = R4.alloc("fmT", [128, 4, NQRY], BF16)
            d64 = R4.alloc("d64", [64, 2, 32], BF16)
            S.dma(lambda e: e.dma_start(out=d64[:], in_=dft64o_d), w=["d64"])
            for k1b in range(16):
                bb = k1b % 2
                S.dma(lambda e, k1b=k1b, bb=bb: e.dma_start(
                    out=HpT[bb][:], in_=hp_scr[k1b * 8:(k1b + 1) * 8, :, :, :].rearrange("k t r c -> t k r c")), w=[("HpT", bb)])
                for g in range(4):
                    pz = PS()
                    zv = psb[pz][:, 0:256].rearrange("p (k r c) -> p k r c", k=8, r=2)
                    for kk in range(8):
                        gc = slice(g * 128, (g + 1) * 128)
                        zo = zv[:, kk, :, :].rearrange("p r c -> p (r c)")
                        S.op("pe", lambda e, zo=zo, kk=kk, gc=gc, bb=bb: e.matmul(zo, HpT[bb][:, kk, 0, gc], d64[:, 0, :],
                                                                               start=True, stop=False), r=[("HpT", bb), "d64"], w=[("ps", pz)])
                        S.op("pe", lambda e, zo=zo, kk=kk, gc=gc, bb=bb: e.matmul(zo, HpT[bb][:, kk, 1, gc], d64[:, 1, :],
                                                                               start=False, stop=True), r=[("HpT", bb), "d64"], w=[("ps", pz)])
                    dstz = ZT[:, g, :, :, k1b * 8:(k1b + 1) * 8]
                    srcz = zv.rearrange("p k r c -> p r c k")
                    if g % 2 == 0:
                        S.op("act", lambda e, dstz=dstz, srcz=srcz: e.copy(dstz, srcz), r=[("ps", pz)], w=[("ZT", g)])
                    else:
                        S.op("dve", lambda e, dstz=dstz, srcz=srcz: e.tensor_copy(dstz, srcz), r=[("ps", pz)], w=[("ZT", g)])
            S.barrier()
            NRM = 1.0 / math.sqrt(8192.0 * 128.0)
            for g in range(4):
                for kb in range(4):
                    pf = PS()
                    S.op("pe", lambda e, pf=pf, g=g, kb=kb: e.matmul(
                        psb[pf][:, :], C128b[:], ZT[:, g, 0, kb * 4:(kb + 1) * 4, :].rearrange("p a b -> p (a b)"), start=True, stop=False),
                        w=[("ps", pf)])
                    S.op("pe", lambda e, pf=pf, g=g, kb=kb: e.matmul(
                        psb[pf][:, :], S128b[:], ZT[:, g, 1, kb * 4:(kb + 1) * 4, :].rearrange("p a b -> p (a b)"), start=False, stop=True),
                        w=[("ps", pf)])
                    S.op("act", lambda e, pf=pf, g=g, kb=kb: e.activation(fmT[:, g, kb * 512:(kb + 1) * 512], psb[pf][:, :], AF.Copy, scale=NRM),
                         r=[("ps", pf)], w=[("fmT", g, kb)])
            S.dma(lambda e: e.dma_start(out=Fc[:], in_=f_scr[8192:8448, :].rearrange("(t p) c -> p t c", p=128)), w=["Fc"])
            NRMC = 1.0 / math.sqrt(256.0 * 128.0)
            for g in range(4):
                pz = PS()
                for ri, tab in ((0, C256b), (1, NS256b)):
                    for tl in range(2):
                        S.op("pe", lambda e, pz=pz, ri=ri, tab=tab, tl=tl, g=g: e.matmul(
                            psb[pz][:, ri * 256:(ri + 1) * 256], Fc[:, tl, g * 128:(g + 1) * 128], tab[:, tl, :],
                            start=(tl == 0), stop=(tl == 1)), r=["Fc", "dfttab"], w=[("ps", pz)])
                S.op("dve", lambda e, pz=pz, g=g: e.tensor_copy(ZcT[:, g, :, :], psb[pz][:, :].rearrange("p (r c) -> p r c", r=2)),
                     r=[("ps", pz)], w=[("ZcT", g)])
                pf = PS()
                S.op("pe", lambda e, pf=pf, g=g: e.matmul(psb[pf][:, 0:256], C128b[:], ZcT[:, g, 0, :], start=True, stop=False),
                     r=[("ZcT", g)], w=[("ps", pf)])
                S.op("pe", lambda e, pf=pf, g=g: e.matmul(psb[pf][:, 0:256], S128b[:], ZcT[:, g, 1, :], start=False, stop=True),
                     r=[("ZcT", g)], w=[("ps", pf)])
                S.op("act", lambda e, pf=pf, g=g: e.activation(fmT[:, g, 2048:2304], psb[pf][:, 0:256], AF.Copy, scale=NRMC),
                     r=[("ps", pf)], w=[("fmT", g, 4)])
            S.barrier()
            dbg_dump("fmT", fmT[:], [128, 4, NQRY], BF16, [])

        if stage >= 4:
            for R in (R1, R2):
                R.reset()
            h_lat = R1.alloc("h_lat", [128, 16, D], F32)
            wo_f_b = R2.alloc("wo_f_b", [128, 4, D], BF16)
            wo_a_b = R2.alloc("wo_a_b", [64, 8, D], BF16)
            wst3 = [R2.alloc("wst3", [128, D], F32) for _ in range(2)]
            g1_rep = [R2.alloc("g1_rep", [128, D], F32) for _ in range(2)]
            tmpy = [R2.alloc("tmpy", [128, 512], F32) for _ in range(2)]
            for g in range(4):
                S.dma(lambda e, g=g: e.dma_start(out=wst3[g % 2][:], in_=ab_w_o[g * 128:(g + 1) * 128, :]), w=[("wst3", g % 2)])
                S.op("pool", lambda e, g=g: e.tensor_copy(wo_f_b[:, g, :], wst3[g % 2][:]), r=[("wst3", g % 2)], w=["wo_f_b"])
            for hh in range(8):
                S.dma(lambda e, hh=hh: e.dma_start(out=wst3[hh % 2][0:64, :], in_=ab_w_o[512 + hh * 64:512 + (hh + 1) * 64, :]),
                      w=[("wst3", hh % 2)])
                S.op("pool", lambda e, hh=hh: e.tensor_copy(wo_a_b[0:64, hh, :], wst3[hh % 2][0:64, :]), r=[("wst3", hh % 2)], w=["wo_a_b"])
            load_mod(g1_rep[0], 0, 2, ("g1_rep", 0))
            load_mod(g1_rep[1], 1, 2, ("g1_rep", 1))
            S.dma(lambda e: e.dma_start(out=h_lat[:], in_=x_own.rearrange("(j p) d -> p j d", p=128)), w=["h_lat_in"])
            S.dma(lambda e: e.dma_start(out=h_ctx[:], in_=ctx_b.rearrange("(j p) d -> p j d", p=128)), w=["h_ctx_in"])
            yc = 0
            for j in range(NQ):
                isc = j >= 16
                for hf in range(2):
                    py = PS()
                    cs = slice(hf * 512, (hf + 1) * 512)
                    for g in range(4):
                        S.op("pe", lambda e, py=py, g=g, cs=cs, j=j: e.matmul(psb[py][:, :], fmT[:, g, j * 128:(j + 1) * 128], wo_f_b[:, g, cs],
                                                                          start=(g == 0), stop=False), r=["wo_f_b"], w=[("ps", py)])
                    for hh in range(8):
                        S.op("pe", lambda e, py=py, hh=hh, cs=cs, j=j: e.matmul(psb[py][:, :], OT[0:64, hh, j * 128:(j + 1) * 128], wo_a_b[0:64, hh, cs],
                                                                            start=False, stop=(hh == 7)), r=["wo_a_b"], w=[("ps", py)])
                    ti = yc % 2
                    yc += 1
                    hdst = h_ctx[:, j - 16, cs] if isc else h_lat[:, j, cs]
                    grep = g1_rep[1 if isc else 0]
                    S.op("dve", lambda e, py=py, ti=ti, cs=cs, grep=grep: e.tensor_tensor(tmpy[ti][:], psb[py][:, :], grep[:, cs], ALU.mult),
                         r=[("ps", py), ("g1_rep", 0), ("g1_rep", 1)], w=[("tmpy", ti)])
                    S.op("pool", lambda e, ti=ti, hdst=hdst: e.tensor_tensor(hdst, hdst, tmpy[ti][:], ALU.add),
                         r=[("tmpy", ti), "h_lat_in", "h_ctx_in"], w=[("h", j, hf)])
            S.barrier()
            S.dma(lambda e: [e.dma_start(out=h_scr[0:2048, :].rearrange("(j p) d -> p j d", p=128), in_=h_lat[:]),
                             e.dma_start(out=h_scr[2048:2304, :].rearrange("(j p) d -> p j d", p=128), in_=h_ctx[:])],
                  w=["h_scr"], n=2)
            if "hmix" in dbg:
                o = dout("dbg_hmix", [NQRY, D])
                S.dma(lambda e, o=o: [e.dma_start(out=o[0:2048, :].rearrange("(j p) d -> p j d", p=128), in_=h_lat[:]),
                                      e.dma_start(out=o[2048:2304, :].rearrange("(j p) d -> p j d", p=128), in_=h_ctx[:])],
                      w=["dbg_hmix"], n=2)
                outs.append("dbg_hmix")

        def moe(l, with_ctx):
            S.barrier()
            for R in (R1, R2, R3, R4):
                R.reset()
            NTm = 66 if with_ctx else 64
            NQm = 18 if with_ctx else 16
            NS = 1056 if with_ctx else 1024
            NC = NTm * 16
            R3b = Region(nc, R3.base, 16896, "Cb%d" % l)
            affA = R3.alloc("affA", [128, 66, 16], F32)
            totA = R3.alloc("totA", [128, 66, 16], F32)
            totB = R3.alloc("totB", [128, 66, 16], F32)
            mask_b = R3.alloc("mask_b", [128, 66, 16], BF16)
            R3.alloc("pad3", [128, 1056], BF16)
            aff_sb = R3.alloc("aff_sb", [128, 18, 16], F32)
            maskA = R3.alloc("maskA", [128, 66, 16], F32)
            posA = R3.alloc("posA", [128, 66, 16], F32)
            cmpA = R3.alloc("cmpA", [128, 66, 16], F32)
            thr = R4.alloc("thr", [128, 6, 2, 16], F32)
            tgt = R4.alloc("tgt", [128, 2, 16], F32)
            esel = R4.alloc("esel", [128, 4, 16], F32)
            ltq = R4.alloc("ltq", [128, 4], F32)
            cbe = R4.alloc("cbe", [128, 16], F32)
            ust = R4.alloc("ust", [128, 128], BF16)
            rowid = R4.alloc("rowid", [128, 64, 2], BF16)
            ident2 = ident_b
            posO = R4.alloc("posO", [128, 18, 16], F32)
            maskO = R4.alloc("maskO", [128, 18, 16], F32)
            gmO = R4.alloc("gmO", [128, 18, 16], F32)
            rowO = R4.alloc("rowO", [128, 18, 16], I32)
            tmpO = R4.alloc("tmpO", [128, 18, 16], F32)
            tmpO2 = R4.alloc("tmpO2", [128, 18, 16], F32)
            rt = R4.alloc("rt", [128, 4, 16], F32)
            baseq = R4.alloc("baseq", [128, 16], F32)
            wr_sb = R4.alloc("wr_sb", [128, 8, 16], F32)
            smx = R4.alloc("smx", [128, 8], F32)
            mctx = R4.alloc("mctx", [128, 2, D], BF16)
            S.dma(lambda e: [e.dma_start(out=tgt[:], in_=tgt_d), e.dma_start(out=esel[:], in_=esel_d),
                             e.dma_start(out=ltq[:], in_=ltq_d), e.dma_start(out=cbe[:], in_=cbe_d),
                             e.dma_start(out=ust[:], in_=ustrict_d), e.dma_start(out=rowid[:], in_=rowid_d),
                             e.dma_start(out=wr_sb[:], in_=w_router[l].rearrange("(k p) e -> p k e", p=128))],
                  w=["rt_consts"], n=7)
            ht = [R1.alloc("ht", [128, D], F32) for _ in range(2)]
            t1m = [R1.alloc("t1m", [128, D], F32) for _ in range(2)]
            m32 = [R1.alloc("m32", [128, D], F32) for _ in range(2)]
            mb = [R1.alloc("mb", [128, D], BF16) for _ in range(2)]
            junkm = R1.alloc("junkm", [128, D], BF16)
            m32T = [R1.alloc("m32T", [128, 8, 128], F32) for _ in range(2)]
            gs2 = [R2.alloc("gs2", [128, D], F32) for _ in range(2)]
            sh2 = [R2.alloc("sh2", [128, D], F32) for _ in range(2)]
            ex16 = R2.alloc("ex16", [128, 16], F32)
            for v in range(2 if with_ctx else 1):
                load_mod(sh2[v], v, 6 * l + 3, ("sh2", v))
                load_mod(t1m[0], v, 6 * l + 4, ("t1m", 0))
                load_rep(t1m[1], norm2[l, :], ("t1m", 1))
                S.op("dve", lambda e, v=v: e.scalar_tensor_tensor(gs2[v][:], t1m[0][:], 1.0, t1m[1][:], ALU.add, ALU.mult),
                     r=[("t1m", 0), ("t1m", 1)], w=[("gs2", v)])
            def p5_stageA(j):
                b2 = j % 2
                v = 1 if j >= 16 else 0
                S.dma(lambda e, j=j, b2=b2: e.dma_start(out=ht[b2][:], in_=h_scr[j * 128:(j + 1) * 128, :]), w=[("ht", b2)])
                S.op("act", lambda e, b2=b2: e.activation(junkm[:], ht[b2][:], AF.Square, accum_out=smx[:, b2:b2 + 1]),
                     r=[("ht", b2)], w=["junkm", ("ssm", b2)])
                S.op("act", lambda e, b2=b2: e.activation(smx[:, 2 + b2:3 + b2], smx[:, b2:b2 + 1], AF.Sqrt, bias=EPS, scale=1.0 / D),
                     r=[("ssm", b2)], w=[("sdm", b2)])
                S.op("dve", lambda e, b2=b2: e.reciprocal(smx[:, 2 + b2:3 + b2], smx[:, 2 + b2:3 + b2]), r=[("sdm", b2)], w=[("sdm", b2)])
                S.op("dve", lambda e, b2=b2, v=v: e.scalar_tensor_tensor(t1m[b2][:], ht[b2][:], smx[:, 2 + b2:3 + b2], gs2[v][:], ALU.mult, ALU.mult),
                     r=[("ht", b2), ("sdm", b2), ("gs2", v)], w=[("t1m", b2)])
                S.op("pool", lambda e, b2=b2, v=v: e.tensor_tensor(m32[b2][:], t1m[b2][:], sh2[v][:], ALU.add),
                     r=[("t1m", b2), ("sh2", v)], w=[("m32", b2)])
                if j < 16:
                    S.op("pool", lambda e, b2=b2: e.tensor_copy(mb[b2][:], m32[b2][:]), r=[("m32", b2)], w=[("mb", b2)])
                    S.dma(lambda e, j=j, b2=b2: e.dma_start(out=m_loc[j * 128:(j + 1) * 128, :], in_=mb[b2][:]), r=[("mb", b2)], w=["m_loc"])
                else:
                    S.op("pool", lambda e, b2=b2, j=j: e.tensor_copy(mctx[:, j - 16, :], m32[b2][:]), r=[("m32", b2)], w=[("mctx", j - 16)])

            def p5_stageB(j):
                b2 = j % 2
                for hf in range(2):
                    pt = PS()
                    for k4 in range(4):
                        k = hf * 4 + k4
                        S.op("pe", lambda e, pt=pt, k=k, k4=k4, b2=b2: e.transpose(psb[pt][:, k4 * 128:(k4 + 1) * 128], m32[b2][:, k * 128:(k + 1) * 128], ident_f[:]),
                             r=[("m32", b2)], w=[("ps", pt)])
                    S.op("act", lambda e, pt=pt, hf=hf, b2=b2: e.copy(m32T[b2][:, hf * 4:(hf + 1) * 4, :], psb[pt][:, :].rearrange("p (a b) -> p a b", a=4)),
                         r=[("ps", pt)], w=[("m32T", b2, hf)])
                pl = PS()
                for k in range(8):
                    S.op("pe", lambda e, pl=pl, k=k, b2=b2: e.matmul(psb[pl][:, 0:16], m32T[b2][:, k, :], wr_sb[:, k, :], start=(k == 0), stop=(k == 7)),
                         r=[("m32T", b2, 0), ("m32T", b2, 1), "rt_consts"], w=[("ps", pl)])
                S.op("dve", lambda e, pl=pl, b2=b2: e.tensor_reduce(smx[:, 4 + b2:5 + b2], psb[pl][:, 0:16], AX.X, ALU.max, negate=True),
                     r=[("ps", pl)], w=[("mx", b2)])
                S.op("act", lambda e, pl=pl, b2=b2: e.activation(ex16[:], psb[pl][:, 0:16], AF.Exp, bias=smx[:, 4 + b2:5 + b2], scale=1.0,
                                                               accum_out=smx[:, 6 + b2:7 + b2]),
                     r=[("ps", pl), ("mx", b2)], w=["ex16", ("sum", b2)])
                S.op("dve", lambda e, b2=b2: e.reciprocal(smx[:, 6 + b2:7 + b2], smx[:, 6 + b2:7 + b2]), r=[("sum", b2)], w=[("sum", b2)])
                S.op("dve", lambda e, b2=b2, j=j: e.tensor_scalar(aff_sb[:, j, :], ex16[:], smx[:, 6 + b2:7 + b2], None, ALU.mult),
                     r=["ex16", ("sum", b2)], w=["aff_sb"])
            p5_stageA(0)
            for j in range(NQm):
                if j + 1 < NQm:
                    p5_stageA(j + 1)
                p5_stageB(j)
            S.barrier()
            S.dma(lambda e: e.dma_start(out=aff_loc, in_=aff_sb[:, 0:16, :]), w=["aff_loc"])
            S.cc(lambda e: e.collective_compute("AllGather", ALU.bypass, replica_groups=GROUPS,
                                                ins=[aff_loc.rearrange("p j e -> p (j e)")],
                                                outs=[aff_all.rearrange("r p j e -> (r p) (j e)")]), r=["aff_loc"], w=["aff_all"])
            for c in range(4):
                S.cc(lambda e, c=c: e.collective_compute("AllGather", ALU.bypass, replica_groups=GROUPS,
                                                         ins=[m_loc[c * 512:(c + 1) * 512, :]],
                                                         outs=[m_all[c].rearrange("r i d -> (r i) d")]), w=[("m_all", c)])
            S.dma(lambda e: e.dma_start(out=affA[:, 0:64, :].rearrange("p (r j) e -> p r j e", r=4),
                                        in_=aff_all.rearrange("r p j e -> p r j e")), r=["aff_all"], w=["affA"])
            if with_ctx:
                S.op("dve", lambda e: e.tensor_copy(affA[:, 64:66, :], aff_sb[:, 16:18, :]), w=["affAc"])
            KN = 2 if with_ctx else 1
            lo, hi, mid, cnt, ge, tq = (thr[:, i, 0:KN, :] for i in range(6))
            S.op("dve", lambda e: e.memset(thr[:, 0, :, :], 0.0), w=["lo"])
            kinds = [(0, 0, 64)] + ([(1, 64, 66)] if with_ctx else [])
            for it in range(30):
                step = 2.0 ** -(it + 1)
                S.op("dve", lambda e, step=step: e.tensor_scalar(mid, lo, step, None, ALU.add), r=["lo"], w=["mid"])
                for (kd, t0, t1_) in kinds:
                    S.op("dve", lambda e, kd=kd, t0=t0, t1_=t1_: e.tensor_tensor(
                        cmpA[:, t0:t1_, :], affA[:, t0:t1_, :], thr[:, 2, kd:kd + 1, :].to_broadcast([128, t1_ - t0, 16]), ALU.is_ge),
                        r=["mid", "affA", "affAc"], w=[("cmp", kd)])
                    S.op("dve", lambda e, kd=kd, t0=t0, t1_=t1_: e.tensor_reduce(
                        thr[:, 3, kd, :], cmpA[:, t0:t1_, :].rearrange("p t e -> p e t"), AX.X, ALU.add),
                        r=[("cmp", kd)], w=[("cnt", kd)])
                pcn = PS()
                S.op("pe", lambda e, pcn=pcn: e.matmul(psb[pcn][:, 0:KN * 16], ones_f[:], cnt.rearrange("p k e -> p (k e)"), start=True, stop=True),
                     r=[("cnt", 0), ("cnt", 1)], w=[("ps", pcn)])
                S.op("dve", lambda e, pcn=pcn: e.tensor_tensor(ge, psb[pcn][:, 0:KN * 16].rearrange("p (k e) -> p k e", k=KN), tgt[:, 0:KN, :], ALU.is_ge),
                     r=[("ps", pcn), "rt_consts"], w=["ge"])
                S.op("dve", lambda e, step=step: e.scalar_tensor_tensor(lo, ge, step, lo, ALU.mult, ALU.add), r=["ge", "mid"], w=["lo"])
            for (kd, t0, t1_) in kinds:
                S.op("dve", lambda e, kd=kd, t0=t0, t1_=t1_: e.tensor_tensor(
                    maskA[:, t0:t1_, :], affA[:, t0:t1_, :], thr[:, 0, kd:kd + 1, :].to_broadcast([128, t1_ - t0, 16]), ALU.is_ge),
                    r=["lo", "affA", "affAc"], w=[("mask", kd)])
            MK = [("mask", 0), ("mask", 1)]
            S.op("pool", lambda e: e.tensor_copy(mask_b[:, 0:NTm, :], maskA[:, 0:NTm, :]), r=MK, w=["mask_b"])
            S.op("dve", lambda e: e.tensor_tensor(maskO[:, 0:16, :], aff_sb[:, 0:16, :], thr[:, 0, 0:1, :].to_broadcast([128, 16, 16]), ALU.is_ge),
                 r=["lo"], w=["maskO"])
            if with_ctx:
                S.op("dve", lambda e: e.tensor_copy(maskO[:, 16:18, :], maskA[:, 64:66, :]), r=MK, w=["maskOc"])
            S.op("dve", lambda e: e.tensor_tensor(gmO[:, 0:NQm, :], aff_sb[:, 0:NQm, :], maskO[:, 0:NQm, :], ALU.mult),
                 r=["maskO", "maskOc"], w=["gmO"])
            mbf = mask_b[:, 0:NTm, :].rearrange("p t e -> p (t e)")
            posf = posA[:, 0:NTm, :].rearrange("p t e -> p (t e)")
            totf = totA[:, 0:NTm, :].rearrange("p t e -> p (t e)")
            c0 = 0
            while c0 < NC:
                n = min(512, NC - c0)
                pw, ptt = PS(), PS()
                S.op("pe", lambda e, pw=pw, c0=c0, n=n: e.matmul(psb[pw][:, 0:n], ust[:], mbf[:, c0:c0 + n], start=True, stop=True),
                     r=["mask_b", "rt_consts"], w=[("ps", pw)])
                S.op("pe", lambda e, ptt=ptt, c0=c0, n=n: e.matmul(psb[ptt][:, 0:n], ones_b[:], mbf[:, c0:c0 + n], start=True, stop=True),
                     r=["mask_b"], w=[("ps", ptt)])
                S.op("act", lambda e, pw=pw, c0=c0, n=n: e.copy(posf[:, c0:c0 + n], psb[pw][:, 0:n]), r=[("ps", pw)], w=["posw"])
                S.op("act", lambda e, ptt=ptt, c0=c0, n=n: e.copy(totf[:, c0:c0 + n], psb[ptt][:, 0:n]), r=[("ps", ptt)], w=["tot"])
                c0 += n
            S.op("dve", lambda e: e.tensor_reduce(rt[:], totA[:, 0:64, :].rearrange("p (r j) e -> p r e j", r=4), AX.X, ALU.add),
                 r=["tot"], w=["rt"])
            S.op("dve", lambda e: e.tensor_tensor(rt[:], rt[:], ltq[:, :].unsqueeze(2).to_broadcast([128, 4, 16]), ALU.mult),
                 r=["rt", "rt_consts"], w=["rt"])
            S.op("dve", lambda e: e.tensor_reduce(baseq[:], rt[:].rearrange("p r e -> p e r"), AX.X, ALU.add), r=["rt"], w=["baseq"])
            src, dst = totA, totB
            S.op("dve", lambda e: e.tensor_copy(cmpA[:, 0:NTm, :], totA[:, 0:NTm, :]), r=["tot"], w=["tot0"])
            sh = 1
            while sh < 64:
                S.op("dve", lambda e, src=src, dst=dst, sh=sh: e.tensor_copy(dst[:, 0:sh, :], src[:, 0:sh, :]), r=["tot", "scan"], w=["scan_a"])
                S.op("dve", lambda e, src=src, dst=dst, sh=sh: e.tensor_tensor(dst[:, sh:64, :], src[:, sh:64, :], src[:, 0:64 - sh, :], ALU.add),
                     r=["tot", "scan", "scan_a"], w=["scan"])
                src, dst = dst, src
                sh *= 2
            incl = src
            if with_ctx:
                S.op("dve", lambda e, incl=incl: e.tensor_copy(incl[:, 64:65, :], cmpA[:, 64:65, :]), r=["tot0", "scan"], w=["scanc"])
                S.op("dve", lambda e, incl=incl: e.tensor_tensor(incl[:, 65:66, :], cmpA[:, 64:65, :], cmpA[:, 65:66, :], ALU.add),
                     r=["tot0", "scan"], w=["scanc2"])
            S.op("dve", lambda e, incl=incl: e.tensor_tensor(incl[:, 0:NTm, :], incl[:, 0:NTm, :], cmpA[:, 0:NTm, :], ALU.subtract),
                 r=["scan", "scanc", "scanc2", "tot0"], w=["excl"])
            S.op("dve", lambda e, incl=incl: e.tensor_tensor(posA[:, 0:NTm, :], posA[:, 0:NTm, :], incl[:, 0:NTm, :], ALU.add),
                 r=["excl", "posw"], w=["pos"])
            mo_b = mask_b
            S.op("pool", lambda e: e.tensor_copy(mo_b[:, 0:16, :], maskO[:, 0:16, :]), r=["maskO", "posw", "tot"], w=["mo_b"])
            mof = mo_b[:, 0:16, :].rearrange("p t e -> p (t e)")
            pw, ptt = PS(), PS()
            S.op("pe", lambda e, pw=pw: e.matmul(psb[pw][:, 0:256], ust[:], mof, start=True, stop=True), r=["mo_b"], w=[("ps", pw)])
            S.op("pe", lambda e, ptt=ptt: e.matmul(psb[ptt][:, 0:256], ones_b[:], mof, start=True, stop=True), r=["mo_b"], w=[("ps", ptt)])
            S.op("act", lambda e, pw=pw: e.copy(posO[:, 0:16, :].rearrange("p t e -> p (t e)"), psb[pw][:, 0:256]), r=[("ps", pw)], w=["posOw"])
            S.op("act", lambda e, ptt=ptt: e.copy(tmpO[:, 0:16, :].rearrange("p t e -> p (t e)"), psb[ptt][:, 0:256]), r=[("ps", ptt)], w=["totO"])
            S.op("dve", lambda e: e.tensor_copy(cmpA[:, 0:16, :], tmpO[:, 0:16, :]), r=["totO", "pos", "excl"], w=["totO0"])
            srcO, dstO = tmpO, tmpO2
            sh = 1
            while sh < 16:
                S.op("dve", lambda e, srcO=srcO, dstO=dstO, sh=sh: e.tensor_copy(dstO[:, 0:sh, :], srcO[:, 0:sh, :]), r=["totO", "scanO"], w=["scanO_a"])
                S.op("dve", lambda e, srcO=srcO, dstO=dstO, sh=sh: e.tensor_tensor(dstO[:, sh:16, :], srcO[:, sh:16, :], srcO[:, 0:16 - sh, :], ALU.add),
                     r=["totO", "scanO", "scanO_a"], w=["scanO"])
                srcO, dstO = dstO, srcO
                sh *= 2
            inclO = srcO
            S.op("dve", lambda e, inclO=inclO: e.tensor_tensor(inclO[:, 0:16, :], inclO[:, 0:16, :], cmpA[:, 0:16, :], ALU.subtract),
                 r=["scanO", "totO0"], w=["exclO"])
            S.op("dve", lambda e, inclO=inclO: e.tensor_tensor(inclO[:, 0:16, :], inclO[:, 0:16, :], baseq[:, :].unsqueeze(1).to_broadcast([128, 16, 16]), ALU.add),
                 r=["exclO", "baseq"], w=["exclO2"])
            S.op("dve", lambda e, inclO=inclO: e.tensor_tensor(posO[:, 0:16, :], posO[:, 0:16, :], inclO[:, 0:16, :], ALU.add),
                 r=["exclO2", "posOw"], w=["posO"])
            if with_ctx:
                S.op("dve", lambda e: e.tensor_scalar(posO[:, 16:18, :], posA[:, 64:66, :], 1024.0, None, ALU.add), r=["pos"], w=["posOc"])
            PO = posO[:, 0:NQm, :]; TA = tmpO[:, 0:NQm, :]; TB = tmpO2[:, 0:NQm, :]
            S.op("dve", lambda e: e.tensor_scalar(TA, PO, 352.0, 1056.0, ALU.is_ge, ALU.mult), r=["posO", "posOc", "exclO2"], w=["TA"])
            S.op("dve", lambda e: e.tensor_scalar(TB, PO, 704.0, 1056.0, ALU.is_ge, ALU.mult), r=["posO", "posOc", "exclO2"], w=["TB"])
            S.op("dve", lambda e: e.tensor_tensor(TA, TA, TB, ALU.add), r=["TA", "TB"], w=["TA"])
            S.op("dve", lambda e: e.tensor_tensor(TA, TA, PO, ALU.add), r=["TA"], w=["TA"])
            S.op("dve", lambda e: e.tensor_tensor(TA, TA, cbe[:, :].unsqueeze(1).to_broadcast([128, NQm, 16]), ALU.add), r=["TA"], w=["TA"])
            S.op("dve", lambda e: e.tensor_scalar(TB, maskO[:, 0:NQm, :], -1.0e8, 1.0e8, ALU.mult, ALU.add), r=["TB", "TA", "maskO", "maskOc"], w=["TB"])
            S.op("dve", lambda e: e.tensor_tensor(TA, TA, TB, ALU.add), r=["TA", "TB"], w=["TA"])
            S.op("dve", lambda e: e.tensor_copy(rowO[:, 0:NQm, :], TA), r=["TA"], w=["rowO"])
            S.barrier()
            dbg_dump("posA%d" % l, posA[:, 0:NTm, :], [128, NTm, 16], F32, [])
            dbg_dump("maskA%d" % l, maskA[:, 0:NTm, :], [128, NTm, 16], F32, [])
            dbg_dump("affA%d" % l, affA[:, 0:NTm, :], [128, NTm, 16], F32, [])
            dbg_dump("posO%d" % l, posO[:, 0:NQm, :], [128, NQm, 16], F32, [])
            dbg_dump("rowO%d" % l, rowO[:, 0:NQm, :], [128, NQm, 16], I32, [])
            dbg_dump("gmO%d" % l, gmO[:, 0:NQm, :], [128, NQm, 16], F32, [])
            if "stopR" in dbg:
                return
            for R in (R1, R2):
                R.reset()
            xsT = R1.alloc("xsT", [128, 8, 1056], BF16)
            hT = R1.alloc("hT", [128, 16, 1056], BF16)
            xs = [R1.alloc("xs", [128, D], BF16) for _ in range(4)]
            sa = [R1.alloc("sa", [128, 512], BF16) for _ in range(2)]
            wgu = [R2.alloc("wgu", [128, 2, 8, 256], BF16) for _ in range(2)]
            wd_b = R2.alloc("wd_b", [128, 16, 512], BF16)
            stg = [R2.alloc("stg", [128, 8, 256], F32) for _ in range(3)]
            iota = R2.alloc("iota", [128, 1024], F32)
            oh = [R2.alloc("oh", [128, 1024], BF16) for _ in range(2)]
            ohc = R2.alloc("ohc", [128, 2, 32], BF16)
            pj = R2.alloc("pj", [128, 66], F32); mj = R2.alloc("mj", [128, 66], F32)
            idxr = R2.alloc("idxr", [2, 1024], F32)
            idxc = R2.alloc("idxc", [128, 8, 2], F32)
            idxf = R2.alloc("idxf", [128, 8], F32)
            idxi = R2.alloc("idxi", [128, 8], I32)
            ybuf = [R2.alloc("ybuf", [128, 512], BF16) for _ in range(2)]
            S.dma(lambda e: e.dma_start(out=iota[:], in_=iota_d), w=["iota"])
            sc = {"stg": 0, "oh": 0, "xs": 0, "sa": 0, "yb": 0, "wgu": 0}
            MALL = [("m_all", c) for c in range(4)]
            m_flat = m_all.rearrange("c r i d -> (c r i) d")
            ps_rng[0], ps_rng[1] = 0, 6
            xsT2 = R3b.alloc("xsT2", [128, 8, 1056], BF16)
            XST = [xsT, xsT] if "samexst" in dbg else [xsT, xsT2]
            nbs = [(0, 512), (512, 512)] + ([(1024, 32)] if with_ctx else [])
            sts = [(st * 128, 128) for st in range(8)] + ([(1024, 32)] if with_ctx else [])

            def prep_select(je):
                S.op("dve", lambda e: e.tensor_tensor(cmpA[:, 0:NTm, :], posA[:, 0:NTm, :], esel[:, je:je + 1, :].to_broadcast([128, NTm, 16]), ALU.mult),
                     r=["pj", "mj"], w=["cmpsel"])
                S.op("dve", lambda e: e.tensor_reduce(pj[:, 0:NTm], cmpA[:, 0:NTm, :], AX.X, ALU.add), r=["cmpsel"], w=["pj"])
                S.op("dve", lambda e: e.tensor_tensor(cmpA[:, 0:NTm, :], maskA[:, 0:NTm, :], esel[:, je:je + 1, :].to_broadcast([128, NTm, 16]), ALU.mult),
                     r=["pj"], w=["cmpsel"])
                S.op("dve", lambda e: e.tensor_reduce(mj[:, 0:NTm], cmpA[:, 0:NTm, :], AX.X, ALU.add), r=["cmpsel"], w=["mj"])

            def prep_oh(t):
                oi = t % 2
                S.op("dve", lambda e: e.tensor_scalar(oh[oi][:], iota[:], pj[:, t:t + 1], mj[:, t:t + 1], ALU.is_equal, ALU.mult),
                     r=["pj", "mj", "iota"], w=[("oh", oi)])

            def prep_idxmm(t):
                oi = t % 2
                for hf, pr in ((0, 6), (1, 7)):
                    S.op("pe", lambda e, hf=hf, pr=pr: e.matmul(psb[pr][0:2, :], rowid[:, t, :], oh[oi][:, hf * 512:(hf + 1) * 512],
                                                                start=(t == 0), stop=(t == 63)), r=[("oh", oi)], w=[("ps", pr)])

            def prep_finalize():
                S.op("act", lambda e: e.copy(idxr[:, 0:512], psb[6][0:2, :]), r=[("ps", 6)], w=["idxr0"])
                S.op("act", lambda e: e.copy(idxr[:, 512:1024], psb[7][0:2, :]), r=[("ps", 7)], w=["idxr1"])
                pi2 = PS()
                for st in range(8):
                    S.op("pe", lambda e, st=st: e.matmul(psb[pi2][:, st * 2:st * 2 + 2], idxr[0:2, st * 128:(st + 1) * 128], ident_f[0:2, 0:2],
                                                         start=True, stop=True), r=["idxr0", "idxr1"], w=[("ps", pi2)])
                S.op("dve", lambda e: e.tensor_copy(idxc[:], psb[pi2][:, 0:16].rearrange("p (s c) -> p s c", c=2)), r=[("ps", pi2)], w=["idxc"])
                S.op("dve", lambda e: e.scalar_tensor_tensor(idxf[:], idxc[:, :, 1], 64.0, idxc[:, :, 0], ALU.mult, ALU.add), r=["idxc"], w=["idxf"])
                S.op("dve", lambda e: e.tensor_copy(idxi[:], idxf[:]), r=["idxf"], w=["idxi"])

            def prep_gather(st):
                xi = st % 4
                S.dma(lambda e: e.indirect_dma_start(
                    out=xs[xi][:], out_offset=None, in_=m_flat, in_offset=bass.IndirectOffsetOnAxis(ap=idxi[:, st:st + 1], axis=0),
                    bounds_check=S.pool_regs[8191], oob_is_err=False), r=["idxi"] + MALL, w=[("xs", xi)], eng="pool")

            def prep_transpose(je, st):
                xi = st % 4
                X = XST[je % 2]
                ptr = PS()
                psT = psb[ptr][:].bitcast(BF16).rearrange("p (k c) -> p k c", k=8)
                for k in range(8):
                    S.op("pe", lambda e, k=k: e.transpose(psT[:, k, :], xs[xi][:, k * 128:(k + 1) * 128], ident_b[:]),
                         r=[("xs", xi)], w=[("ps", ptr)])
                if st % 2 == 0:
                    S.op("act", lambda e: e.copy(X[:, :, st * 128:(st + 1) * 128], psT), r=[("ps", ptr)], w=[("xsT", je % 2, st)])
                else:
                    S.op("dve", lambda e: e.tensor_copy(X[:, :, st * 128:(st + 1) * 128], psT), r=[("ps", ptr)], w=[("xsT", je % 2, st)])

            def prep_ctx(je):
                X = XST[je % 2]
                for tl in range(2):
                    S.op("dve", lambda e, tl=tl: e.tensor_scalar(ohc[:, tl, :], iota[:, 0:32], pj[:, 64 + tl:65 + tl], mj[:, 64 + tl:65 + tl], ALU.is_equal, ALU.mult),
                         r=["pj", "mj", "iota"], w=[("ohc", tl)])
                pcx = PS()
                for k in range(8):
                    for tl in range(2):
                        S.op("pe", lambda e, k=k, tl=tl: e.matmul(psb[pcx][:, k * 32:(k + 1) * 32], mctx[:, tl, k * 128:(k + 1) * 128], ohc[:, tl, :],
                                                                  start=(tl == 0), stop=(tl == 1)), r=[("ohc", 0), ("ohc", 1)], w=[("ps", pcx)])
                S.op("act", lambda e: e.copy(X[:, :, 1024:1056], psb[pcx][:, 0:256].rearrange("p (k c) -> p k c", k=8)), r=[("ps", pcx)], w=[("xsT", je % 2, 8)])

            prep_select(0)
            for t in range(64):
                prep_oh(t)
                prep_idxmm(t)
            prep_finalize()
            for st in range(8):
                prep_gather(st)
                prep_transpose(0, st)
            if with_ctx:
                prep_ctx(0)
            for je in range(4):
                nxt = je + 1 < 4
                if "serialprep" in dbg:
                    if je > 0:
                        prep_select(je)
                        for t in range(64):
                            prep_oh(t)
                            prep_idxmm(t)
                        prep_finalize()
                        for st in range(8):
                            prep_gather(st)
                            prep_transpose(je, st)
                        if with_ctx:
                            prep_ctx(je)
                    nxt = False
                X = XST[je % 2]
                XS = [("xsT", je % 2, st) for st in range(9 if with_ctx else 8)]
                if nxt:
                    prep_select(je + 1)
                tq = list(range(64)) if nxt else []
                for fc in range(8):
                    wi = sc["wgu"] % 2; sc["wgu"] += 1
                    for gi, wsrc in ((0, moe_g), (1, moe_u)):
                        si = sc["stg"] % 3; sc["stg"] += 1
                        S.dma(lambda e, si=si, wsrc=wsrc, fc=fc, je=je: e.dma_start(
                            out=stg[si][:], in_=wsrc[l, je, :, fc * 256:(fc + 1) * 256].rearrange("(k p) f -> p k f", p=128)), w=[("stg", si)])
                        S.op("act", lambda e, si=si, wi=wi, gi=gi: e.copy(wgu[wi][:, gi, :, :], stg[si][:]), r=[("stg", si)], w=[("wgu", wi, gi)])
                    for f2 in range(2):
                        f = fc * 2 + f2
                        for (n0, nn) in nbs:
                            mine = [tq.pop(0) for _ in range(min(2, len(tq)))]
                            for t in mine:
                                prep_oh(t)
                            pa, pu = PS(), PS()
                            for k in range(8):
                                S.op("pe", lambda e, pa=pa, k=k, wi=wi, f2=f2, n0=n0, nn=nn, X=X: e.matmul(
                                    psb[pa][:, 0:nn], wgu[wi][:, 0, k, f2 * 128:(f2 + 1) * 128], X[:, k, n0:n0 + nn], start=(k == 0), stop=(k == 7)),
                                    r=[("wgu", wi, 0)] + XS, w=[("ps", pa)])
                            for k in range(8):
                                S.op("pe", lambda e, pu=pu, k=k, wi=wi, f2=f2, n0=n0, nn=nn, X=X: e.matmul(
                                    psb[pu][:, 0:nn], wgu[wi][:, 1, k, f2 * 128:(f2 + 1) * 128], X[:, k, n0:n0 + nn], start=(k == 0), stop=(k == 7)),
                                    r=[("wgu", wi, 1)] + XS, w=[("ps", pu)])
                            ai = sc["sa"] % 2; sc["sa"] += 1
                            S.op("act", lambda e, pa=pa, ai=ai, nn=nn: e.activation(sa[ai][:, 0:nn], psb[pa][:, 0:nn], AF.Silu), r=[("ps", pa)], w=[("sa", ai)])
                            S.op("dve", lambda e, pu=pu, ai=ai, f=f, n0=n0, nn=nn: e.tensor_tensor(hT[:, f, n0:n0 + nn], psb[pu][:, 0:nn], sa[ai][:, 0:nn], ALU.mult),
                                 r=[("ps", pu), ("sa", ai)], w=[("hT", f)])
                            for t in mine:
                                prep_idxmm(t)
                while tq:
                    t = tq.pop(0)
                    prep_oh(t)
                    prep_idxmm(t)
                if nxt:
                    prep_finalize()
                HT = [("hT", f) for f in range(16)]
                ydmas = []
                gq = list(range(8)) if nxt else []
                tqx = []
                for hf in range(2):
                    for f4 in range(4):
                        si = sc["stg"] % 3; sc["stg"] += 1
                        S.dma(lambda e, si=si, f4=f4, hf=hf, je=je: e.dma_start(
                            out=stg[si][:].rearrange("p (a b) c -> p a (b c)", a=4),
                            in_=moe_d[l, je, f4 * 512:(f4 + 1) * 512, hf * 512:(hf + 1) * 512].rearrange("(a p) c -> p a c", p=128)), w=[("stg", si)])
                        S.op("act", lambda e, si=si, f4=f4: e.copy(wd_b[:, f4 * 4:(f4 + 1) * 4, :], stg[si][:].rearrange("p (a b) c -> p a (b c)", a=4)),
                             r=[("stg", si)], w=[("wd_b", f4)])
                    for (s0, sn) in sts:
                        if gq:
                            g_ = gq.pop(0)
                            prep_gather(g_)
                            tqx.append(g_)
                        py = PS()
                        for f in range(16):
                            S.op("pe", lambda e, py=py, f=f, s0=s0, sn=sn: e.matmul(psb[py][0:sn, :], hT[:, f, s0:s0 + sn], wd_b[:, f, :], start=(f == 0), stop=(f == 15)),
                                 r=HT + [("wd_b", i) for i in range(4)], w=[("ps", py)])
                        yi = sc["yb"] % 2; sc["yb"] += 1
                        S.op("act", lambda e, py=py, yi=yi, sn=sn: e.copy(ybuf[yi][0:sn, :], psb[py][0:sn, :]), r=[("ps", py)], w=[("ybuf", yi)])
                        pieces = []
                        s_ = s0
                        while s_ < s0 + sn:
                            ch = s_ // 352
                            e1 = min(s0 + sn, (ch + 1) * 352)
                            pieces.append((s_, e1, ch))
                            s_ = e1
                        yd = S.dma(lambda e, pieces=pieces, yi=yi, s0=s0, hf=hf, je=je: [
                            e.dma_start(out=y_loc[3 * je + ch, a - ch * 352:b_ - ch * 352, hf * 512:(hf + 1) * 512], in_=ybuf[yi][a - s0:b_ - s0, :])
                            for (a, b_, ch) in pieces], r=[("ybuf", yi)], w=[("y_loc", je)], n=len(pieces))
                        ydmas.append(yd)
                        if len(tqx) > 1 or (tqx and not gq):
                            prep_transpose(je + 1, tqx.pop(0))
                while tqx:
                    prep_transpose(je + 1, tqx.pop(0))
                if nxt and with_ctx:
                    prep_ctx(je + 1)
                for c3 in range(3):
                    S.cc(lambda e, c3=c3, je=je: e.collective_compute("AllGather", ALU.bypass, replica_groups=GROUPS,
                                                                     ins=[y_loc[3 * je + c3]], outs=[y_all[3 * je + c3].rearrange("r i d -> (r i) d")]),
                         r=[("y_loc", je)], w=[("y_all", je)], after=ydmas)
            ps_rng[0], ps_rng[1] = 0, 8
            S.barrier()
            for R in (R1, R2):
                R.reset()
            G = [R1.alloc("G", [128, D], BF16) for _ in range(6)]
            dgm = [R1.alloc("dgm", [128, 128], BF16) for _ in range(6)]
            acc = [R1.alloc("acc", [128, D], F32) for _ in range(2)]
            hcb = [R1.alloc("hcb", [128, D], F32) for _ in range(2)]
            g2r = [R2.alloc("g2r", [128, D], F32) for _ in range(2)]
            for v in range(2 if with_ctx else 1):
                load_mod(g2r[v], v, 6 * l + 5, ("g2r", v))
            for gi in range(6):
                S.op("pool", lambda e, gi=gi: e.memset(G[gi][:], 0.0), w=[("G", gi)])
            y_flat = y_all.rearrange("c r i d -> (c r i) d")
            gc = 0

            def _dbg_ind(j, ee, gi):
                if "trace_ind" in dbg:
                    print("IND", j, ee, gi, flush=True)
                return None
            for j in range(NQm):
                b2 = j % 2
                v = 1 if j >= 16 else 0
                S.dma(lambda e, j=j, b2=b2: e.dma_start(out=hcb[b2][:], in_=h_scr[j * 128:(j + 1) * 128, :]), w=[("hcb", b2)])
                pyc = [PS(), PS()]
                for ee in range(16):
                    gi = gc % 6; gc += 1
                    S.dma(lambda e, j=j, ee=ee, gi=gi: _dbg_ind(j, ee, gi) or e.indirect_dma_start(
                        out=G[gi][:], out_offset=None, in_=y_flat, in_offset=bass.IndirectOffsetOnAxis(ap=rowO[:].rearrange("p j e -> p (j e)")[:, j * 16 + ee:j * 16 + ee + 1], axis=0),
                        bounds_check=S.pool_regs[12 * 4 * 352 - 1], oob_is_err=False), w=[("G", gi)], eng="pool")
                    di = gi
                    S.op("dve", lambda e, di=di, j=j, ee=ee: e.tensor_scalar(dgm[di][:], ident_b[:], gmO[:, j, ee:ee + 1], None, ALU.mult), w=[("dgm", di)])
                    for hf in range(2):
                        S.op("pe", lambda e, hf=hf, gi=gi, di=di, ee=ee, pyc=pyc: e.matmul(psb[pyc[hf]][:, :], dgm[di][:], G[gi][:, hf * 512:(hf + 1) * 512],
                                                                                  start=(ee == 0), stop=(ee == 15)),
                             r=[("G", gi), ("dgm", di)], w=[("ps", pyc[hf])])
                for hf in range(2):
                    S.op("dve", lambda e, hf=hf, b2=b2, v=v, pyc=pyc: e.tensor_tensor(acc[b2][:, hf * 512:(hf + 1) * 512], psb[pyc[hf]][:, :], g2r[v][:, hf * 512:(hf + 1) * 512], ALU.mult),
                         r=[("ps", pyc[hf]), ("g2r", v)], w=[("acc", b2)])
                S.op("dve", lambda e, b2=b2: e.tensor_tensor(hcb[b2][:], hcb[b2][:], acc[b2][:], ALU.add), r=[("acc", b2), ("hcb", b2)], w=[("hcb", b2)])
                S.dma(lambda e, j=j, b2=b2: e.dma_start(out=h_scr[j * 128:(j + 1) * 128, :], in_=hcb[b2][:]), r=[("hcb", b2)], w=["h_scr"])
            S.barrier()

        if stage >= 5:
            moe(0, True)
            if "hmoe" in dbg:
                o = dout("dbg_hmoe", [NQRY, D])
                S.dma(lambda e, o=o: e.dma_start(out=o, in_=h_scr), w=["dbg_hmoe"])
                outs.append("dbg_hmoe")

        if stage >= 6:
            S.barrier()
            for R in (R1, R2, R3, R4):
                R.reset()
            QT1 = R2.alloc("QT1", [128, 8, 2048], BF16)
            kTs = R2.alloc("kTs", [128, 2, 2048], BF16)
            kTc = R2.alloc("kTc", [128, 2, 256], BF16)
            vctx = R2.alloc("vctx", [128, 2, 256], BF16)
            cwin_b = R3.alloc("cwin_b", [128, 8, 1536], BF16)
            gsr = [R3.alloc("gsr", [128, D], F32) for _ in range(2)]
            cosr = R2.alloc("cosr", [128, 16, 128], F32)
            sinr = R2.alloc("sinr", [128, 16, 128], F32)
            xt1 = [R1.alloc("xt1", [128, D], F32) for _ in range(2)]
            t11 = [R1.alloc("t11", [128, D], F32) for _ in range(2)]
            ab1 = [R1.alloc("ab1", [128, D], BF16) for _ in range(2)]
            junk1 = R1.alloc("junk1", [128, D], BF16)
            aTt = [R1.alloc("aTt", [128, 8, 128], BF16) for _ in range(2)]
            sq1 = R1.alloc("sq1", [128, 10, 128], F32)
            qn1 = R1.alloc("qn1", [128, 10, 128], F32)
            tr1 = R1.alloc("tr1", [128, 10, 128], F32)
            tr2 = R1.alloc("tr2", [128, 10, 128], F32)
            qr1 = [R1.alloc("qr1", [128, 10, 128], BF16) for _ in range(2)]
            vb1 = [R1.alloc("vb1", [128, 256], BF16) for _ in range(2)]
            shr = [R4.alloc("shr", [128, D], F32) for _ in range(2)]
            gqr = R4.alloc("gqr", [128, 128], F32); gkr = R4.alloc("gkr", [128, 128], F32)
            ss1 = R4.alloc("ss1", [128, 8], F32)
            rs1 = R4.alloc("rs1", [128, 2, 10], F32)
            wst4 = [R4.alloc("wst4", [128, 768], F32) for _ in range(2)]
            wc = 0
            for k in range(8):
                for hf in range(2):
                    wi = wc % 2; wc += 1
                    S.dma(lambda e, k=k, hf=hf, wi=wi: e.dma_start(out=wst4[wi][:], in_=c_w_in[k * 128:(k + 1) * 128, hf * 768:(hf + 1) * 768]), w=[("wst4", wi)])
                    S.op("pool", lambda e, k=k, hf=hf, wi=wi: e.tensor_copy(cwin_b[:, k, hf * 768:(hf + 1) * 768], wst4[wi][:]), r=[("wst4", wi)], w=["cwin_b"])
            S.dma(lambda e: [e.dma_start(out=cosr[:], in_=cos1o), e.dma_start(out=sinr[:], in_=sin1o)], w=["ropet"], n=2)
            load_rep(gqr, c_q_gain, "gqr"); load_rep(gkr, c_k_gain, "gkr")
            for v in range(2):
                load_mod(shr[v], v, 6 + 0, ("shr", v))
                load_mod(t11[0], v, 6 + 1, ("t11", 0))
                load_rep(t11[1], norm1[1, :], ("t11", 1))
                S.op("dve", lambda e, v=v: e.scalar_tensor_tensor(gsr[v][:], t11[0][:], 1.0, t11[1][:], ALU.add, ALU.mult),
                     r=[("t11", 0), ("t11", 1)], w=[("gsr", v)])
            def p9_stageA(j):
                b2 = j % 2
                isc = j >= 16
                v = 1 if isc else 0
                S.dma(lambda e, j=j, b2=b2: e.dma_start(out=xt1[b2][:], in_=h_scr[j * 128:(j + 1) * 128, :]), w=[("xt1", b2)])
                S.op("act", lambda e, b2=b2: e.activation(junk1[:], xt1[b2][:], AF.Square, accum_out=ss1[:, b2:b2 + 1]), r=[("xt1", b2)], w=["junk1", ("ss1", b2)])
                S.op("act", lambda e, b2=b2: e.activation(ss1[:, 2 + b2:3 + b2], ss1[:, b2:b2 + 1], AF.Sqrt, bias=EPS, scale=1.0 / D), r=[("ss1", b2)], w=[("sd1", b2)])
                S.op("dve", lambda e, b2=b2: e.reciprocal(ss1[:, 2 + b2:3 + b2], ss1[:, 2 + b2:3 + b2]), r=[("sd1", b2)], w=[("sd1", b2)])
                S.op("dve", lambda e, b2=b2, v=v: e.scalar_tensor_tensor(t11[b2][:], xt1[b2][:], ss1[:, 2 + b2:3 + b2], gsr[v][:], ALU.mult, ALU.mult),
                     r=[("xt1", b2), ("sd1", b2), ("gsr", v)], w=[("t11", b2)])
                S.op("pool", lambda e, b2=b2, v=v: e.tensor_tensor(ab1[b2][:], t11[b2][:], shr[v][:], ALU.add), r=[("t11", b2), ("shr", v)], w=[("ab1", b2)])

            def p9_stageB(j):
                b2 = j % 2
                isc = j >= 16
                v = 1 if isc else 0
                pt = PS()
                psT = psb[pt][:].bitcast(BF16).rearrange("p (k c) -> p k c", k=8)
                for k in range(8):
                    S.op("pe", lambda e, psT=psT, k=k, b2=b2: e.transpose(psT[:, k, :], ab1[b2][:, k * 128:(k + 1) * 128], ident_b[:]), r=[("ab1", b2)], w=[("ps", pt)])
                S.op("act", lambda e, psT=psT, b2=b2: e.copy(aTt[b2][:], psT), r=[("ps", pt)], w=[("aTt", b2)])
                pq = [PS(), PS(), PS()]
                for c3 in range(3):
                    for k in range(8):
                        S.op("pe", lambda e, c3=c3, k=k, b2=b2, pq=pq: e.matmul(psb[pq[c3]][:, :], aTt[b2][:, k, :], cwin_b[:, k, c3 * 512:(c3 + 1) * 512],
                                                                            start=(k == 0), stop=(k == 7)), r=[("aTt", b2), "cwin_b"], w=[("ps", pq[c3])])
                for c3, (h0, nh) in enumerate(((0, 4), (4, 4), (8, 2))):
                    S.op("act", lambda e, c3=c3, h0=h0, nh=nh, pq=pq: e.activation(
                        sq1[:, h0:h0 + nh, :], psb[pq[c3]][:, 0:nh * 128].rearrange("p (h d) -> p h d", d=128), AF.Square),
                        r=[("ps", pq[c3])], w=[("sq1", c3)])
                S.op("dve", lambda e, b2=b2: e.tensor_reduce(rs1[:, b2, :], sq1[:], AX.X, ALU.add), r=[("sq1", 0), ("sq1", 1), ("sq1", 2)], w=[("rs1", b2)])
                S.op("act", lambda e, b2=b2: e.activation(rs1[:, b2, :], rs1[:, b2, :], AF.Sqrt, bias=EPS, scale=1.0 / 128), r=[("rs1", b2)], w=[("rs1", b2)])
                S.op("dve", lambda e, b2=b2: e.reciprocal(rs1[:, b2, :], rs1[:, b2, :]), r=[("rs1", b2)], w=[("rs1", b2)])
                for c3, (h0, nh) in enumerate(((0, 4), (4, 4), (8, 2))):
                    S.op("dve", lambda e, c3=c3, h0=h0, nh=nh, pq=pq, b2=b2: e.tensor_tensor(
                        qn1[:, h0:h0 + nh, :], psb[pq[c3]][:, 0:nh * 128].rearrange("p (h d) -> p h d", d=128),
                        rs1[:, b2, h0:h0 + nh].unsqueeze(2).to_broadcast([128, nh, 128]), ALU.mult),
                        r=[("ps", pq[c3]), ("rs1", b2)], w=[("qn1", c3)])
                vi = j % 2
                S.op("act", lambda e, pq=pq, vi=vi: e.copy(vb1[vi][:], psb[pq[2]][:, 256:512]), r=[("ps", pq[2])], w=[("vb1", vi)])
                QN = [("qn1", 0), ("qn1", 1), ("qn1", 2)]
                S.op("pool", lambda e: e.tensor_tensor(qn1[:, 0:8, :], qn1[:, 0:8, :], gqr[:, :].unsqueeze(1).to_broadcast([128, 8, 128]), ALU.mult),
                     r=QN + ["gqr"], w=["qg"])
                S.op("pool", lambda e: e.tensor_tensor(qn1[:, 8:10, :], qn1[:, 8:10, :], gkr[:, :].unsqueeze(1).to_broadcast([128, 2, 128]), ALU.mult),
                     r=QN + ["gkr"], w=["kg"])
                qi = j % 2
                if not isc:
                    S.op("dve", lambda e, j=j: e.tensor_tensor(tr1[:], qn1[:], cosr[:, j, :].unsqueeze(1).to_broadcast([128, 10, 128]), ALU.mult),
                         r=QN + ["qg", "kg", "ropet"], w=["tr1"])
                    qv = qn1[:].rearrange("p h (g a c) -> p h g a c", g=2, a=2)
                    tv = tr2[:].rearrange("p h (g a c) -> p h g a c", g=2, a=2)
                    sv = sinr[:, j, :].rearrange("p (g a c) -> p g a c", g=2, a=2)
                    for a in range(2):
                        S.op("pool", lambda e, a=a, qv=qv, tv=tv, sv=sv: e.tensor_tensor(
                            tv[:, :, :, a, :], qv[:, :, :, 1 - a, :], sv[:, :, a, :].unsqueeze(1).to_broadcast([128, 10, 2, 32]), ALU.mult),
                            r=QN + ["qg", "kg", "ropet"], w=[("tr2", a)])
                    S.op("dve", lambda e, qi=qi: e.tensor_tensor(qr1[qi][:], tr1[:], tr2[:], ALU.add), r=["tr1", ("tr2", 0), ("tr2", 1)], w=[("qr1", qi)])
                else:
                    S.op("dve", lambda e, qi=qi: e.tensor_copy(qr1[qi][:], qn1[:]), r=QN + ["qg", "kg"], w=[("qr1", qi)])
                if not isc:
                    ptq = PS()
                    psQ = psb[ptq][:].bitcast(BF16).rearrange("p (k c) -> p k c", k=8)
                    for hh in range(8):
                        S.op("pe", lambda e, psQ=psQ, hh=hh, qi=qi: e.transpose(psQ[:, hh, :], qr1[qi][:, hh, :], ident_b[:]), r=[("qr1", qi)], w=[("ps", ptq)])
                    S.op("act", lambda e, psQ=psQ, j=j: e.copy(QT1[:, :, j * 128:(j + 1) * 128], psQ), r=[("ps", ptq)], w=[("QT1", j)])
                ptk = PS()
                psK = psb[ptk][:].bitcast(BF16).rearrange("p (k c) -> p k c", k=8)
                for hh in range(2):
                    S.op("pe", lambda e, psK=psK, hh=hh, qi=qi: e.transpose(psK[:, hh, :], qr1[qi][:, 8 + hh, :], ident_b[:]), r=[("qr1", qi)], w=[("ps", ptk)])
                if not isc:
                    S.op("dve", lambda e, psK=psK, j=j: e.tensor_copy(kTs[:, :, j * 128:(j + 1) * 128], psK[:, 0:2, :]), r=[("ps", ptk)], w=["kTs"])
                    S.dma(lambda e, j=j, vi=vi: e.dma_start(out=v_loc[j * 128:(j + 1) * 128, :], in_=vb1[vi][:]), r=[("vb1", vi)], w=["v_loc"])
                else:
                    S.op("dve", lambda e, psK=psK, j=j: e.tensor_copy(kTc[:, :, (j - 16) * 128:(j - 15) * 128], psK[:, 0:2, :]), r=[("ps", ptk)], w=["kTc"])
                    S.op("pool", lambda e, j=j, vi=vi: e.tensor_copy(vctx[:, j - 16, :], vb1[vi][:]), r=[("vb1", vi)], w=["vctx"])
            p9_stageA(0)
            for j in range(NQ):
                if j + 1 < NQ:
                    p9_stageA(j + 1)
                p9_stageB(j)
            S.dma(lambda e: e.dma_start(out=kT_loc, in_=kTs[:]), r=["kTs"], w=["kT_loc"])
            S.barrier()
            S.cc(lambda e: e.collective_compute("AllGather", ALU.bypass, replica_groups=GROUPS,
                                                ins=[kT_loc.rearrange("d g t -> d (g t)")], outs=[kT_all.rearrange("r d g t -> (r d) (g t)")]), w=["kT_all"])
            S.cc(lambda e: e.collective_compute("AllGather", ALU.bypass, replica_groups=GROUPS, ins=[v_loc], outs=[v_all]), w=["v_all"])
            dbg_dump("QT1", QT1[:], [128, 8, 2048], BF16, [])
            for R in (R1, R3, R4):
                R.reset()
            KTg = R1.alloc("KTg", [128, NKEY], BF16)
            Vg = R1.alloc("Vg", [128, NT, 128], BF16)
            PT1 = [R1.alloc("PT1", [128, 1024], BF16) for _ in range(3)]
            rden1 = R1.alloc("rden1", [128, 512], F32)
            OT1 = R3.alloc("OT1", [128, 8, 2048], BF16)
            SCALE1 = 128.0 ** -0.5
            st1 = {"blk": 0, "pt": 0, "sp": 0, "ds": 0}
            dsum = [R1.alloc("dsum", [128, 512], BF16) for _ in range(3)]
            pend = [None]
            for g in range(2):
                S.dma(lambda e, g=g: e.dma_start(out=KTg[:, 0:8192].rearrange("d (r t) -> d r t", r=4), in_=kT_all[:, :, g, :].rearrange("r d t -> d r t")),
                      r=["kT_all"], w=["KTg"])
                S.op("dve", lambda e, g=g: e.tensor_copy(KTg[:, 8192:8448], kTc[:, g, :]), w=["KTgc"])
                S.dma(lambda e, g=g: e.dma_start(out=Vg[:, 0:64, :], in_=v_all[:, g * 128:(g + 1) * 128].rearrange("(t p) c -> p t c", p=128)),
                      r=["v_all"], w=["Vg"])
                S.op("pool", lambda e, g=g: e.tensor_copy(Vg[:, 64:66, :], vctx[:, :, g * 128:(g + 1) * 128]), w=["Vgc"])
                for hq in range(4):
                    hh = 4 * g + hq
                    for qb in range(4):
                        ab = st1["blk"] % 2; st1["blk"] += 1
                        po, pd = 4 + ab, 6 + ab
                        n = NT // 2
                        sbank = {}

                        def Sm(i, hh=hh, qb=qb, sbank=sbank):
                            sp = st1["sp"] % 2
                            st1["sp"] += 1
                            sbank[i] = sp
                            for hf in range(2):
                                tt = 2 * i + hf
                                S.op("pe", lambda e, sp=sp, hf=hf, tt=tt: e.matmul(psw[sp][:, hf * 512:(hf + 1) * 512], KTg[:, tt * 128:(tt + 1) * 128],
                                                                               QT1[:, hh, qb * 512:(qb + 1) * 512], start=True, stop=True),
                                     r=["KTg", "KTgc"], w=[("ps", 2 * sp), ("ps", 2 * sp + 1)])
                        Sm(0)
                        for i in range(n):
                            sp = sbank[i]
                            pbi = st1["pt"] % 3; st1["pt"] += 1
                            S.op("act", lambda e, sp=sp, pbi=pbi: e.activation(PT1[pbi][:], psw[sp][:], AF.Exp, scale=SCALE1),
                                 r=[("ps", 2 * sp), ("ps", 2 * sp + 1)], w=[("PT1", pbi)])
                            di = st1["ds"] % 3; st1["ds"] += 1
                            S.op("dve", lambda e, pbi=pbi, di=di: e.tensor_tensor(dsum[di][:], PT1[pbi][:, 0:512], PT1[pbi][:, 512:1024], ALU.add),
                                 r=[("PT1", pbi)], w=[("dsum", di)])
                            if i + 1 < n:
                                Sm(i + 1)
                            for hf in range(2):
                                tt = 2 * i + hf
                                first = (i == 0 and hf == 0)
                                last = (i == n - 1 and hf == 1)
                                S.op("pe", lambda e, tt=tt, pbi=pbi, hf=hf, po=po, first=first, last=last: e.matmul(
                                    psb[po][:, :], Vg[:, tt, :], PT1[pbi][:, hf * 512:(hf + 1) * 512], start=first, stop=last),
                                    r=["Vg", "Vgc", ("PT1", pbi)], w=[("ps", po)])
                            if pend[0] is not None:
                                pend[0]()
                            pend[0] = (lambda di=di, pd=pd, i=i: S.op(
                                "pe", lambda e: e.matmul(psb[pd][:, :], ones_b[:], dsum[di][:], start=(i == 0), stop=(i == n - 1)),
                                r=[("dsum", di)], w=[("ps", pd)]))
                        pend[0]()
                        pend[0] = None
                        S.op("dve", lambda e, pd=pd: e.reciprocal(rden1[:], psb[pd][:, :]), r=[("ps", pd)], w=["rden1"])
                        S.op("dve", lambda e, po=po, hh=hh, qb=qb: e.tensor_tensor(OT1[:, hh, qb * 512:(qb + 1) * 512], psb[po][:, :], rden1[:], ALU.mult),
                             r=[("ps", po), "rden1"], w=[("OT1", hh, qb)])
            S.barrier()
            dbg_dump("OT1", OT1[:], [128, 8, 2048], BF16, [])
            for R in (R1, R2, R4):
                R.reset()
            cwo_b = R2.alloc("cwo_b", [128, 8, D], BF16)
            wst5 = [R2.alloc("wst5", [128, D], F32) for _ in range(2)]
            g1r = R2.alloc("g1r", [128, D], F32)
            tmy = [R2.alloc("tmy", [128, 512], F32) for _ in range(2)]
            hb1 = [R1.alloc("hb1", [128, D], F32) for _ in range(3)]
            for k in range(8):
                S.dma(lambda e, k=k: e.dma_start(out=wst5[k % 2][:], in_=c_w_o[k * 128:(k + 1) * 128, :]), w=[("wst5", k % 2)])
                S.op("pool", lambda e, k=k: e.tensor_copy(cwo_b[:, k, :], wst5[k % 2][:]), r=[("wst5", k % 2)], w=["cwo_b"])
            load_mod(g1r, 0, 6 + 2, "g1r")
            yc = 0
            for j in range(16):
                b3 = j % 3
                S.dma(lambda e, j=j, b3=b3: e.dma_start(out=hb1[b3][:], in_=h_scr[j * 128:(j + 1) * 128, :]), w=[("hb1", b3)])
                for hf in range(2):
                    py = PS()
                    cs = slice(hf * 512, (hf + 1) * 512)
                    for hh in range(8):
                        S.op("pe", lambda e, py=py, hh=hh, cs=cs, j=j: e.matmul(psb[py][:, :], OT1[:, hh, j * 128:(j + 1) * 128], cwo_b[:, hh, cs], start=(hh == 0), stop=(hh == 7)),
                             r=["cwo_b"], w=[("ps", py)])
                    ti = yc % 2; yc += 1
                    S.op("dve", lambda e, py=py, ti=ti, cs=cs: e.tensor_tensor(tmy[ti][:], psb[py][:, :], g1r[:, cs], ALU.mult), r=[("ps", py), "g1r"], w=[("tmy", ti)])
                    S.op("pool", lambda e, ti=ti, b3=b3, cs=cs: e.tensor_tensor(hb1[b3][:, cs], hb1[b3][:, cs], tmy[ti][:], ALU.add), r=[("tmy", ti), ("hb1", b3)], w=[("hb1", b3)])
                S.dma(lambda e, j=j, b3=b3: e.dma_start(out=h_scr[j * 128:(j + 1) * 128, :], in_=hb1[b3][:]), r=[("hb1", b3)], w=["h_scr"])
            S.barrier()
            if "hmix1" in dbg:
                o = dout("dbg_hmix1", [2048, D])
                S.dma(lambda e, o=o: e.dma_start(out=o, in_=h_scr[0:2048, :]), w=["dbg_hmix1"])
                outs.append("dbg_hmix1")

        if stage >= 7:
            moe(1, False)
            for R in (R1, R2):
                R.reset()
            out_d = dout("out", [2048, D])
            hf1 = [R1.alloc("hf1", [128, D], F32) for _ in range(3)]
            of1 = [R1.alloc("of1", [128, D], F32) for _ in range(2)]
            junkf = R1.alloc("junkf", [128, D], BF16)
            fnr = R2.alloc("fnr", [128, D], F32)
            ssf = R2.alloc("ssf", [128, 8], F32)
            load_rep(fnr, final_norm, "fnr")
            odmas = []
            for j in range(16):
                b3 = j % 3; b2 = j % 2
                S.dma(lambda e, j=j, b3=b3: e.dma_start(out=hf1[b3][:], in_=h_scr[j * 128:(j + 1) * 128, :]), w=[("hf1", b3)])
                S.op("act", lambda e, b3=b3, b2=b2: e.activation(junkf[:], hf1[b3][:], AF.Square, accum_out=ssf[:, b2:b2 + 1]), r=[("hf1", b3)], w=["junkf", ("ssf", b2)])
                S.op("act", lambda e, b2=b2: e.activation(ssf[:, 2 + b2:3 + b2], ssf[:, b2:b2 + 1], AF.Sqrt, bias=EPS, scale=1.0 / D), r=[("ssf", b2)], w=[("sdf", b2)])
                S.op("dve", lambda e, b2=b2: e.reciprocal(ssf[:, 2 + b2:3 + b2], ssf[:, 2 + b2:3 + b2]), r=[("sdf", b2)], w=[("sdf", b2)])
                S.op("dve", lambda e, b3=b3, b2=b2: e.scalar_tensor_tensor(of1[b2][:], hf1[b3][:], ssf[:, 2 + b2:3 + b2], fnr[:], ALU.mult, ALU.mult),
                     r=[("hf1", b3), ("sdf", b2), "fnr"], w=[("of1", b2)])
                odmas.append(S.dma(lambda e, j=j, b2=b2: e.dma_start(out=out_d[j * 128:(j + 1) * 128, :], in_=of1[b2][:]), r=[("of1", b2)], w=["out"]))
            S.op("sp", None, after=odmas)
            outs.append("out")

        S.op("sp", None, r=list(outs))
        S.barrier()
        S.emit()
    return nc, outs
_DIN_NAMES = ['x_own', 'ctx_b', 'cT', 'ada_s', 'ada_bs', 'w_in', 'w_uq', 'w_uqsw', 'w_uk', 'w_uv', 'ab_w_o', 'q_norm', 'kv_norm', 'norm1', 'norm2', 'C32o', 'S32o', 'ident_f', 'C128b', 'S128b', 'tw', 'dft64o', 'C256b', 'NS256b', 'moe_g', 'moe_u', 'moe_d', 'esel', 'ltq', 'w_router', 'rowid', 'iota_row', 'ustrict', 'tgt', 'cbe', 'c_w_in', 'c_w_o', 'c_q_gain', 'c_k_gain', 'final_norm', 'cos1o', 'sin1o']


_INPUT_NAMES = None


def kernel(**inputs):
    per_core = host_prep(inputs)
    nc, outs = build(stage=99, dbg=())
    names = [a for a in per_core[0].keys()]
    import re as _re
    in_maps = [{n: m[n] for n in _DIN_NAMES} for m in per_core]
    res = run_bass_kernel_spmd(nc, in_maps, core_ids=list(range(8)))
    out = np.empty((2, S_LAT, D), np.float32)
    for r in range(8):
        b, q = r // 4, r % 4
        out[b, 2048 * q:2048 * (q + 1)] = res.results[r]["out"]
    return out
```
